# Optimizing a Trainium2 kernel written in Bass

```python
import jax, jax.numpy as jnp
from jax import lax
import numpy as np

D_MODEL = 1024
BATCH = 8
SEQ = 4096
DEPTH = 4

CHUNK = 64
EPS = 1e-6

RET_HEADS = 4
RET_QK_DIM = 128
RET_V_DIM = 128
RET_QK_WIDTH = RET_HEADS * RET_QK_DIM
RET_V_WIDTH = RET_HEADS * RET_V_DIM
ROPE_BASE = 10000.0

SGU_GROUPS = 4
SGU_GROUP_DIM = 128
SGU_WIDTH = SGU_GROUPS * SGU_GROUP_DIM
SGU_BLOCK = 128

AB_IN = 2 * RET_QK_WIDTH + 2 * RET_V_WIDTH + 2 * SGU_WIDTH
AB_MIX = RET_V_WIDTH + SGU_WIDTH

MLSTM_HEADS = 4
MLSTM_QK_DIM = 128
MLSTM_V_DIM = 256
MLSTM_QK_WIDTH = MLSTM_HEADS * MLSTM_QK_DIM
MLSTM_V_WIDTH = MLSTM_HEADS * MLSTM_V_DIM
C_IN = 2 * MLSTM_QK_WIDTH + 2 * MLSTM_V_WIDTH + 2 * MLSTM_HEADS

D_FF = -(-8 * D_MODEL // (3 * 256)) * 256

kernel_name = "hybrid_retention_sgu_mlstm_trunk"


def rmsnorm(t, g):
    tf = t.astype(jnp.float32)
    y = tf * lax.rsqrt(jnp.mean(tf * tf, axis=-1, keepdims=True) + EPS)
    return (y * g.astype(jnp.float32)).astype(t.dtype)


def norm_last(t):
    mu = jnp.mean(t, axis=-1, keepdims=True)
    c = t - mu
    return c * lax.rsqrt(jnp.mean(c * c, axis=-1, keepdims=True) + EPS)


def rope(t, pos):
    half = t.shape[-1] // 2
    inv = ROPE_BASE ** (-jnp.arange(half, dtype=jnp.float32) / half)
    ang = pos[:, None] * inv[None, :]
    cos = jnp.cos(ang)[None, :, None, :]
    sin = jnp.sin(ang)[None, :, None, :]
    t1, t2 = t[..., :half], t[..., half:]
    return jnp.concatenate([t1 * cos - t2 * sin, t1 * sin + t2 * cos], axis=-1)


def retention(q, k, v):
    B, S, H, dk = q.shape
    dv = v.shape[-1]
    n = S // CHUNK
    log_g = jnp.log1p(-jnp.power(2.0, -5.0 - jnp.arange(H, dtype=jnp.float32)))
    idx = jnp.arange(CHUNK, dtype=jnp.float32)
    d_intra = jnp.exp(log_g[:, None, None] * jnp.abs(idx[:, None] - idx[None, :]))
    q_dec = jnp.exp(log_g[:, None] * (idx + 1.0)).T
    k_dec = jnp.exp(log_g[:, None] * (CHUNK - 1.0 - idx)).T
    chunk_dec = jnp.exp(log_g * CHUNK)
    qc = q.reshape(B, n, CHUNK, H, dk)
    kc = k.reshape(B, n, CHUNK, H, dk)
    vc = v.reshape(B, n, CHUNK, H, dv)
    scores = jnp.einsum('bnchd,bnshd->bnhcs', qc, kc) * d_intra
    intra = jnp.einsum('bnhcs,bnshe->bnche', scores, vc)
    kv = jnp.einsum('bnshd,bnshe->bnhde', kc * k_dec[:, :, None], vc)

    def step(state, kv_j):
        return state * chunk_dec[None, :, None, None] + kv_j, state

    _, prev = lax.scan(step, jnp.zeros((B, H, dk, dv), jnp.float32), jnp.moveaxis(kv, 1, 0))
    prev = jnp.moveaxis(prev, 0, 1)
    cross = jnp.einsum('bnchd,bnhde->bnche', qc * q_dec[:, :, None], prev)
    return (intra + cross).reshape(B, S, H, dv)


def spatial_gating(u, v, w_s, b_s):
    B, S, G, dg = v.shape
    nb = S // SGU_BLOCK
    chunk_id = jnp.arange(SGU_BLOCK) // CHUNK
    mask = chunk_id[None, :] <= chunk_id[:, None]
    w = jnp.where(mask[None], w_s, 0.0)
    vb = v.reshape(B, nb, SGU_BLOCK, G, dg)
    s = jnp.einsum('gpq,bnqgd->bnpgd', w, vb) + b_s.T[:, :, None]
    return u * s.reshape(B, S, G, dg)


def retention_sgu_mixer(hn, w_in, w_out, ret_norm_g, sgu_ln_g, sgu_ln_b, w_s, b_s):
    B, S, _ = hn.shape
    f32 = jnp.float32
    proj = (hn @ w_in).astype(f32)
    cuts = np.cumsum([RET_QK_WIDTH, RET_QK_WIDTH, RET_V_WIDTH, RET_V_WIDTH, SGU_WIDTH]).tolist()
    q, k, v, g, u, vs = jnp.split(proj, cuts, axis=-1)
    pos = jnp.arange(S, dtype=f32)
    q = rope(q.reshape(B, S, RET_HEADS, RET_QK_DIM), pos)
    k = rope(k.reshape(B, S, RET_HEADS, RET_QK_DIM), pos) * (RET_QK_DIM ** -0.5)
    v = v.reshape(B, S, RET_HEADS, RET_V_DIM)
    r = norm_last(retention(q, k, v)) * ret_norm_g.astype(f32)
    a = jax.nn.silu(g) * r.reshape(B, S, RET_V_WIDTH)
    u = jax.nn.gelu(u)
    vs = norm_last(jax.nn.gelu(vs)) * sgu_ln_g.astype(f32) + sgu_ln_b.astype(f32)
    bo = spatial_gating(u.reshape(B, S, SGU_GROUPS, SGU_GROUP_DIM),
                        vs.reshape(B, S, SGU_GROUPS, SGU_GROUP_DIM),
                        w_s.astype(f32), b_s.astype(f32)).reshape(B, S, SGU_WIDTH)
    mixed = jnp.concatenate([a, bo], axis=-1).astype(hn.dtype)
    return mixed @ w_out


def mlstm(q, k, v, log_i, log_f):
    B, S, H, dk = q.shape
    dv = v.shape[-1]
    n = S // CHUNK
    causal = jnp.tril(jnp.ones((CHUNK, CHUNK), dtype=bool))

    def to_chunks(t):
        return jnp.moveaxis(t.reshape((B, n, CHUNK) + t.shape[2:]), 1, 0)

    def step(carry, xs):
        c_st, n_st, m_st = carry
        qj, kj, vj, lij, lfj = xs
        b_h = jnp.moveaxis(jnp.cumsum(lfj, axis=1), 1, 2)
        li_h = jnp.moveaxis(lij, 1, 2)
        d = b_h[:, :, :, None] - b_h[:, :, None, :] + li_h[:, :, None, :]
        d = jnp.where(causal, d, -jnp.inf)
        inter = b_h + m_st[:, :, None]
        m_t = jnp.maximum(inter, jnp.max(d, axis=-1))
        w_intra = jnp.exp(d - m_t[..., None])
        w_inter = jnp.exp(inter - m_t)
        qk = jnp.einsum('bthd,bshd->bhts', qj, kj) * w_intra
        num = (jnp.einsum('bhts,bshe->bthe', qk, vj)
               + jnp.einsum('bthd,bhde->bthe', qj, c_st) * jnp.moveaxis(w_inter, 1, 2)[..., None])
        den = jnp.sum(qk, axis=-1) + jnp.einsum('bthd,bhd->bht', qj, n_st) * w_inter
        lower = jnp.maximum(jnp.abs(den), jnp.exp(-m_t))
        h = num / jnp.moveaxis(lower, 1, 2)[..., None]
        b_last = b_h[:, :, -1]
        w_k = b_last[:, :, None] - b_h + li_h
        m_new = jnp.maximum(b_last + m_st, jnp.max(w_k, axis=-1))
        e_k = jnp.exp(w_k - m_new[:, :, None])
        decay = jnp.exp(b_last + m_st - m_new)
        c_new = c_st * decay[..., None, None] + jnp.einsum('bshd,bshe->bhde', kj * jnp.moveaxis(e_k, 1, 2)[..., None], vj)
        n_new = n_st * decay[..., None] + jnp.einsum('bhs,bshd->bhd', e_k, kj)
        return (c_new, n_new, m_new), h

    init = (jnp.zeros((B, H, dk, dv), jnp.float32), jnp.zeros((B, H, dk), jnp.float32),
            jnp.zeros((B, H), jnp.float32))
    _, hs = lax.scan(step, init, (to_chunks(q), to_chunks(k), to_chunks(v),
                                  to_chunks(log_i), to_chunks(log_f)))
    return jnp.moveaxis(hs, 0, 1).reshape(B, S, H, dv)


def mlstm_mixer(hn, w_in, w_out, b_i, b_f, norm_g):
    B, S, _ = hn.shape
    f32 = jnp.float32
    proj = (hn @ w_in).astype(f32)
    cuts = np.cumsum([MLSTM_QK_WIDTH, MLSTM_QK_WIDTH, MLSTM_V_WIDTH, MLSTM_V_WIDTH, MLSTM_HEADS]).tolist()
    q, k, v, o, ig, fg = jnp.split(proj, cuts, axis=-1)
    q = q.reshape(B, S, MLSTM_HEADS, MLSTM_QK_DIM)
    k = k.reshape(B, S, MLSTM_HEADS, MLSTM_QK_DIM) * (MLSTM_QK_DIM ** -0.5)
    v = v.reshape(B, S, MLSTM_HEADS, MLSTM_V_DIM)
    log_i = ig + b_i.astype(f32)
    log_f = jax.nn.log_sigmoid(fg + b_f.astype(f32))
    hh = norm_last(mlstm(q, k, v, log_i, log_f)) * norm_g.astype(f32)
    out = jax.nn.sigmoid(o) * hh.reshape(B, S, MLSTM_V_WIDTH)
    return out.astype(hn.dtype) @ w_out


def swiglu(hn, w_gate, w_up, w_down):
    return (jax.nn.silu(hn @ w_gate) * (hn @ w_up)) @ w_down


def setup_inputs(seed: int = 0) -> dict:
    key = jax.random.key(seed)
    ks = jax.random.split(key, 20)
    f32 = jnp.float32
    n_even = (DEPTH + 1) // 2
    n_odd = DEPTH // 2

    def dense(k, shape, fan_in):
        return jax.random.normal(k, shape, f32) * (fan_in ** -0.5)

    def gain(k, shape):
        return 1.0 + 0.02 * jax.random.normal(k, shape, f32)

    return {
        "x": jax.random.normal(ks[0], (BATCH, SEQ, D_MODEL), f32),
        "mix_norm_g": gain(ks[1], (DEPTH, D_MODEL)),
        "ffn_norm_g": gain(ks[2], (DEPTH, D_MODEL)),
        "final_norm_g": gain(ks[3], (D_MODEL,)),
        "ab_w_in": dense(ks[4], (n_even, D_MODEL, AB_IN), D_MODEL),
        "ab_w_out": dense(ks[5], (n_even, AB_MIX, D_MODEL), AB_MIX),
        "ret_norm_g": gain(ks[6], (n_even, RET_HEADS, RET_V_DIM)),
        "sgu_ln_g": gain(ks[7], (n_even, SGU_WIDTH)),
        "sgu_ln_b": 0.02 * jax.random.normal(ks[8], (n_even, SGU_WIDTH), f32),
        "sgu_w": dense(ks[9], (n_even, SGU_GROUPS, SGU_BLOCK, SGU_BLOCK), SGU_BLOCK),
        "sgu_b": gain(ks[10], (n_even, SGU_GROUPS, SGU_BLOCK)),
        "c_w_in": dense(ks[11], (n_odd, D_MODEL, C_IN), D_MODEL),
        "c_w_out": dense(ks[12], (n_odd, MLSTM_V_WIDTH, D_MODEL), MLSTM_V_WIDTH),
        "c_b_i": 0.1 * jax.random.normal(ks[13], (n_odd, MLSTM_HEADS), f32),
        "c_b_f": jnp.linspace(3.0, 6.0, MLSTM_HEADS, dtype=f32)[None, :]
                 + 0.01 * jax.random.normal(ks[14], (n_odd, MLSTM_HEADS), f32),
        "c_norm_g": gain(ks[15], (n_odd, MLSTM_HEADS, MLSTM_V_DIM)),
        "ffn_w_gate": dense(ks[16], (DEPTH, D_MODEL, D_FF), D_MODEL),
        "ffn_w_up": dense(ks[17], (DEPTH, D_MODEL, D_FF), D_MODEL),
        "ffn_w_down": dense(ks[18], (DEPTH, D_FF, D_MODEL), D_FF),
    }


def reference(x, mix_norm_g, ffn_norm_g, final_norm_g, ab_w_in, ab_w_out, ret_norm_g,
              sgu_ln_g, sgu_ln_b, sgu_w, sgu_b, c_w_in, c_w_out, c_b_i, c_b_f, c_norm_g,
              ffn_w_gate, ffn_w_up, ffn_w_down):
    h = x
    for layer in range(DEPTH):
        j = layer // 2
        hn = rmsnorm(h, mix_norm_g[layer])
        if layer % 2 == 0:
            h = h + retention_sgu_mixer(hn, ab_w_in[j], ab_w_out[j], ret_norm_g[j],
                                        sgu_ln_g[j], sgu_ln_b[j], sgu_w[j], sgu_b[j])
        else:
            h = h + mlstm_mixer(hn, c_w_in[j], c_w_out[j], c_b_i[j], c_b_f[j], c_norm_g[j])
        h = h + swiglu(rmsnorm(h, ffn_norm_g[layer]), ffn_w_gate[layer], ffn_w_up[layer],
                       ffn_w_down[layer])
    return rmsnorm(h, final_norm_g)
```

```python
import numpy as np
from contextlib import ExitStack
import concourse.bass as bass
import concourse.mybir as mybir
from concourse.bass_utils import run_bass_kernel_spmd

F32 = mybir.dt.float32
BF16 = mybir.dt.bfloat16
AF = mybir.ActivationFunctionType
ALU = mybir.AluOpType

D = 1024
SEQ = 4096
DEPTH = 4
TB = 128
NBLK = 4
SBT = TB * NBLK
DFF = 2816
NFI = DFF // 128
EPS = 1e-6
AB_IN = 3072
C_IN = 3080
NSLOT = 5


class _Op:
    __slots__ = ("fn", "waits", "signal", "sigval", "is_dma", "token")

    def __init__(self, fn):
        self.fn = fn
        self.waits = []
        self.signal = False
        self.sigval = None
        self.is_dma = False
        self.token = None


class _Rec:
    def __init__(self):
        self.calls = []

    def __getattr__(self, name):
        def f(*a, **k):
            self.calls.append((name, a, k))
            return None
        return f


def _freeze(fn):
    r = _Rec()
    fn(r)
    assert len(r.calls) == 1, r.calls
    name, a, k = r.calls[0]
    return lambda h: getattr(h, name)(*a, **k)


class _Buf:
    __slots__ = ("writer", "readers", "dma_sem", "dma_count")

    def __init__(self):
        self.writer = None
        self.readers = []
        self.dma_sem = None
        self.dma_count = 0


class Sched:
    ENGS = ("pe", "act", "dve", "pool", "sp")

    def __init__(self):
        self.ops = {e: [] for e in self.ENGS}
        self.bufs = {}
        self.known = {e: {} for e in self.ENGS}
        self.dma_sems = []
        self.out_tokens = {}

    def _buf(self, k):
        b = self.bufs.get(k)
        if b is None:
            b = self.bufs[k] = _Buf()
        return b

    def _add_wait(self, eng, op, tok):
        kn = self.known[eng]
        if tok[0] == "e":
            _, peng, pidx = tok
            if kn.get(peng, -1) >= pidx:
                return
            kn[peng] = pidx
            self.ops[peng][pidx].signal = True
            op.waits.append(tok)
        else:
            _, sname, val = tok
            if kn.get(sname, -1) >= val:
                return
            kn[sname] = val
            op.waits.append(tok)

    def op(self, eng, fn, reads=(), writes=(), safe_same=()):
        o = _Op(_freeze(fn))
        idx = len(self.ops[eng])
        tok = ("e", eng, idx)
        o.token = tok
        deps = []
        for k in reads:
            b = self._buf(k)
            if b.writer is not None:
                deps.append((k, b.writer))
        for k in writes:
            b = self._buf(k)
            if b.writer is not None:
                deps.append((k, b.writer))
            for r in b.readers:
                deps.append((k, r))
        for k, d in deps:
            if d[0] == "e" and d[1] == eng and k in safe_same:
                continue
            self._add_wait(eng, o, d)
        self.ops[eng].append(o)
        for k in reads:
            self._buf(k).readers.append(tok)
        for k in writes:
            b = self._buf(k)
            b.writer = tok
            b.readers = []
        return tok

    def dma(self, eng, fn, reads=(), write=None, is_output=False):
        o = _Op(_freeze(fn))
        o.is_dma = True
        b = self._buf(write)
        if b.dma_sem is None:
            b.dma_sem = "dsem%d" % len(self.dma_sems)
            self.dma_sems.append(b.dma_sem)
        deps = []
        for k in reads:
            rb = self._buf(k)
            if rb.writer is not None:
                deps.append(rb.writer)
        if b.writer is not None:
            deps.append(b.writer)
        deps.extend(b.readers)
        for d in deps:
            self._add_wait(eng, o, d)
        b.dma_count += 1
        tok = ("d", b.dma_sem, 16 * b.dma_count)
        o.token = tok
        self.ops[eng].append(o)
        for k in reads:
            self._buf(k).readers.append(tok)
        b.writer = tok
        b.readers = []
        if is_output:
            self.out_tokens[b.dma_sem] = tok
        return tok

    def emit(self, nc, final_eng="sp"):
        EPOCH = 30000
        with ExitStack() as st:
            for e in self.ENGS:
                c = 0
                for o in self.ops[e]:
                    if (not o.is_dma) and o.signal:
                        c += 1
                        o.sigval = c
            nsig = {e: sum(1 for o in self.ops[e] if (o.signal and not o.is_dma)) for e in self.ENGS}
            esem = {e: [st.enter_context(nc.semaphore("s_%s_%d" % (e, i)))
                        for i in range(max(1, (nsig[e] + EPOCH - 1) // EPOCH))] for e in self.ENGS}
            dsem = {n: st.enter_context(nc.semaphore(n)) for n in self.dma_sems}
            block = st.enter_context(nc.Block())

            def run(eng_name, handle):
                for o in self.ops[eng_name]:
                    for w in o.waits:
                        if w[0] == "e":
                            sv = self.ops[w[1]][w[2]].sigval - 1
                            handle.wait_ge(esem[w[1]][sv // EPOCH], sv % EPOCH + 1)
                        else:
                            handle.wait_ge(dsem[w[1]], w[2])
                    ins = o.fn(handle)
                    if o.is_dma:
                        ins.then_inc(dsem[o.token[1]], 16)
                    elif o.signal:
                        ins.then_inc(esem[eng_name][(o.sigval - 1) // EPOCH], 1)
                if eng_name == final_eng:
                    for t in self.out_tokens.values():
                        handle.wait_ge(dsem[t[1]], t[2])

            @block.tensor
            def _(h):
                run("pe", h)

            @block.scalar
            def _(h):
                run("act", h)

            @block.vector
            def _(h):
                run("dve", h)

            @block.gpsimd
            def _(h):
                run("pool", h)

            @block.sync
            def _(h):
                run("sp", h)


def _const_tables(seq):
    half = 64
    inv = (10000.0 ** (-np.arange(half, dtype=np.float32) / np.float32(half))).astype(np.float32)
    pos = np.arange(seq, dtype=np.float32)
    ang = (pos[:, None] * inv[None, :]).astype(np.float32)
    cos = np.cos(ang).astype(np.float32)
    sin = np.sin(ang).astype(np.float32)
    cos4 = np.tile(cos, (1, 4))
    sin4 = np.tile(sin, (1, 4))
    cs = np.concatenate([cos4, sin4], axis=1).astype(np.float32)
    H = 4
    log_g = np.log1p(-np.power(2.0, -5.0 - np.arange(H, dtype=np.float64)))
    idx = np.arange(128, dtype=np.float64)
    s = idx[:, None]
    c = idx[None, :]
    same = (np.floor(s / 64) == np.floor(c / 64))
    lower = (np.floor(s / 64) < np.floor(c / 64))
    scale = 128.0 ** -0.5
    DQ = np.zeros((128, H, 128), np.float64)
    QDT = np.zeros((128, H, 128), np.float64)
    KD = np.zeros((128, H, 128), np.float64)
    g128 = []
    for h in range(H):
        lg = log_g[h]
        Dm = np.where(same, np.exp(lg * np.abs(c - s)), np.where(lower, np.exp(lg * (c - s)), 0.0))
        DQ[:, h, :] = scale * Dm / np.exp(lg * (c + 1.0))
        QDT[:, h, :] = np.exp(lg * (c + 1.0))
        KD[:, h, :] = scale * np.exp(lg * (127.0 - s))
        g128.append(float(np.exp(lg * 128.0)))
    CM = (s <= c).astype(np.float32)
    return dict(cs=cs, DQ=DQ.reshape(128, 512).astype(np.float32),
                QDT=QDT.reshape(128, 512).astype(np.float32),
                KD=KD.reshape(128, 512).astype(np.float32), CM=CM), g128


def build(nsb=SEQ // SBT, depth=DEPTH, parts=('mix', 'ffn')):
    seq = nsb * SBT
    _, G128 = _const_tables(128)
    nc = bass.Bass("TRN2", target_bir_lowering=False)

    def din(name, shape):
        return nc.dram_tensor(name, list(shape), F32, kind="ExternalInput").ap()

    x = din("x", [seq, D])
    mix_g = din("mix_norm_g", [4, D])
    ffn_g = din("ffn_norm_g", [4, D])
    fin_g = din("final_norm_g", [1, D])
    ab_w_in = din("ab_w_in", [2, D, AB_IN])
    ab_w_out = din("ab_w_out", [2, D, D])
    ret_g = din("ret_norm_g", [2, 512])
    sgu_g = din("sgu_ln_g", [2, 512])
    sgu_bb = din("sgu_ln_b", [2, 512])
    sgu_wT = din("sgu_wT", [2, 4, 128, 128])
    sgu_bT = din("sgu_bT", [2, 128, 4])
    c_w_in = din("c_w_in", [2, D, C_IN])
    c_w_out = din("c_w_out", [2, D, D])
    c_bi = din("c_b_i", [2, 4, 1])
    c_bf = din("c_b_f", [2, 4, 1])
    c_ng = din("c_norm_g", [2, 1024])
    w_gate = din("ffn_w_gate", [4, D, DFF])
    w_up = din("ffn_w_up", [4, D, DFF])
    w_down = din("ffn_w_down", [4, DFF, D])
    cs_d = din("cs", [seq, 512])
    DQ_d = din("DQ", [128, 512])
    QDT_d = din("QDT", [128, 512])
    KD_d = din("KD", [128, 512])
    CM_d = din("CM", [128, 128])
    y = nc.dram_tensor("y", [seq, D], F32, kind="ExternalOutput").ap()

    S = Sched()
    with ExitStack() as st:
        def T(name, shape, dt=F32):
            return st.enter_context(nc.sbuf_tensor("sb_" + name, list(shape), dt))

        h = T("h", [128, NBLK, D])
        hnT = T("hnT", [128, 8, SBT], BF16)
        hn = T("hn", [128, D], BF16)
        junk = T("junk", [128, D], BF16)
        slots = [T("slot%d" % i, [128, 4096], BF16) for i in range(NSLOT)]
        stg = T("stg", [128, 14400], BF16)
        actT = stg[:, 0:NFI * SBT].rearrange("p (a b) -> p a b", a=NFI)
        mixed = T("mixed", [128, D], BF16)
        mT = T("mT", [128, 8, 128], BF16)
        Gm = T("Gm", [128, D]); Gf = T("Gf", [128, D]); Gfin = T("Gfin", [128, D])
        NDs = [T("NDs%d" % i, [128, 4, 258]) for i in range(2)]
        low4 = T("low4", [128, 4]); rl4 = T("rl4", [128, 4])
        CS = T("CS", [128, NBLK, 512])
        DQ = T("DQ", [128, 512]); QDT = T("QDT", [128, 512]); KD = T("KD", [128, 512])
        CM = T("CM", [128, 128])
        idf = T("idf", [128, 128]); idb = T("idb", [128, 128], BF16)
        ones4 = T("ones4", [4, 128]); zrow = T("zrow", [4, 128])
        RG = T("RG", [128, 512]); SG = T("SG", [128, 512]); SBB = T("SBB", [128, 512])
        WT = T("WT", [128, 4, 128], BF16); sbias = T("sbias", [128, 4])
        CNG = T("CNG", [128, 1024])
        bi = T("bi", [4, 1]); bfm = T("bfm", [4, 1])
        Sret = [T("Sret%d" % i, [128, 512]) for i in range(2)]
        Sretb = [T("Sretb%d" % i, [128, 512], BF16) for i in range(2)]
        Cst = [T("Cst%d" % i, [128, 4, 258]) for i in range(2)]
        Cb = T("Cb", [128, 4, 258], BF16)
        mst = [T("mst%d" % i, [4, 1]) for i in range(2)]
        ss = T("ss", [128, 1]); sd = T("sd", [128, 1]); rstd = T("rstd", [128, 1])
        tA = T("tA", [128, 256]); tB = T("tB", [128, 256])
        rot = T("rot", [128, 512], BF16)
        gv = T("gv", [128, 512])
        st6 = T("st6", [128, 4, 6]); mv = T("mv", [128, 4, 2])
        sd4 = T("sd4", [128, 4]); rs4 = T("rs4", [128, 4]); nm4 = T("nm4", [128, 4])
        rn = T("rn", [128, 512])
        Pb = T("Pb", [128, 512], BF16)
        sgt = T("sgt", [128, 512])
        li = T("li", [4, SBT]); lfn = T("lfn", [4, SBT]); ex = lfn
        nb = T("nb", [4, 128]); aa = T("aa", [4, 128]); gg = T("gg", [4, 128])
        R3 = T("R3", [4, 3, 128])
        ngl = T("ngl", [4, NBLK]); dec = T("dec", [4, NBLK]); dg = T("dg", [4, NBLK, 4])
        tmpr = T("tmpr", [4, 128])
        cols = T("cols", [128, NBLK, 16])
        low = T("low", [128, 1]); rl = T("rl", [128, 1])
        hs = T("hs", [128, 256]); hy = T("hy", [128, 1024])

        banks = [st.enter_context(nc.psum_tensor("bank%d" % i, [128, 512], F32)) for i in range(8)]
        bank_ctr = [0]

        def newbank():
            i = bank_ctr[0] % 8
            bank_ctr[0] += 1
            return banks[i], "bank%d" % i

        def stg_view(off, shape):
            n = int(np.prod(shape))
            ap = stg[:, off:off + n]
            if len(shape) == 2:
                return ap.rearrange("p (a b) -> p a b", a=shape[0])
            if len(shape) == 3:
                return ap.rearrange("p (a b c) -> p a b c", a=shape[0], b=shape[1])
            return ap

        qdT = stg_view(0, (NBLK, 4, 128)); kT = stg_view(2048, (NBLK, 4, 128))
        kd = stg_view(4096, (NBLK, 512)); vb = stg_view(6144, (NBLK, 512))
        sgl = stg_view(8192, (NBLK, 512)); ug = stg_view(10240, (NBLK, 512))
        vsn = stg_view(12288, (NBLK, 512))
        qrT = qdT; kdT = kT; kdc = kd
        vext = stg_view(6144, (NBLK, 4, 258))
        so = stg_view(10272, (NBLK, 1024))
        assert 10272 + 4096 <= 14400

        slot_ctr = [0]

        def load_tile(dmas):
            i = slot_ctr[0] % NSLOT
            slot_ctr[0] += 1
            sl = slots[i]
            key = "slot%d" % i
            for (lo, a, b, src) in dmas:
                dst = sl[:, lo:lo + a * b].rearrange("p (a b) -> p a b", a=a)
                S.dma("pool", (lambda dst, src: lambda e: e.dma_start(out=dst, in_=src))(dst, src), write=key)
            return sl, key

        def wcols(w2d, c0, ncol):
            return w2d.rearrange("(kc p) n -> p kc n", p=128)[:, :, c0:c0 + ncol]

        sp_dma = lambda dst, src, key: S.dma("sp", (lambda e: e.dma_start(out=dst, in_=src)), write=key)
        sp_dma(DQ[:], DQ_d[:], "DQ"); sp_dma(QDT[:], QDT_d[:], "QDT"); sp_dma(KD[:], KD_d[:], "KD")
        sp_dma(CM[:], CM_d[:], "CM")
        S.op("pool", lambda e: e.memset(idf[:], 0.0), writes=["idf"])
        S.op("pool", lambda e: e.affine_select(out=idf[:], in_=idf[:], compare_op=ALU.not_equal, fill=1.0,
                                               base=0, pattern=[[-1, 128]], channel_multiplier=1),
             reads=["idf"], writes=["idf"])
        S.op("pool", lambda e: e.tensor_copy(idb[:], idf[:]), reads=["idf"], writes=["idb"])
        S.op("pool", lambda e: e.memset(ones4[:], 1.0), writes=["ones4"])
        S.op("pool", lambda e: e.memset(zrow[:], 0.0), writes=["zrow"])
        for i in range(2):
            S.op("pool", (lambda i: lambda e: e.memset(Sret[i][:], 0.0))(i), writes=["Sret%d" % i])
            S.op("pool", (lambda i: lambda e: e.memset(Sretb[i][:], 0.0))(i), writes=["Sretb%d" % i])
            S.op("pool", (lambda i: lambda e: e.memset(Cst[i][:], 0.0))(i), writes=["Cst%d" % i])
            S.op("pool", (lambda i: lambda e: e.memset(mst[i][:], 0.0))(i), writes=["mst%d" % i])

        def B(i):
            return banks[i], "bank%d" % i

        def norm_block(j, Gt, gkey, bank=None):
            hk = "h%d" % j
            S.op("act", lambda e: e.activation(out=junk[:], in_=h[:, j, :], func=AF.Square, accum_out=ss[:]),
                 reads=[hk], writes=["junk", "ss"])
            S.op("act", lambda e: e.activation(out=sd[:], in_=ss[:], func=AF.Sqrt, bias=EPS, scale=1.0 / D),
                 reads=["ss"], writes=["sd"])
            S.op("dve", lambda e: e.reciprocal(rstd[:], sd[:]), reads=["sd"], writes=["rstd"])
            S.op("dve", lambda e: e.scalar_tensor_tensor(out=hn[:], in0=h[:, j, :], scalar=rstd[:, 0:1],
                                                         in1=Gt[:], op0=ALU.mult, op1=ALU.mult),
                 reads=[hk, "rstd", gkey], writes=["hn"])
            bk, bkey = bank if bank is not None else newbank()
            bb = bk[:].bitcast(BF16)
            for kc in range(8):
                S.op("pe", lambda e: e.transpose(bb[:, kc * 128:(kc + 1) * 128], hn[:, kc * 128:(kc + 1) * 128], idb[:]),
                     reads=["hn", "idb"], writes=[bkey], safe_same=[bkey])
            S.op("act", lambda e: e.activation(out=hnT[:, :, j * 128:(j + 1) * 128],
                                               in_=bb[:, 0:1024].rearrange("p (a b) -> p a b", a=8), func=AF.Copy),
                 reads=[bkey], writes=["hnT%d" % j])

        def final_block(j, r0):
            hk = "h%d" % j
            hyk = ["hy_%d" % q for q in range(4)]
            S.op("act", lambda e: e.activation(out=junk[:], in_=h[:, j, :], func=AF.Square, accum_out=ss[:]),
                 reads=[hk], writes=["junk", "ss"])
            S.op("act", lambda e: e.activation(out=sd[:], in_=ss[:], func=AF.Sqrt, bias=EPS, scale=1.0 / D),
                 reads=["ss"], writes=["sd"])
            S.op("dve", lambda e: e.reciprocal(rstd[:], sd[:]), reads=["sd"], writes=["rstd"])
            S.op("dve", lambda e: e.scalar_tensor_tensor(out=hy[:], in0=h[:, j, :], scalar=rstd[:, 0:1],
                                                         in1=Gfin[:], op0=ALU.mult, op1=ALU.mult),
                 reads=[hk, "rstd", "Gfin"], writes=hyk)
            S.dma("sp", lambda e: e.dma_start(out=y[r0 + j * 128:r0 + (j + 1) * 128, :], in_=hy[:]),
                  reads=hyk, write="y", is_output=True)

        def proj_block(j, sl, skey, ncol=512, coff=0, stride=None):
            stride = stride or ncol
            bk, bkey = newbank()
            wv = sl[:, 0:8 * stride].rearrange("p (a b) -> p a b", a=8)
            for kc in range(8):
                S.op("pe", (lambda kc: lambda e: e.matmul(bk[:, 0:ncol], hnT[:, kc, j * 128:(j + 1) * 128],
                                                          wv[:, kc, coff:coff + ncol],
                                                          start=(kc == 0), stop=(kc == 7)))(kc),
                     reads=["hnT%d" % j, skey], writes=[bkey], safe_same=[bkey])
            return bk, bkey

        def rope_block(j, bk, bkey, outT):
            pv = bk[:, 0:512].rearrange("p (h t i) -> p h t i", h=4, t=2)
            t1 = pv[:, :, 0, :]
            t2 = pv[:, :, 1, :]
            cos = CS[:, j, 0:256].rearrange("p (h i) -> p h i", h=4)
            sin = CS[:, j, 256:512].rearrange("p (h i) -> p h i", h=4)
            rv = rot[:, :].rearrange("p (h t i) -> p h t i", h=4, t=2)
            A = tA[:, :].rearrange("p (h i) -> p h i", h=4)
            B = tB[:, :].rearrange("p (h i) -> p h i", h=4)
            ck = "CS"
            S.op("dve", lambda e: e.tensor_tensor(out=A, in0=t1, in1=cos, op=ALU.mult), reads=[bkey, ck], writes=["tA"])
            S.op("dve", lambda e: e.tensor_tensor(out=B, in0=t2, in1=sin, op=ALU.mult), reads=[bkey, ck], writes=["tB"])
            S.op("dve", lambda e: e.tensor_tensor(out=rv[:, :, 0, :], in0=A, in1=B, op=ALU.subtract),
                 reads=["tA", "tB"], writes=["rot"])
            S.op("dve", lambda e: e.tensor_tensor(out=A, in0=t1, in1=sin, op=ALU.mult), reads=[bkey, ck], writes=["tA"])
            S.op("dve", lambda e: e.tensor_tensor(out=B, in0=t2, in1=cos, op=ALU.mult), reads=[bkey, ck], writes=["tB"])
            S.op("dve", lambda e: e.tensor_tensor(out=rv[:, :, 1, :], in0=A, in1=B, op=ALU.add),
                 reads=["tA", "tB"], writes=["rot"])

        def transpose4(src_ap_fn, src_key):
            bk, bkey = newbank()
            bb = bk[:].bitcast(BF16)
            for hh in range(4):
                S.op("pe", (lambda hh: lambda e: e.transpose(bb[:, hh * 128:(hh + 1) * 128], src_ap_fn(hh), idb[:]))(hh),
                     reads=[src_key, "idb"], writes=[bkey], safe_same=[bkey])
            return bb, bkey

        def out_proj_block(j, tiles, tb, pbs):
            bk, bkey = tb
            bb = bk[:].bitcast(BF16)
            for kc in range(8):
                S.op("pe", lambda e: e.transpose(bb[:, kc * 128:(kc + 1) * 128], mixed[:, kc * 128:(kc + 1) * 128], idb[:]),
                     reads=["mixed", "idb"], writes=[bkey], safe_same=[bkey])
            S.op("act", lambda e: e.activation(out=mT[:, :, :], in_=bb[:, 0:1024].rearrange("p (a b) -> p a b", a=8),
                                               func=AF.Copy), reads=[bkey], writes=["mT"])
            for half in range(2):
                sl, skey = tiles[half]
                wv = sl[:, 0:4096].rearrange("p (a b) -> p a b", a=8)
                pk, pkey = pbs[half]
                for kc in range(8):
                    S.op("pe", lambda e: e.matmul(pk[:, :], mT[:, kc, :], wv[:, kc, :], start=(kc == 0), stop=(kc == 7)),
                         reads=["mT", skey], writes=[pkey], safe_same=[pkey])
                S.op("dve", lambda e: e.tensor_tensor(out=h[:, j, half * 512:(half + 1) * 512], in0=pk[:, :],
                                                      in1=h[:, j, half * 512:(half + 1) * 512], op=ALU.add),
                     reads=[pkey, "h%d" % j], writes=["h%d" % j])

        def ln_stats(src_aps, skeys, n):
            for g_ in range(n):
                S.op("dve", (lambda g_: lambda e: e.bn_stats(st6[:, g_, :], src_aps[g_]))(g_),
                     reads=[skeys[g_]], writes=["st6_%d" % g_])
                S.op("dve", (lambda g_: lambda e: e.bn_aggr(mv[:, g_, :], st6[:, g_, :]))(g_),
                     reads=["st6_%d" % g_], writes=["mv_%d" % g_])
            mvk = ["mv_%d" % g_ for g_ in range(n)]
            S.op("act", lambda e: e.activation(out=sd4[:, 0:n], in_=mv[:, 0:n, 1], func=AF.Sqrt, bias=EPS, scale=1.0),
                 reads=mvk, writes=["sd4"])
            S.op("dve", lambda e: e.reciprocal(rs4[:, 0:n], sd4[:, 0:n]), reads=["sd4"], writes=["rs4"])
            S.op("dve", lambda e: e.scalar_tensor_tensor(out=nm4[:, 0:n], in0=mv[:, 0:n, 0], scalar=-1.0,
                                                         in1=rs4[:, 0:n], op0=ALU.mult, op1=ALU.mult),
                 reads=mvk + ["rs4"], writes=["nm4"])

        def even_mixer(l):
            jl = l // 2
            w_in = ab_w_in[jl]
            sp_dma(Gm[:], mix_g[l:l + 1, :].partition_broadcast(128), "Gm")
            sp_dma(RG[:], ret_g[jl:jl + 1, :].partition_broadcast(128), "RG")
            sp_dma(SG[:], sgu_g[jl:jl + 1, :].partition_broadcast(128), "SG")
            sp_dma(SBB[:], sgu_bb[jl:jl + 1, :].partition_broadcast(128), "SBB")
            sp_dma(sbias[:], sgu_bT[jl], "sbias")
            S.dma("pool", lambda e: e.dma_start(out=WT[:], in_=sgu_wT[jl].rearrange("g q p -> q g p")), write="WT")
            S.op("pool", lambda e: e.memset(WT[64:128, :, 0:64], 0.0), reads=["WT"], writes=["WT"])
            sp_dma(Gf[:], ffn_g[l:l + 1, :].partition_broadcast(128), "Gf")
            for ct in range(6):
                sl, skey = load_tile([(0, 8, 512, wcols(w_in, ct * 512, 512))])
                for j in range(NBLK):
                    bk, bkey = proj_block(j, sl, skey)
                    if ct == 0:
                        rope_block(j, bk, bkey, None)
                        bb, tkey = transpose4(lambda hh: rot[:, hh * 128:(hh + 1) * 128], "rot")
                        S.op("dve", (lambda j, bb: lambda e: e.tensor_tensor(
                            out=qdT[:, j, :, :], in0=bb[:, 0:512].rearrange("p (a b) -> p a b", a=4),
                            in1=QDT[:, :].rearrange("p (a b) -> p a b", a=4), op=ALU.mult))(j, bb),
                             reads=[tkey, "QDT"], writes=["qdT%d" % j])
                    elif ct == 1:
                        rope_block(j, bk, bkey, None)
                        S.op("dve", (lambda j: lambda e: e.tensor_tensor(out=kd[:, j, :], in0=rot[:, :], in1=KD[:, :],
                                                                         op=ALU.mult))(j),
                             reads=["rot", "KD"], writes=["kd%d" % j])
                        bb, tkey = transpose4(lambda hh: rot[:, hh * 128:(hh + 1) * 128], "rot")
                        S.op("act", (lambda j, bb: lambda e: e.activation(
                            out=kT[:, j, :, :], in_=bb[:, 0:512].rearrange("p (a b) -> p a b", a=4), func=AF.Copy))(j, bb),
                             reads=[tkey], writes=["kT%d" % j])
                    elif ct == 2:
                        S.op("act", (lambda j, bk: lambda e: e.activation(out=vb[:, j, :], in_=bk[:, :], func=AF.Copy))(j, bk),
                             reads=[bkey], writes=["vb%d" % j])
                    elif ct == 3:
                        S.op("act", (lambda j, bk: lambda e: e.activation(out=sgl[:, j, :], in_=bk[:, :], func=AF.Silu))(j, bk),
                             reads=[bkey], writes=["sgl%d" % j])
                    elif ct == 4:
                        S.op("act", (lambda j, bk: lambda e: e.activation(out=ug[:, j, :], in_=bk[:, :],
                                                                          func=AF.Gelu_apprx_tanh))(j, bk),
                             reads=[bkey], writes=["ug%d" % j])
                    else:
                        S.op("act", (lambda bk: lambda e: e.activation(out=gv[:, :], in_=bk[:, :],
                                                                       func=AF.Gelu_apprx_tanh))(bk),
                             reads=[bkey], writes=["gv"])
                        ln_stats([gv[:, :]], ["gv"], 1)
                        S.op("act", lambda e: e.activation(out=gv[:, :], in_=gv[:, :], func=AF.Identity,
                                                           scale=rs4[:, 0:1], bias=nm4[:, 0:1]),
                             reads=["gv", "rs4", "nm4"], writes=["gv"])
                        S.op("dve", lambda e: e.tensor_tensor(out=gv[:, :], in0=gv[:, :], in1=SG[:, :], op=ALU.mult),
                             reads=["gv", "SG"], writes=["gv"])
                        S.op("dve", (lambda j: lambda e: e.tensor_tensor(out=vsn[:, j, :], in0=gv[:, :], in1=SBB[:, :],
                                                                         op=ALU.add))(j),
                             reads=["gv", "SBB"], writes=["vsn%d" % j])
            otiles = [load_tile([(0, 8, 512, wcols(ab_w_out[jl], half * 512, 512))]) for half in range(2)]
            Sr, Srb = Sret[jl], Sretb[jl]
            sk, sbk = "Sret%d" % jl, "Sretb%d" % jl

            def S1(j):
                pk, pkey = B(0)
                for hh in range(4):
                    S.op("pe", lambda e: e.matmul(pk[:, hh * 128:(hh + 1) * 128], kT[:, j, hh, :], qdT[:, j, hh, :],
                                                  start=True, stop=True),
                         reads=["kT%d" % j, "qdT%d" % j], writes=[pkey], safe_same=[pkey])
                S.op("dve", lambda e: e.tensor_tensor(out=Pb[:, :], in0=pk[:, :], in1=DQ[:, :], op=ALU.mult),
                     reads=[pkey, "DQ"], writes=["Pb"])
                ok_, okey = B(1 + (j % 2))
                for hh in range(4):
                    S.op("pe", lambda e: e.matmul(ok_[:, hh * 128:(hh + 1) * 128], Pb[:, hh * 128:(hh + 1) * 128],
                                                  vb[:, j, hh * 128:(hh + 1) * 128], start=True, stop=False),
                         reads=["Pb", "vb%d" % j], writes=[okey], safe_same=[okey])
                    S.op("pe", lambda e: e.matmul(ok_[:, hh * 128:(hh + 1) * 128], qdT[:, j, hh, :],
                                                  Srb[:, hh * 128:(hh + 1) * 128], start=False, stop=True),
                         reads=["qdT%d" % j, sbk], writes=[okey], safe_same=[okey])
                kk, kkey = B(3)
                for hh in range(4):
                    S.op("pe", lambda e: e.matmul(kk[:, hh * 128:(hh + 1) * 128], kd[:, j, hh * 128:(hh + 1) * 128],
                                                  vb[:, j, hh * 128:(hh + 1) * 128], start=True, stop=True),
                         reads=["kd%d" % j, "vb%d" % j], writes=[kkey], safe_same=[kkey])
                for hh in range(4):
                    S.op("dve", lambda e: e.scalar_tensor_tensor(
                        out=Sr[:, hh * 128:(hh + 1) * 128], in0=Sr[:, hh * 128:(hh + 1) * 128], scalar=G128[hh],
                        in1=kk[:, hh * 128:(hh + 1) * 128], op0=ALU.mult, op1=ALU.add),
                         reads=[kkey, sk + "_%d" % hh], writes=[sk + "_%d" % hh])
                S.op("act", lambda e: e.activation(out=Srb[:, :], in_=Sr[:, :], func=AF.Copy),
                     reads=[sk + "_%d" % hh for hh in range(4)], writes=[sbk])

            def S2(j):
                ok_, okey = B(1 + (j % 2))
                rnk = ["rn_%d" % hh for hh in range(4)]
                ln_stats([ok_[:, hh * 128:(hh + 1) * 128] for hh in range(4)], [okey] * 4, 4)
                for hh in range(4):
                    S.op("act", lambda e: e.activation(
                        out=rn[:, hh * 128:(hh + 1) * 128], in_=ok_[:, hh * 128:(hh + 1) * 128], func=AF.Identity,
                        scale=rs4[:, hh:hh + 1], bias=nm4[:, hh:hh + 1]),
                         reads=[okey, "rs4", "nm4"], writes=["rn_%d" % hh])
                S.op("dve", lambda e: e.tensor_tensor(out=rn[:, :], in0=rn[:, :], in1=RG[:, :], op=ALU.mult),
                     reads=rnk + ["RG"], writes=rnk)
                S.op("dve", lambda e: e.tensor_tensor(out=mixed[:, 0:512], in0=rn[:, :], in1=sgl[:, j, :], op=ALU.mult),
                     reads=rnk + ["sgl%d" % j], writes=["mixed"])
                gk, gkey = B(4)
                for g_ in range(4):
                    S.op("pe", lambda e: e.matmul(gk[:, g_ * 128:(g_ + 1) * 128], WT[:, g_, :],
                                                  vsn[:, j, g_ * 128:(g_ + 1) * 128], start=True, stop=True),
                         reads=["WT", "vsn%d" % j], writes=[gkey], safe_same=[gkey])
                for g_ in range(4):
                    S.op("dve", lambda e: e.scalar_tensor_tensor(
                        out=mixed[:, 512 + g_ * 128:512 + (g_ + 1) * 128], in0=gk[:, g_ * 128:(g_ + 1) * 128],
                        scalar=sbias[:, g_:g_ + 1], in1=ug[:, j, g_ * 128:(g_ + 1) * 128],
                        op0=ALU.add, op1=ALU.mult),
                         reads=[gkey, "sbias", "ug%d" % j], writes=["mixed"])
                out_proj_block(j, otiles, B(5), [B(6), B(7)])
                norm_block(j, Gf, "Gf", B(5))

            S1(0)
            for j in range(NBLK):
                if j + 1 < NBLK:
                    S1(j + 1)
                S2(j)

        def odd_mixer(l):
            jl = l // 2
            w_in = c_w_in[jl]
            C = Cst[jl]
            ck = "Cst%d" % jl
            msk = "mst%d" % jl
            ms = mst[jl]
            sp_dma(Gm[:], mix_g[l:l + 1, :].partition_broadcast(128), "Gm")
            sp_dma(CNG[:], c_ng[jl:jl + 1, :].partition_broadcast(128), "CNG")
            sp_dma(bi[:], c_bi[jl], "bi")
            sp_dma(bfm[:], c_bf[jl], "bfm")
            S.op("dve", lambda e: e.tensor_scalar(bfm[:], bfm[:], -1.0, None, ALU.mult), reads=["bfm"], writes=["bfm"])
            sp_dma(Gf[:], ffn_g[l:l + 1, :].partition_broadcast(128), "Gf")
            sl, skey = load_tile([(0, 8, 8, wcols(w_in, 3072, 8))])
            wv = sl[:, 0:64].rearrange("p (a b) -> p a b", a=8)
            pi, pikey = newbank()
            pf, pfkey = newbank()
            for kc in range(8):
                S.op("pe", (lambda kc: lambda e: e.matmul(pi[0:4, :], wv[:, kc, 0:4], hnT[:, kc, :],
                                                          start=(kc == 0), stop=(kc == 7)))(kc),
                     reads=["hnT%d" % j for j in range(NBLK)] + [skey], writes=[pikey], safe_same=[pikey])
            for kc in range(8):
                S.op("pe", (lambda kc: lambda e: e.matmul(pf[0:4, :], wv[:, kc, 4:8], hnT[:, kc, :],
                                                          start=(kc == 0), stop=(kc == 7)))(kc),
                     reads=["hnT%d" % j for j in range(NBLK)] + [skey], writes=[pfkey], safe_same=[pfkey])
            S.op("act", lambda e: e.activation(out=li[:, :], in_=pi[0:4, :], func=AF.Identity, bias=bi[:, 0:1], scale=1.0),
                 reads=[pikey, "bi"], writes=["li"])
            S.op("act", lambda e: e.activation(out=ex[:, :], in_=pf[0:4, :], func=AF.Exp, bias=bfm[:, 0:1], scale=-1.0),
                 reads=[pfkey, "bfm"], writes=["lfn"])
            S.op("act", lambda e: e.activation(out=lfn[:, :], in_=ex[:, :], func=AF.Ln, bias=1.0, scale=1.0),
                 reads=["lfn"], writes=["lfn"])
            for j in range(NBLK):
                cs_ = slice(j * 128, (j + 1) * 128)
                S.op("dve", (lambda cs_: lambda e: e.tensor_tensor_scan(nb[:, :], lfn[:, cs_], zrow[:, :], 0.0,
                                                                        ALU.add, ALU.add))(cs_),
                     reads=["lfn", "zrow"], writes=["nb"])
                S.op("dve", (lambda cs_: lambda e: e.tensor_tensor(out=aa[:, :], in0=li[:, cs_], in1=nb[:, :],
                                                                   op=ALU.add))(cs_),
                     reads=["li", "nb"], writes=["aa"])
                S.op("dve", lambda e: e.tensor_tensor_scan(gg[:, :], aa[:, :], aa[:, :], ms[:, 0:1], ALU.max, ALU.max),
                     reads=["aa", msk], writes=["gg"])
                S.op("dve", (lambda j: lambda e: e.tensor_scalar(ngl[:, j:j + 1], gg[:, 127:128], -1.0, None, ALU.mult))(j),
                     reads=["gg"], writes=["ngl%d" % j])
                S.op("act", (lambda j: lambda e: e.activation(out=dec[:, j:j + 1], in_=ms[:, 0:1], func=AF.Exp,
                                                              bias=ngl[:, j:j + 1], scale=1.0))(j),
                     reads=[msk, "ngl%d" % j], writes=["dec%d" % j])
                S.op("act", (lambda j: lambda e: e.activation(out=R3[:, 0, :], in_=aa[:, :], func=AF.Exp,
                                                              bias=ngl[:, j:j + 1], scale=1.0))(j),
                     reads=["aa", "ngl%d" % j], writes=["R3_0"])
                S.op("act", lambda e: e.activation(out=R3[:, 1, :], in_=gg[:, :], func=AF.Exp,
                                                   bias=gg[:, 127:128], scale=-1.0),
                     reads=["gg"], writes=["R3_1"])
                S.op("dve", lambda e: e.tensor_tensor(out=tmpr[:, :], in0=nb[:, :], in1=gg[:, :], op=ALU.subtract),
                     reads=["nb", "gg"], writes=["tmpr"])
                S.op("act", lambda e: e.activation(out=R3[:, 2, :], in_=tmpr[:, :], func=AF.Exp),
                     reads=["tmpr"], writes=["R3_2"])
                S.op("dve", lambda e: e.tensor_tensor(out=ms[:, 0:1], in0=gg[:, 127:128], in1=nb[:, 127:128],
                                                      op=ALU.subtract),
                     reads=["gg", "nb"], writes=[msk])
                S.op("dve", (lambda j: lambda e: e.tensor_scalar(dg[:, j, :], idf[0:4, 0:4], dec[:, j:j + 1], None, ALU.mult))(j),
                     reads=["idf", "dec%d" % j], writes=["dg%d" % j])
                ck_, ckey = newbank()
                for q_ in range(3):
                    S.op("pe", (lambda q_, ck_: lambda e: e.matmul(ck_[:, q_ * 4:(q_ + 1) * 4], R3[:, q_, :],
                                                                  idf[0:4, 0:4], start=True, stop=True))(q_, ck_),
                         reads=["R3_%d" % q_, "idf"], writes=[ckey], safe_same=[ckey])
                S.op("pe", (lambda j, ck_: lambda e: e.matmul(ck_[:, 12:16], ones4[:, :], dg[:, j, :],
                                                              start=True, stop=True))(j, ck_),
                     reads=["ones4", "dg%d" % j], writes=[ckey], safe_same=[ckey])
                S.op("dve", (lambda j, ck_: lambda e: e.tensor_copy(cols[:, j, :], ck_[:, 0:16]))(j, ck_),
                     reads=[ckey], writes=["cols%d" % j])
                S.op("dve", (lambda j: lambda e: e.tensor_scalar(cols[:, j, 0:4], cols[:, j, 0:4], 128.0 ** -0.5, None,
                                                                 ALU.mult))(j),
                     reads=["cols%d" % j], writes=["cols%d" % j])
            sl, skey = load_tile([(0, 8, 512, wcols(w_in, 0, 512))])
            for j in range(NBLK):
                bk, bkey = proj_block(j, sl, skey)
                for hh in range(4):
                    S.op("act", (lambda hh, bk, j: lambda e: e.activation(
                        out=rot[:, hh * 128:(hh + 1) * 128], in_=bk[:, hh * 128:(hh + 1) * 128], func=AF.Copy,
                        scale=cols[:, j, 4 + hh:5 + hh]))(hh, bk, j),
                         reads=[bkey, "cols%d" % j], writes=["rot"])
                bb, tkey = transpose4(lambda hh: rot[:, hh * 128:(hh + 1) * 128], "rot")
                S.op("act", (lambda j, bb: lambda e: e.activation(
                    out=qrT[:, j, :, :], in_=bb[:, 0:512].rearrange("p (a b) -> p a b", a=4), func=AF.Copy))(j, bb),
                     reads=[tkey], writes=["qdT%d" % j])
            sl, skey = load_tile([(0, 8, 512, wcols(w_in, 512, 512))])
            for j in range(NBLK):
                bk, bkey = proj_block(j, sl, skey)
                for hh in range(4):
                    S.op("act", (lambda hh, bk, j: lambda e: e.activation(
                        out=kdc[:, j, hh * 128:(hh + 1) * 128], in_=bk[:, hh * 128:(hh + 1) * 128], func=AF.Copy,
                        scale=cols[:, j, hh:hh + 1]))(hh, bk, j),
                         reads=[bkey, "cols%d" % j], writes=["kd%d" % j])
                bb, tkey = transpose4((lambda j: lambda hh: kdc[:, j, hh * 128:(hh + 1) * 128])(j), "kd%d" % j)
                S.op("act", (lambda j, bb: lambda e: e.activation(
                    out=kdT[:, j, :, :], in_=bb[:, 0:512].rearrange("p (a b) -> p a b", a=4), func=AF.Copy))(j, bb),
                     reads=[tkey], writes=["kT%d" % j])
            for vt in range(2):
                sl, skey = load_tile([(0, 8, 512, wcols(w_in, 1024 + vt * 512, 512))])
                for j in range(NBLK):
                    bk, bkey = proj_block(j, sl, skey)
                    S.op("act", (lambda j, bk, vt: lambda e: e.activation(
                        out=vext[:, j, 2 * vt:2 * vt + 2, 0:256], in_=bk[:, :].rearrange("p (a b) -> p a b", a=2),
                        func=AF.Copy))(j, bk, vt),
                         reads=[bkey], writes=["vb%d" % j])
            for j in range(NBLK):
                S.op("dve", (lambda j: lambda e: e.memset(vext[:, j, :, 256:257], 1.0))(j), reads=[], writes=["vb%d" % j])
            for ot in range(2):
                sl, skey = load_tile([(0, 8, 512, wcols(w_in, 2048 + ot * 512, 512))])
                for j in range(NBLK):
                    bk, bkey = proj_block(j, sl, skey)
                    S.op("act", (lambda j, bk, ot: lambda e: e.activation(
                        out=so[:, j, ot * 512:(ot + 1) * 512], in_=bk[:, :], func=AF.Sigmoid))(j, bk, ot),
                         reads=[bkey], writes=["so%d" % j])
            otiles = [load_tile([(0, 8, 512, wcols(c_w_out[jl], half * 512, 512))]) for half in range(2)]
            def S1(j):
                nd = NDs[j % 2]
                for hh in range(4):
                    chk = ck + "_%d" % hh
                    S.op("dve", lambda e: e.tensor_scalar(Cb[:, hh, 0:257], C[:, hh, 0:257],
                                                          cols[:, j, 12 + hh:13 + hh], None, ALU.mult),
                         reads=[chk, "cols%d" % j], writes=["Cb_%d" % hh])
                    pk, pkey = B(hh % 2)
                    S.op("pe", lambda e: e.matmul(pk[:, 0:128], kdT[:, j, hh, :], qrT[:, j, hh, :], start=True, stop=True),
                         reads=["kT%d" % j, "qdT%d" % j], writes=[pkey])
                    S.op("dve", lambda e: e.tensor_tensor(out=Pb[:, hh * 128:(hh + 1) * 128], in0=pk[:, 0:128],
                                                          in1=CM[:, :], op=ALU.mult),
                         reads=[pkey, "CM"], writes=["Pb_%d" % hh])
                    nk, nkey = B(2 + hh % 2)
                    S.op("pe", lambda e: e.matmul(nk[:, 0:257], Pb[:, hh * 128:(hh + 1) * 128],
                                                  vext[:, j, hh, 0:257], start=True, stop=False),
                         reads=["Pb_%d" % hh, "vb%d" % j], writes=[nkey])
                    S.op("pe", lambda e: e.matmul(nk[:, 0:257], qrT[:, j, hh, :], Cb[:, hh, 0:257], start=False, stop=True),
                         reads=["qdT%d" % j, "Cb_%d" % hh], writes=[nkey], safe_same=[nkey])
                    kk, kkey = B(4 + hh % 2)
                    S.op("pe", lambda e: e.matmul(kk[:, 0:257], kdc[:, j, hh * 128:(hh + 1) * 128],
                                                  vext[:, j, hh, 0:257], start=True, stop=True),
                         reads=["kd%d" % j, "vb%d" % j], writes=[kkey])
                    S.op("dve", lambda e: e.scalar_tensor_tensor(
                        out=C[:, hh, 0:257], in0=C[:, hh, 0:257], scalar=cols[:, j, 12 + hh:13 + hh], in1=kk[:, 0:257],
                        op0=ALU.mult, op1=ALU.add),
                         reads=[kkey, chk, "cols%d" % j], writes=[chk])
                    S.op("act", lambda e: e.activation(out=nd[:, hh, 0:257], in_=nk[:, 0:257], func=AF.Copy),
                         reads=[nkey], writes=["NDs%d_%d" % (j % 2, hh)])

            def S2(j):
                nd = NDs[j % 2]
                ndk = ["NDs%d_%d" % (j % 2, hh) for hh in range(4)]
                hyk = ["hy_%d" % hh for hh in range(4)]
                S.op("act", lambda e: e.activation(out=low4[:, :], in_=nd[:, :, 256], func=AF.Abs),
                     reads=ndk, writes=["low4"])
                S.op("dve", lambda e: e.tensor_tensor(out=low4[:, :], in0=low4[:, :], in1=cols[:, j, 8:12], op=ALU.max),
                     reads=["low4", "cols%d" % j], writes=["low4"])
                S.op("dve", lambda e: e.reciprocal(rl4[:, :], low4[:, :]), reads=["low4"], writes=["rl4"])
                for hh in range(4):
                    S.op("act", lambda e: e.activation(out=nd[:, hh, 0:256], in_=nd[:, hh, 0:256], func=AF.Copy,
                                                       scale=rl4[:, hh:hh + 1]),
                         reads=[ndk[hh], "rl4"], writes=[ndk[hh]])
                ln_stats([nd[:, hh, 0:256] for hh in range(4)], ndk, 4)
                for hh in range(4):
                    S.op("act", lambda e: e.activation(out=hy[:, hh * 256:(hh + 1) * 256], in_=nd[:, hh, 0:256],
                                                       func=AF.Identity, scale=rs4[:, hh:hh + 1], bias=nm4[:, hh:hh + 1]),
                         reads=[ndk[hh], "rs4", "nm4"], writes=["hy_%d" % hh])
                S.op("dve", lambda e: e.tensor_tensor(out=hy[:, :], in0=hy[:, :], in1=CNG[:, :], op=ALU.mult),
                     reads=hyk + ["CNG"], writes=hyk)
                S.op("dve", lambda e: e.tensor_tensor(out=mixed[:, :], in0=hy[:, :], in1=so[:, j, :], op=ALU.mult),
                     reads=hyk + ["so%d" % j], writes=["mixed"])
                out_proj_block(j, otiles, B(6), [B(7), B(6)])
                norm_block(j, Gf, "Gf", B(7))

            S1(0)
            for j in range(NBLK):
                if j + 1 < NBLK:
                    S1(j + 1)
                S2(j)

        def ffn(l, last, r0):
            if not last:
                sp_dma(Gm[:], mix_g[l + 1:l + 2, :].partition_broadcast(128), "Gm")
            hk = ["hnT%d" % j for j in range(NBLK)]
            for ft in range(NFI // 2):
                sl, skey = load_tile([(0, 8, 256, wcols(w_gate[l], ft * 256, 256)),
                                      (2048, 8, 256, wcols(w_up[l], ft * 256, 256))])
                gvw = sl[:, 0:2048].rearrange("p (a b) -> p a b", a=8)
                uvw = sl[:, 2048:4096].rearrange("p (a b) -> p a b", a=8)
                for sub in range(2):
                    fi = ft * 2 + sub
                    pg, pgk = newbank()
                    pu, puk = newbank()
                    for kc in range(8):
                        S.op("pe", (lambda kc, pg, sub, gvw: lambda e: e.matmul(pg[:, :], gvw[:, kc, sub * 128:(sub + 1) * 128],
                                                                           hnT[:, kc, :], start=(kc == 0), stop=(kc == 7)))(kc, pg, sub, gvw),
                             reads=hk + [skey], writes=[pgk], safe_same=[pgk])
                    for kc in range(8):
                        S.op("pe", (lambda kc, pu, sub, uvw: lambda e: e.matmul(pu[:, :], uvw[:, kc, sub * 128:(sub + 1) * 128],
                                                                           hnT[:, kc, :], start=(kc == 0), stop=(kc == 7)))(kc, pu, sub, uvw),
                             reads=hk + [skey], writes=[puk], safe_same=[puk])
                    S.op("act", (lambda pg: lambda e: e.activation(out=sgt[:, :], in_=pg[:, :], func=AF.Silu))(pg),
                         reads=[pgk], writes=["sgt"])
                    S.op("dve", (lambda pu, fi: lambda e: e.tensor_tensor(out=actT[:, fi, :], in0=sgt[:, :], in1=pu[:, :],
                                                                          op=ALU.mult))(pu, fi),
                         reads=["sgt", puk], writes=["actT%d" % fi])
            accs = [[newbank() for half in range(2)] for j in range(NBLK)]
            wdv = w_down[l].rearrange("(fi p) n -> p fi n", p=128)
            f0 = 0
            while f0 < NFI:
                nf = min(4, NFI - f0)
                sl, skey = load_tile([(0, nf, 1024, wdv[:, f0:f0 + nf, :])])
                wv = sl[:, 0:nf * 1024].rearrange("p (a b) -> p a b", a=nf)
                for fl in range(nf):
                    fi = f0 + fl
                    for j in range(NBLK):
                        for half in range(2):
                            ak, akey = accs[j][half]
                            S.op("pe", (lambda fi, fl, j, half, ak, wv: lambda e: e.matmul(
                                ak[:, :], actT[:, fi, j * 128:(j + 1) * 128], wv[:, fl, half * 512:(half + 1) * 512],
                                start=(fi == 0), stop=(fi == NFI - 1)))(fi, fl, j, half, ak, wv),
                                 reads=["actT%d" % fi, skey], writes=[akey], safe_same=[akey])
                f0 += nf
            for j in range(NBLK):
                for half in range(2):
                    ak, akey = accs[j][half]
                    S.op("dve", lambda e: e.tensor_tensor(
                        out=h[:, j, half * 512:(half + 1) * 512], in0=ak[:, :], in1=h[:, j, half * 512:(half + 1) * 512],
                        op=ALU.add), reads=[akey, "h%d" % j], writes=["h%d" % j])
                if last:
                    final_block(j, r0)
                else:
                    norm_block(j, Gm, "Gm", accs[j][0])

        sp_dma(Gfin[:], fin_g[0:1, :].partition_broadcast(128), "Gfin")
        for sb in range(nsb):
            r0 = sb * SBT
            for j in range(NBLK):
                S.dma("sp", lambda e: e.dma_start(out=h[:, j, :], in_=x[r0 + j * 128:r0 + (j + 1) * 128, :]),
                      write="h%d" % j)
            S.dma("sp", lambda e: e.dma_start(out=CS[:, :, :], in_=cs_d[r0:r0 + SBT, :].rearrange("(j p) n -> p j n", p=128)),
                  write="CS")
            first_mix = ('mix' in parts)
            sp_dma(Gm[:], (mix_g if first_mix else ffn_g)[0:1, :].partition_broadcast(128), "Gm")
            for j in range(NBLK):
                norm_block(j, Gm, "Gm")
            for l in range(depth):
                if 'mix' in parts:
                    if l % 2 == 0:
                        even_mixer(l)
                    else:
                        odd_mixer(l)
                if 'ffn' in parts:
                    ffn(l, l == depth - 1, r0)
        S.emit(nc)
    return nc


_NC_CACHE = {}


def _prep_inputs(inputs, b, nsb):
    seq = nsb * SBT
    tabs, _ = _const_tables(seq)
    f = lambda a: np.ascontiguousarray(np.asarray(a, dtype=np.float32))
    m = {
        "x": f(inputs["x"][b, :seq]),
        "mix_norm_g": f(inputs["mix_norm_g"]),
        "ffn_norm_g": f(inputs["ffn_norm_g"]),
        "final_norm_g": f(inputs["final_norm_g"]).reshape(1, D),
        "ab_w_in": f(inputs["ab_w_in"]),
        "ab_w_out": f(inputs["ab_w_out"]),
        "ret_norm_g": f(inputs["ret_norm_g"]).reshape(2, 512),
        "sgu_ln_g": f(inputs["sgu_ln_g"]),
        "sgu_ln_b": f(inputs["sgu_ln_b"]),
        "sgu_wT": f(np.transpose(np.asarray(inputs["sgu_w"]), (0, 1, 3, 2))),
        "sgu_bT": f(np.transpose(np.asarray(inputs["sgu_b"]), (0, 2, 1))),
        "c_w_in": f(inputs["c_w_in"]),
        "c_w_out": f(inputs["c_w_out"]),
        "c_b_i": f(inputs["c_b_i"]).reshape(2, 4, 1),
        "c_b_f": f(inputs["c_b_f"]).reshape(2, 4, 1),
        "c_norm_g": f(inputs["c_norm_g"]).reshape(2, 1024),
        "ffn_w_gate": f(inputs["ffn_w_gate"]),
        "ffn_w_up": f(inputs["ffn_w_up"]),
        "ffn_w_down": f(inputs["ffn_w_down"]),
    }
    m.update(tabs)
    return m


def run(inputs, nsb=SEQ // SBT, depth=DEPTH, ncores=8, trace=False, parts=('mix', 'ffn')):
    key = (nsb, depth, parts)
    if key not in _NC_CACHE:
        _NC_CACHE[key] = build(nsb, depth, parts)
    nc = _NC_CACHE[key]
    in_maps = [_prep_inputs(inputs, b, nsb) for b in range(ncores)]
    res = run_bass_kernel_spmd(nc, in_maps, core_ids=list(range(ncores)), **({"trace": True} if trace else {}))
    out = np.stack([np.asarray(r["y"], dtype=np.float32) for r in res.results], axis=0)
    return out, res


def kernel(**inputs):
    out, _ = run(inputs)
    return out
```

```python
import numpy as np
from contextlib import ExitStack
import concourse.bass as bass
import concourse.mybir as mybir
from concourse.bass_utils import run_bass_kernel_spmd

F32 = mybir.dt.float32
BF16 = mybir.dt.bfloat16
AF = mybir.ActivationFunctionType
ALU = mybir.AluOpType

D = 1024
SEQ = 4096
DEPTH = 4
TB = 128
NBLK = 4
SBT = TB * NBLK
DFF = 2816
NFI = DFF // 128
EPS = 1e-6
AB_IN = 3072
C_IN = 3080
NSLOT = 5


class _Op:
    __slots__ = ("fn", "eng", "seq", "deps", "odeps", "is_dma", "token", "dur", "xfer",
                 "signal", "sigval", "pos", "waits", "finish", "nsucc", "succ", "npend", "ready")

    def __init__(self, fn, eng, seq):
        self.fn = fn
        self.eng = eng
        self.seq = seq
        self.deps = []
        self.odeps = []
        self.is_dma = False
        self.token = None
        self.dur = 0.1
        self.xfer = 0.0
        self.signal = False
        self.sigval = None
        self.pos = None
        self.waits = []
        self.finish = 0.0
        self.succ = []
        self.npend = 0
        self.ready = 0.0


class _Rec:
    def __init__(self):
        self.calls = []

    def __getattr__(self, name):
        def f(*a, **k):
            self.calls.append((name, a, k))
            return None
        return f


def _freeze(fn, eng):
    r = _Rec()
    fn(r)
    assert len(r.calls) == 1, r.calls
    name, a, k = r.calls[0]
    out = k.get("out", a[0] if a else None)
    try:
        shp = tuple(out.shape)
        n = 1
        for d_ in shp[1:]:
            n *= int(d_)
        npart = int(shp[0])
    except Exception:
        n, npart = 64, 128
    if eng == "pe":
        dur = 0.12 if name == "transpose" else 0.06 + n / 2400.0
    elif eng == "act":
        dur = 0.22 + n * 1.0e-3
    elif eng == "dve":
        dur = 0.10 + n * 1.2e-3
    elif eng == "pool":
        dur = 0.2 + n * 2.0e-3
    else:
        dur = 0.1
    xfer = 0.0
    if name == "dma_start":
        dur = 1.0 if eng == "pool" else 0.15
        xfer = 2.0 + (n * npart * 4) / 300e3
    return (lambda h: getattr(h, name)(*a, **k)), dur, xfer


class _Buf:
    __slots__ = ("writer", "readers", "dma_sem", "dma_count")

    def __init__(self):
        self.writer = None
        self.readers = []
        self.dma_sem = None
        self.dma_count = 0


class Sched:
    ENGS = ("pe", "act", "dve", "pool", "sp")
    RESCHEDULE = True

    def __init__(self):
        self.all = []
        self.bufs = {}
        self.dma_sems = []
        self.out_ops = {}

    def _buf(self, k):
        b = self.bufs.get(k)
        if b is None:
            b = self.bufs[k] = _Buf()
        return b

    def op(self, eng, fn, reads=(), writes=(), safe_same=()):
        f, dur, _ = _freeze(fn, eng)
        o = _Op(f, eng, len(self.all))
        o.dur = dur
        seen = set()
        for k in reads:
            b = self._buf(k)
            if b.writer is not None:
                self._dep(o, b.writer, k in safe_same, seen)
        for k in writes:
            b = self._buf(k)
            if b.writer is not None:
                self._dep(o, b.writer, k in safe_same, seen)
            for r in b.readers:
                self._dep(o, r, k in safe_same, seen)
        self.all.append(o)
        for k in reads:
            self._buf(k).readers.append(o)
        for k in writes:
            b = self._buf(k)
            b.writer = o
            b.readers = []
        return o

    def _dep(self, o, d, safe, seen):
        if d is o:
            return
        key = (id(d), safe and d.eng == o.eng and not d.is_dma)
        if key in seen:
            return
        seen.add(key)
        if safe and d.eng == o.eng and not d.is_dma:
            o.odeps.append(d)
        else:
            o.deps.append(d)

    def dma(self, eng, fn, reads=(), write=None, is_output=False):
        f, dur, xfer = _freeze(fn, eng)
        o = _Op(f, eng, len(self.all))
        o.is_dma = True
        o.dur = dur
        o.xfer = xfer
        b = self._buf(write)
        if b.dma_sem is None:
            b.dma_sem = "dsem%d" % len(self.dma_sems)
            self.dma_sems.append(b.dma_sem)
        seen = set()
        for k in reads:
            rb = self._buf(k)
            if rb.writer is not None:
                self._dep(o, rb.writer, False, seen)
        if b.writer is not None:
            self._dep(o, b.writer, False, seen)
        for r in b.readers:
            self._dep(o, r, False, seen)
        b.dma_count += 1
        o.token = (b.dma_sem, 16 * b.dma_count)
        self.all.append(o)
        for k in reads:
            self._buf(k).readers.append(o)
        b.writer = o
        b.readers = []
        if is_output:
            self.out_ops[b.dma_sem] = o
        return o

    def _schedule(self):
        import heapq
        order = {e: [] for e in self.ENGS}
        if not self.RESCHEDULE:
            for o in self.all:
                order[o.eng].append(o)
            return order
        LAT = 0.25
        for o in self.all:
            o.succ = []
            o.npend = 0
        for o in self.all:
            for d in o.deps:
                d.succ.append((o, True))
                o.npend += 1
            for d in o.odeps:
                d.succ.append((o, False))
                o.npend += 1
        waiting = {e: [] for e in self.ENGS}
        runnable = {e: [] for e in self.ENGS}
        tfree = {e: 0.0 for e in self.ENGS}
        dma_free = [0.0]
        for o in self.all:
            if o.npend == 0:
                heapq.heappush(waiting[o.eng], (0.0, o.seq, o))
        left = len(self.all)
        while left:
            best = None
            for e in self.ENGS:
                w, r = waiting[e], runnable[e]
                while w and w[0][0] <= tfree[e]:
                    _, sq, o = heapq.heappop(w)
                    heapq.heappush(r, (sq, o))
                if r:
                    cand = (tfree[e], r[0][0], e, True)
                elif w:
                    cand = (w[0][0], w[0][1], e, False)
                else:
                    continue
                if best is None or cand < best:
                    best = cand
            start, _, e, from_r = best
            if from_r:
                _, o = heapq.heappop(runnable[e])
            else:
                _, _, o = heapq.heappop(waiting[e])
            tfree[e] = start + o.dur
            if o.is_dma:
                t0 = max(start + o.dur, dma_free[0])
                dma_free[0] = t0 + (o.xfer - 2.0)
                o.finish = t0 + o.xfer
            else:
                o.finish = start + o.dur
            order[e].append(o)
            left -= 1
            for (s_, hard) in o.succ:
                rdy = o.finish + (LAT if hard else 0.0)
                if rdy > s_.ready:
                    s_.ready = rdy
                s_.npend -= 1
                if s_.npend == 0:
                    heapq.heappush(waiting[s_.eng], (s_.ready, s_.seq, s_))
        return order

    def emit(self, nc, final_eng="sp"):
        EPOCH = 30000
        order = self._schedule()
        for e in self.ENGS:
            for i, o in enumerate(order[e]):
                o.pos = i
        for e in self.ENGS:
            known = {}
            for o in order[e]:
                o.waits = []
                for d in sorted(o.deps, key=lambda d_: -(d_.token[1] if d_.is_dma else d_.pos)):
                    if d.is_dma:
                        sname, val = d.token
                        if known.get(sname, -1) >= val:
                            continue
                        known[sname] = val
                        o.waits.append(d)
                    else:
                        if known.get(d.eng, -1) >= d.pos:
                            continue
                        known[d.eng] = d.pos
                        d.signal = True
                        o.waits.append(d)
                for d in o.odeps:
                    assert d.eng == e and d.pos < o.pos
        with ExitStack() as st:
            nsig = {}
            for e in self.ENGS:
                c = 0
                for o in order[e]:
                    if (not o.is_dma) and o.signal:
                        c += 1
                        o.sigval = c
                nsig[e] = c
            esem = {e: [st.enter_context(nc.semaphore("s_%s_%d" % (e, i)))
                        for i in range(max(1, (nsig[e] + EPOCH - 1) // EPOCH))] for e in self.ENGS}
            dsem = {n: st.enter_context(nc.semaphore(n)) for n in self.dma_sems}
            block = st.enter_context(nc.Block())

            def run(eng_name, handle):
                for o in order[eng_name]:
                    for d in o.waits:
                        if d.is_dma:
                            handle.wait_ge(dsem[d.token[0]], d.token[1])
                        else:
                            sv = d.sigval - 1
                            handle.wait_ge(esem[d.eng][sv // EPOCH], sv % EPOCH + 1)
                    ins = o.fn(handle)
                    if o.is_dma:
                        ins.then_inc(dsem[o.token[0]], 16)
                    elif o.signal:
                        ins.then_inc(esem[eng_name][(o.sigval - 1) // EPOCH], 1)
                if eng_name == final_eng:
                    for d in self.out_ops.values():
                        handle.wait_ge(dsem[d.token[0]], d.token[1])

            @block.tensor
            def _(h):
                run("pe", h)

            @block.scalar
            def _(h):
                run("act", h)

            @block.vector
            def _(h):
                run("dve", h)

            @block.gpsimd
            def _(h):
                run("pool", h)

            @block.sync
            def _(h):
                run("sp", h)


def _const_tables(seq):
    half = 64
    inv = (10000.0 ** (-np.arange(half, dtype=np.float32) / np.float32(half))).astype(np.float32)
    pos = np.arange(seq, dtype=np.float32)
    ang = (pos[:, None] * inv[None, :]).astype(np.float32)
    cos = np.cos(ang).astype(np.float32)
    sin = np.sin(ang).astype(np.float32)
    cos4 = np.tile(cos, (1, 4))
    sin4 = np.tile(sin, (1, 4))
    cs = np.concatenate([cos4, sin4], axis=1).astype(np.float32)
    H = 4
    log_g = np.log1p(-np.power(2.0, -5.0 - np.arange(H, dtype=np.float64)))
    idx = np.arange(128, dtype=np.float64)
    s = idx[:, None]
    c = idx[None, :]
    same = (np.floor(s / 64) == np.floor(c / 64))
    lower = (np.floor(s / 64) < np.floor(c / 64))
    scale = 128.0 ** -0.5
    DQ = np.zeros((128, H, 128), np.float64)
    QDT = np.zeros((128, H, 128), np.float64)
    KD = np.zeros((128, H, 128), np.float64)
    g128 = []
    for h in range(H):
        lg = log_g[h]
        Dm = np.where(same, np.exp(lg * np.abs(c - s)), np.where(lower, np.exp(lg * (c - s)), 0.0))
        DQ[:, h, :] = scale * Dm / np.exp(lg * (c + 1.0))
        QDT[:, h, :] = np.exp(lg * (c + 1.0))
        KD[:, h, :] = scale * np.exp(lg * (127.0 - s))
        g128.append(float(np.exp(lg * 128.0)))
    CM = (s <= c).astype(np.float32)
    return dict(cs=cs, DQ=DQ.reshape(128, 512).astype(np.float32),
                QDT=QDT.reshape(128, 512).astype(np.float32),
                KD=KD.reshape(128, 512).astype(np.float32), CM=CM), g128


def build(nsb=SEQ // SBT, depth=DEPTH, parts=('mix', 'ffn')):
    seq = nsb * SBT
    _, G128 = _const_tables(128)
    nc = bass.Bass("TRN2", target_bir_lowering=False)

    def din(name, shape):
        return nc.dram_tensor(name, list(shape), F32, kind="ExternalInput").ap()

    x = din("x", [seq, D])
    mix_g = din("mix_norm_g", [4, D])
    ffn_g = din("ffn_norm_g", [4, D])
    fin_g = din("final_norm_g", [1, D])
    ab_w_in = din("ab_w_in", [2, D, AB_IN])
    ab_w_out = din("ab_w_out", [2, D, D])
    ret_g = din("ret_norm_g", [2, 512])
    sgu_g = din("sgu_ln_g", [2, 512])
    sgu_bb = din("sgu_ln_b", [2, 512])
    sgu_wT = din("sgu_wT", [2, 4, 128, 128])
    sgu_bT = din("sgu_bT", [2, 128, 4])
    c_w_in = din("c_w_in", [2, D, C_IN])
    c_w_out = din("c_w_out", [2, D, D])
    c_bi = din("c_b_i", [2, 4, 1])
    c_bf = din("c_b_f", [2, 4, 1])
    c_ng = din("c_norm_g", [2, 1024])
    w_gate = din("ffn_w_gate", [4, D, DFF])
    w_up = din("ffn_w_up", [4, D, DFF])
    w_down = din("ffn_w_down", [4, DFF, D])
    cs_d = din("cs", [seq, 512])
    DQ_d = din("DQ", [128, 512])
    QDT_d = din("QDT", [128, 512])
    KD_d = din("KD", [128, 512])
    CM_d = din("CM", [128, 128])
    y = nc.dram_tensor("y", [seq, D], F32, kind="ExternalOutput").ap()

    S = Sched()
    with ExitStack() as st:
        def T(name, shape, dt=F32):
            return st.enter_context(nc.sbuf_tensor("sb_" + name, list(shape), dt))

        h = T("h", [128, NBLK, D])
        hnT = T("hnT", [128, 8, SBT], BF16)
        hn = T("hn", [128, D], BF16)
        junk = T("junk", [128, D], BF16)
        slots = [T("slot%d" % i, [128, 4096], BF16) for i in range(NSLOT)]
        stg = T("stg", [128, 14400], BF16)
        actT = stg[:, 0:NFI * SBT].rearrange("p (a b) -> p a b", a=NFI)
        mixed = T("mixed", [128, D], BF16)
        mT = T("mT", [128, 8, 128], BF16)
        Gm = T("Gm", [128, D]); Gf = T("Gf", [128, D]); Gfin = T("Gfin", [128, D])
        NDs = [T("NDs%d" % i, [128, 4, 258]) for i in range(2)]
        low4 = T("low4", [128, 4]); rl4 = T("rl4", [128, 4])
        CS = T("CS", [128, NBLK, 512])
        DQ = T("DQ", [128, 512]); QDT = T("QDT", [128, 512]); KD = T("KD", [128, 512])
        CM = T("CM", [128, 128])
        idf = T("idf", [128, 128]); idb = T("idb", [128, 128], BF16)
        ones4 = T("ones4", [4, 128]); zrow = T("zrow", [4, 128])
        RG = T("RG", [128, 512]); SG = T("SG", [128, 512]); SBB = T("SBB", [128, 512])
        WT = T("WT", [128, 4, 128], BF16); sbias = T("sbias", [128, 4])
        CNG = T("CNG", [128, 1024])
        bi = T("bi", [4, 1]); bfm = T("bfm", [4, 1])
        Sret = [T("Sret%d" % i, [128, 512]) for i in range(2)]
        Sretb = [T("Sretb%d" % i, [128, 512], BF16) for i in range(2)]
        Cst = [T("Cst%d" % i, [128, 4, 258]) for i in range(2)]
        Cb = T("Cb", [128, 4, 258], BF16)
        mst = [T("mst%d" % i, [4, 1]) for i in range(2)]
        ss = T("ss", [128, 1]); sd = T("sd", [128, 1]); rstd = T("rstd", [128, 1])
        tA = T("tA", [128, 256]); tB = T("tB", [128, 256])
        rot = T("rot", [128, 512], BF16)
        gv = T("gv", [128, 512])
        st6 = T("st6", [128, 4, 6]); mv = T("mv", [128, 4, 2])
        sd4 = T("sd4", [128, 4]); rs4 = T("rs4", [128, 4]); nm4 = T("nm4", [128, 4])
        rn = T("rn", [128, 512])
        Pb = T("Pb", [128, 512], BF16)
        sgt = T("sgt", [128, 512])
        li = T("li", [4, SBT]); lfn = T("lfn", [4, SBT]); ex = lfn
        nb = T("nb", [4, 128]); aa = T("aa", [4, 128]); gg = T("gg", [4, 128])
        R3 = T("R3", [4, 3, 128])
        ngl = T("ngl", [4, NBLK]); dec = T("dec", [4, NBLK]); dg = T("dg", [4, NBLK, 4])
        tmpr = T("tmpr", [4, 128])
        cols = T("cols", [128, NBLK, 16])
        low = T("low", [128, 1]); rl = T("rl", [128, 1])
        hs = T("hs", [128, 256]); hy = T("hy", [128, 1024])

        banks = [st.enter_context(nc.psum_tensor("bank%d" % i, [128, 512], F32)) for i in range(8)]
        bank_ctr = [0]

        def newbank():
            i = bank_ctr[0] % 8
            bank_ctr[0] += 1
            return banks[i], "bank%d" % i

        def stg_view(off, shape):
            n = int(np.prod(shape))
            ap = stg[:, off:off + n]
            if len(shape) == 2:
                return ap.rearrange("p (a b) -> p a b", a=shape[0])
            if len(shape) == 3:
                return ap.rearrange("p (a b c) -> p a b c", a=shape[0], b=shape[1])
            return ap

        qdT = stg_view(0, (NBLK, 4, 128)); kT = stg_view(2048, (NBLK, 4, 128))
        kd = stg_view(4096, (NBLK, 512)); vb = stg_view(6144, (NBLK, 512))
        sgl = stg_view(8192, (NBLK, 512)); ug = stg_view(10240, (NBLK, 512))
        vsn = stg_view(12288, (NBLK, 512))
        qrT = qdT; kdT = kT; kdc = kd
        vext = stg_view(6144, (NBLK, 4, 258))
        so = stg_view(10272, (NBLK, 1024))
        assert 10272 + 4096 <= 14400

        slot_ctr = [0]

        def load_tile(dmas):
            i = slot_ctr[0] % NSLOT
            slot_ctr[0] += 1
            sl = slots[i]
            key = "slot%d" % i
            for (lo, a, b, src) in dmas:
                dst = sl[:, lo:lo + a * b].rearrange("p (a b) -> p a b", a=a)
                S.dma("pool", (lambda dst, src: lambda e: e.dma_start(out=dst, in_=src))(dst, src), write=key)
            return sl, key

        def wcols(w2d, c0, ncol):
            return w2d.rearrange("(kc p) n -> p kc n", p=128)[:, :, c0:c0 + ncol]

        sp_dma = lambda dst, src, key: S.dma("sp", (lambda e: e.dma_start(out=dst, in_=src)), write=key)
        sp_dma(DQ[:], DQ_d[:], "DQ"); sp_dma(QDT[:], QDT_d[:], "QDT"); sp_dma(KD[:], KD_d[:], "KD")
        sp_dma(CM[:], CM_d[:], "CM")
        S.op("pool", lambda e: e.memset(idf[:], 0.0), writes=["idf"])
        S.op("pool", lambda e: e.affine_select(out=idf[:], in_=idf[:], compare_op=ALU.not_equal, fill=1.0,
                                               base=0, pattern=[[-1, 128]], channel_multiplier=1),
             reads=["idf"], writes=["idf"])
        S.op("pool", lambda e: e.tensor_copy(idb[:], idf[:]), reads=["idf"], writes=["idb"])
        S.op("pool", lambda e: e.memset(ones4[:], 1.0), writes=["ones4"])
        S.op("pool", lambda e: e.memset(zrow[:], 0.0), writes=["zrow"])
        for i in range(2):
            S.op("pool", (lambda i: lambda e: e.memset(Sret[i][:], 0.0))(i), writes=["Sret%d" % i])
            S.op("pool", (lambda i: lambda e: e.memset(Sretb[i][:], 0.0))(i), writes=["Sretb%d" % i])
            S.op("pool", (lambda i: lambda e: e.memset(Cst[i][:], 0.0))(i), writes=["Cst%d" % i])
            S.op("pool", (lambda i: lambda e: e.memset(mst[i][:], 0.0))(i), writes=["mst%d" % i])

        def B(i):
            return banks[i], "bank%d" % i

        def norm_block(j, Gt, gkey, bank=None):
            hk = "h%d" % j
            S.op("act", lambda e: e.activation(out=junk[:], in_=h[:, j, :], func=AF.Square, accum_out=ss[:]),
                 reads=[hk], writes=["junk", "ss"])
            S.op("act", lambda e: e.activation(out=sd[:], in_=ss[:], func=AF.Sqrt, bias=EPS, scale=1.0 / D),
                 reads=["ss"], writes=["sd"])
            S.op("dve", lambda e: e.reciprocal(rstd[:], sd[:]), reads=["sd"], writes=["rstd"])
            S.op("dve", lambda e: e.scalar_tensor_tensor(out=hn[:], in0=h[:, j, :], scalar=rstd[:, 0:1],
                                                         in1=Gt[:], op0=ALU.mult, op1=ALU.mult),
                 reads=[hk, "rstd", gkey], writes=["hn"])
            bk, bkey = bank if bank is not None else newbank()
            bb = bk[:].bitcast(BF16)
            for kc in range(8):
                S.op("pe", lambda e: e.transpose(bb[:, kc * 128:(kc + 1) * 128], hn[:, kc * 128:(kc + 1) * 128], idb[:]),
                     reads=["hn", "idb"], writes=[bkey], safe_same=[bkey])
            S.op("act", lambda e: e.activation(out=hnT[:, :, j * 128:(j + 1) * 128],
                                               in_=bb[:, 0:1024].rearrange("p (a b) -> p a b", a=8), func=AF.Copy),
                 reads=[bkey], writes=["hnT%d" % j])

        def final_block(j, r0):
            hk = "h%d" % j
            hyk = ["hy_%d" % q for q in range(4)]
            S.op("act", lambda e: e.activation(out=junk[:], in_=h[:, j, :], func=AF.Square, accum_out=ss[:]),
                 reads=[hk], writes=["junk", "ss"])
            S.op("act", lambda e: e.activation(out=sd[:], in_=ss[:], func=AF.Sqrt, bias=EPS, scale=1.0 / D),
                 reads=["ss"], writes=["sd"])
            S.op("dve", lambda e: e.reciprocal(rstd[:], sd[:]), reads=["sd"], writes=["rstd"])
            S.op("dve", lambda e: e.scalar_tensor_tensor(out=hy[:], in0=h[:, j, :], scalar=rstd[:, 0:1],
                                                         in1=Gfin[:], op0=ALU.mult, op1=ALU.mult),
                 reads=[hk, "rstd", "Gfin"], writes=hyk)
            S.dma("sp", lambda e: e.dma_start(out=y[r0 + j * 128:r0 + (j + 1) * 128, :], in_=hy[:]),
                  reads=hyk, write="y", is_output=True)

        def proj_block(j, sl, skey, ncol=512, coff=0, stride=None):
            stride = stride or ncol
            bk, bkey = newbank()
            wv = sl[:, 0:8 * stride].rearrange("p (a b) -> p a b", a=8)
            for kc in range(8):
                S.op("pe", (lambda kc: lambda e: e.matmul(bk[:, 0:ncol], hnT[:, kc, j * 128:(j + 1) * 128],
                                                          wv[:, kc, coff:coff + ncol],
                                                          start=(kc == 0), stop=(kc == 7)))(kc),
                     reads=["hnT%d" % j, skey], writes=[bkey], safe_same=[bkey])
            return bk, bkey

        def rope_block(j, bk, bkey, outT):
            pv = bk[:, 0:512].rearrange("p (h t i) -> p h t i", h=4, t=2)
            t1 = pv[:, :, 0, :]
            t2 = pv[:, :, 1, :]
            cos = CS[:, j, 0:256].rearrange("p (h i) -> p h i", h=4)
            sin = CS[:, j, 256:512].rearrange("p (h i) -> p h i", h=4)
            rv = rot[:, :].rearrange("p (h t i) -> p h t i", h=4, t=2)
            A = tA[:, :].rearrange("p (h i) -> p h i", h=4)
            B = tB[:, :].rearrange("p (h i) -> p h i", h=4)
            ck = "CS"
            S.op("dve", lambda e: e.tensor_tensor(out=A, in0=t1, in1=cos, op=ALU.mult), reads=[bkey, ck], writes=["tA"])
            S.op("dve", lambda e: e.tensor_tensor(out=B, in0=t2, in1=sin, op=ALU.mult), reads=[bkey, ck], writes=["tB"])
            S.op("dve", lambda e: e.tensor_tensor(out=rv[:, :, 0, :], in0=A, in1=B, op=ALU.subtract),
                 reads=["tA", "tB"], writes=["rot"])
            S.op("dve", lambda e: e.tensor_tensor(out=A, in0=t1, in1=sin, op=ALU.mult), reads=[bkey, ck], writes=["tA"])
            S.op("dve", lambda e: e.tensor_tensor(out=B, in0=t2, in1=cos, op=ALU.mult), reads=[bkey, ck], writes=["tB"])
            S.op("dve", lambda e: e.tensor_tensor(out=rv[:, :, 1, :], in0=A, in1=B, op=ALU.add),
                 reads=["tA", "tB"], writes=["rot"])

        def transpose4(src_ap_fn, src_key):
            bk, bkey = newbank()
            bb = bk[:].bitcast(BF16)
            for hh in range(4):
                S.op("pe", (lambda hh: lambda e: e.transpose(bb[:, hh * 128:(hh + 1) * 128], src_ap_fn(hh), idb[:]))(hh),
                     reads=[src_key, "idb"], writes=[bkey], safe_same=[bkey])
            return bb, bkey

        def out_proj_block(j, tiles, tb, pbs):
            bk, bkey = tb
            bb = bk[:].bitcast(BF16)
            for kc in range(8):
                S.op("pe", lambda e: e.transpose(bb[:, kc * 128:(kc + 1) * 128], mixed[:, kc * 128:(kc + 1) * 128], idb[:]),
                     reads=["mixed", "idb"], writes=[bkey], safe_same=[bkey])
            S.op("act", lambda e: e.activation(out=mT[:, :, :], in_=bb[:, 0:1024].rearrange("p (a b) -> p a b", a=8),
                                               func=AF.Copy), reads=[bkey], writes=["mT"])
            for half in range(2):
                sl, skey = tiles[half]
                wv = sl[:, 0:4096].rearrange("p (a b) -> p a b", a=8)
                pk, pkey = pbs[half]
                for kc in range(8):
                    S.op("pe", lambda e: e.matmul(pk[:, :], mT[:, kc, :], wv[:, kc, :], start=(kc == 0), stop=(kc == 7)),
                         reads=["mT", skey], writes=[pkey], safe_same=[pkey])
                S.op("dve", lambda e: e.tensor_tensor(out=h[:, j, half * 512:(half + 1) * 512], in0=pk[:, :],
                                                      in1=h[:, j, half * 512:(half + 1) * 512], op=ALU.add),
                     reads=[pkey, "h%d" % j], writes=["h%d" % j])

        def ln_stats(src_aps, skeys, n):
            for g_ in range(n):
                S.op("dve", (lambda g_: lambda e: e.bn_stats(st6[:, g_, :], src_aps[g_]))(g_),
                     reads=[skeys[g_]], writes=["st6_%d" % g_])
                S.op("dve", (lambda g_: lambda e: e.bn_aggr(mv[:, g_, :], st6[:, g_, :]))(g_),
                     reads=["st6_%d" % g_], writes=["mv_%d" % g_])
            mvk = ["mv_%d" % g_ for g_ in range(n)]
            S.op("act", lambda e: e.activation(out=sd4[:, 0:n], in_=mv[:, 0:n, 1], func=AF.Sqrt, bias=EPS, scale=1.0),
                 reads=mvk, writes=["sd4"])
            S.op("dve", lambda e: e.reciprocal(rs4[:, 0:n], sd4[:, 0:n]), reads=["sd4"], writes=["rs4"])
            S.op("dve", lambda e: e.scalar_tensor_tensor(out=nm4[:, 0:n], in0=mv[:, 0:n, 0], scalar=-1.0,
                                                         in1=rs4[:, 0:n], op0=ALU.mult, op1=ALU.mult),
                 reads=mvk + ["rs4"], writes=["nm4"])

        def even_mixer(l):
            jl = l // 2
            w_in = ab_w_in[jl]
            sp_dma(Gm[:], mix_g[l:l + 1, :].partition_broadcast(128), "Gm")
            sp_dma(RG[:], ret_g[jl:jl + 1, :].partition_broadcast(128), "RG")
            sp_dma(SG[:], sgu_g[jl:jl + 1, :].partition_broadcast(128), "SG")
            sp_dma(SBB[:], sgu_bb[jl:jl + 1, :].partition_broadcast(128), "SBB")
            sp_dma(sbias[:], sgu_bT[jl], "sbias")
            S.dma("pool", lambda e: e.dma_start(out=WT[:], in_=sgu_wT[jl].rearrange("g q p -> q g p")), write="WT")
            S.op("pool", lambda e: e.memset(WT[64:128, :, 0:64], 0.0), reads=["WT"], writes=["WT"])
            sp_dma(Gf[:], ffn_g[l:l + 1, :].partition_broadcast(128), "Gf")
            for ct in range(6):
                sl, skey = load_tile([(0, 8, 512, wcols(w_in, ct * 512, 512))])
                for j in range(NBLK):
                    bk, bkey = proj_block(j, sl, skey)
                    if ct == 0:
                        rope_block(j, bk, bkey, None)
                        bb, tkey = transpose4(lambda hh: rot[:, hh * 128:(hh + 1) * 128], "rot")
                        S.op("dve", (lambda j, bb: lambda e: e.tensor_tensor(
                            out=qdT[:, j, :, :], in0=bb[:, 0:512].rearrange("p (a b) -> p a b", a=4),
                            in1=QDT[:, :].rearrange("p (a b) -> p a b", a=4), op=ALU.mult))(j, bb),
                             reads=[tkey, "QDT"], writes=["qdT%d" % j])
                    elif ct == 1:
                        rope_block(j, bk, bkey, None)
                        S.op("dve", (lambda j: lambda e: e.tensor_tensor(out=kd[:, j, :], in0=rot[:, :], in1=KD[:, :],
                                                                         op=ALU.mult))(j),
                             reads=["rot", "KD"], writes=["kd%d" % j])
                        bb, tkey = transpose4(lambda hh: rot[:, hh * 128:(hh + 1) * 128], "rot")
                        S.op("act", (lambda j, bb: lambda e: e.activation(
                            out=kT[:, j, :, :], in_=bb[:, 0:512].rearrange("p (a b) -> p a b", a=4), func=AF.Copy))(j, bb),
                             reads=[tkey], writes=["kT%d" % j])
                    elif ct == 2:
                        S.op("act", (lambda j, bk: lambda e: e.activation(out=vb[:, j, :], in_=bk[:, :], func=AF.Copy))(j, bk),
                             reads=[bkey], writes=["vb%d" % j])
                    elif ct == 3:
                        S.op("act", (lambda j, bk: lambda e: e.activation(out=sgl[:, j, :], in_=bk[:, :], func=AF.Silu))(j, bk),
                             reads=[bkey], writes=["sgl%d" % j])
                    elif ct == 4:
                        S.op("act", (lambda j, bk: lambda e: e.activation(out=ug[:, j, :], in_=bk[:, :],
                                                                          func=AF.Gelu_apprx_tanh))(j, bk),
                             reads=[bkey], writes=["ug%d" % j])
                    else:
                        S.op("act", (lambda bk: lambda e: e.activation(out=gv[:, :], in_=bk[:, :],
                                                                       func=AF.Gelu_apprx_tanh))(bk),
                             reads=[bkey], writes=["gv"])
                        ln_stats([gv[:, :]], ["gv"], 1)
                        S.op("act", lambda e: e.activation(out=gv[:, :], in_=gv[:, :], func=AF.Identity,
                                                           scale=rs4[:, 0:1], bias=nm4[:, 0:1]),
                             reads=["gv", "rs4", "nm4"], writes=["gv"])
                        S.op("dve", lambda e: e.tensor_tensor(out=gv[:, :], in0=gv[:, :], in1=SG[:, :], op=ALU.mult),
                             reads=["gv", "SG"], writes=["gv"])
                        S.op("dve", (lambda j: lambda e: e.tensor_tensor(out=vsn[:, j, :], in0=gv[:, :], in1=SBB[:, :],
                                                                         op=ALU.add))(j),
                             reads=["gv", "SBB"], writes=["vsn%d" % j])
            otiles = [load_tile([(0, 8, 512, wcols(ab_w_out[jl], half * 512, 512))]) for half in range(2)]
            Sr, Srb = Sret[jl], Sretb[jl]
            sk, sbk = "Sret%d" % jl, "Sretb%d" % jl

            def S1(j):
                pk, pkey = B(0)
                for hh in range(4):
                    S.op("pe", lambda e: e.matmul(pk[:, hh * 128:(hh + 1) * 128], kT[:, j, hh, :], qdT[:, j, hh, :],
                                                  start=True, stop=True),
                         reads=["kT%d" % j, "qdT%d" % j], writes=[pkey], safe_same=[pkey])
                S.op("dve", lambda e: e.tensor_tensor(out=Pb[:, :], in0=pk[:, :], in1=DQ[:, :], op=ALU.mult),
                     reads=[pkey, "DQ"], writes=["Pb"])
                ok_, okey = B(1 + (j % 2))
                for hh in range(4):
                    S.op("pe", lambda e: e.matmul(ok_[:, hh * 128:(hh + 1) * 128], Pb[:, hh * 128:(hh + 1) * 128],
                                                  vb[:, j, hh * 128:(hh + 1) * 128], start=True, stop=False),
                         reads=["Pb", "vb%d" % j], writes=[okey], safe_same=[okey])
                    S.op("pe", lambda e: e.matmul(ok_[:, hh * 128:(hh + 1) * 128], qdT[:, j, hh, :],
                                                  Srb[:, hh * 128:(hh + 1) * 128], start=False, stop=True),
                         reads=["qdT%d" % j, sbk], writes=[okey], safe_same=[okey])
                kk, kkey = B(3)
                for hh in range(4):
                    S.op("pe", lambda e: e.matmul(kk[:, hh * 128:(hh + 1) * 128], kd[:, j, hh * 128:(hh + 1) * 128],
                                                  vb[:, j, hh * 128:(hh + 1) * 128], start=True, stop=True),
                         reads=["kd%d" % j, "vb%d" % j], writes=[kkey], safe_same=[kkey])
                for hh in range(4):
                    S.op("dve", lambda e: e.scalar_tensor_tensor(
                        out=Sr[:, hh * 128:(hh + 1) * 128], in0=Sr[:, hh * 128:(hh + 1) * 128], scalar=G128[hh],
                        in1=kk[:, hh * 128:(hh + 1) * 128], op0=ALU.mult, op1=ALU.add),
                         reads=[kkey, sk + "_%d" % hh], writes=[sk + "_%d" % hh])
                S.op("act", lambda e: e.activation(out=Srb[:, :], in_=Sr[:, :], func=AF.Copy),
                     reads=[sk + "_%d" % hh for hh in range(4)], writes=[sbk])

            def S2(j):
                ok_, okey = B(1 + (j % 2))
                rnk = ["rn_%d" % hh for hh in range(4)]
                ln_stats([ok_[:, hh * 128:(hh + 1) * 128] for hh in range(4)], [okey] * 4, 4)
                for hh in range(4):
                    S.op("act", lambda e: e.activation(
                        out=rn[:, hh * 128:(hh + 1) * 128], in_=ok_[:, hh * 128:(hh + 1) * 128], func=AF.Identity,
                        scale=rs4[:, hh:hh + 1], bias=nm4[:, hh:hh + 1]),
                         reads=[okey, "rs4", "nm4"], writes=["rn_%d" % hh])
                S.op("dve", lambda e: e.tensor_tensor(out=rn[:, :], in0=rn[:, :], in1=RG[:, :], op=ALU.mult),
                     reads=rnk + ["RG"], writes=rnk)
                S.op("dve", lambda e: e.tensor_tensor(out=mixed[:, 0:512], in0=rn[:, :], in1=sgl[:, j, :], op=ALU.mult),
                     reads=rnk + ["sgl%d" % j], writes=["mixed"])
                gk, gkey = B(4)
                for g_ in range(4):
                    S.op("pe", lambda e: e.matmul(gk[:, g_ * 128:(g_ + 1) * 128], WT[:, g_, :],
                                                  vsn[:, j, g_ * 128:(g_ + 1) * 128], start=True, stop=True),
                         reads=["WT", "vsn%d" % j], writes=[gkey], safe_same=[gkey])
                for g_ in range(4):
                    S.op("dve", lambda e: e.scalar_tensor_tensor(
                        out=mixed[:, 512 + g_ * 128:512 + (g_ + 1) * 128], in0=gk[:, g_ * 128:(g_ + 1) * 128],
                        scalar=sbias[:, g_:g_ + 1], in1=ug[:, j, g_ * 128:(g_ + 1) * 128],
                        op0=ALU.add, op1=ALU.mult),
                         reads=[gkey, "sbias", "ug%d" % j], writes=["mixed"])
                out_proj_block(j, otiles, B(5), [B(6), B(7)])
                norm_block(j, Gf, "Gf", B(5))

            S1(0)
            for j in range(NBLK):
                if j + 1 < NBLK:
                    S1(j + 1)
                S2(j)

        def odd_mixer(l):
            jl = l // 2
            w_in = c_w_in[jl]
            C = Cst[jl]
            ck = "Cst%d" % jl
            msk = "mst%d" % jl
            ms = mst[jl]
            sp_dma(Gm[:], mix_g[l:l + 1, :].partition_broadcast(128), "Gm")
            sp_dma(CNG[:], c_ng[jl:jl + 1, :].partition_broadcast(128), "CNG")
            sp_dma(bi[:], c_bi[jl], "bi")
            sp_dma(bfm[:], c_bf[jl], "bfm")
            S.op("dve", lambda e: e.tensor_scalar(bfm[:], bfm[:], -1.0, None, ALU.mult), reads=["bfm"], writes=["bfm"])
            sp_dma(Gf[:], ffn_g[l:l + 1, :].partition_broadcast(128), "Gf")
            sl, skey = load_tile([(0, 8, 8, wcols(w_in, 3072, 8))])
            wv = sl[:, 0:64].rearrange("p (a b) -> p a b", a=8)
            pi, pikey = newbank()
            pf, pfkey = newbank()
            for kc in range(8):
                S.op("pe", (lambda kc: lambda e: e.matmul(pi[0:4, :], wv[:, kc, 0:4], hnT[:, kc, :],
                                                          start=(kc == 0), stop=(kc == 7)))(kc),
                     reads=["hnT%d" % j for j in range(NBLK)] + [skey], writes=[pikey], safe_same=[pikey])
            for kc in range(8):
                S.op("pe", (lambda kc: lambda e: e.matmul(pf[0:4, :], wv[:, kc, 4:8], hnT[:, kc, :],
                                                          start=(kc == 0), stop=(kc == 7)))(kc),
                     reads=["hnT%d" % j for j in range(NBLK)] + [skey], writes=[pfkey], safe_same=[pfkey])
            S.op("act", lambda e: e.activation(out=li[:, :], in_=pi[0:4, :], func=AF.Identity, bias=bi[:, 0:1], scale=1.0),
                 reads=[pikey, "bi"], writes=["li"])
            S.op("act", lambda e: e.activation(out=ex[:, :], in_=pf[0:4, :], func=AF.Exp, bias=bfm[:, 0:1], scale=-1.0),
                 reads=[pfkey, "bfm"], writes=["lfn"])
            S.op("act", lambda e: e.activation(out=lfn[:, :], in_=ex[:, :], func=AF.Ln, bias=1.0, scale=1.0),
                 reads=["lfn"], writes=["lfn"])
            for j in range(NBLK):
                cs_ = slice(j * 128, (j + 1) * 128)
                S.op("dve", (lambda cs_: lambda e: e.tensor_tensor_scan(nb[:, :], lfn[:, cs_], zrow[:, :], 0.0,
                                                                        ALU.add, ALU.add))(cs_),
                     reads=["lfn", "zrow"], writes=["nb"])
                S.op("dve", (lambda cs_: lambda e: e.tensor_tensor(out=aa[:, :], in0=li[:, cs_], in1=nb[:, :],
                                                                   op=ALU.add))(cs_),
                     reads=["li", "nb"], writes=["aa"])
                S.op("dve", lambda e: e.tensor_tensor_scan(gg[:, :], aa[:, :], aa[:, :], ms[:, 0:1], ALU.max, ALU.max),
                     reads=["aa", msk], writes=["gg"])
                S.op("dve", (lambda j: lambda e: e.tensor_scalar(ngl[:, j:j + 1], gg[:, 127:128], -1.0, None, ALU.mult))(j),
                     reads=["gg"], writes=["ngl%d" % j])
                S.op("act", (lambda j: lambda e: e.activation(out=dec[:, j:j + 1], in_=ms[:, 0:1], func=AF.Exp,
                                                              bias=ngl[:, j:j + 1], scale=1.0))(j),
                     reads=[msk, "ngl%d" % j], writes=["dec%d" % j])
                S.op("act", (lambda j: lambda e: e.activation(out=R3[:, 0, :], in_=aa[:, :], func=AF.Exp,
                                                              bias=ngl[:, j:j + 1], scale=1.0))(j),
                     reads=["aa", "ngl%d" % j], writes=["R3_0"])
                S.op("act", lambda e: e.activation(out=R3[:, 1, :], in_=gg[:, :], func=AF.Exp,
                                                   bias=gg[:, 127:128], scale=-1.0),
                     reads=["gg"], writes=["R3_1"])
                S.op("dve", lambda e: e.tensor_tensor(out=tmpr[:, :], in0=nb[:, :], in1=gg[:, :], op=ALU.subtract),
                     reads=["nb", "gg"], writes=["tmpr"])
                S.op("act", lambda e: e.activation(out=R3[:, 2, :], in_=tmpr[:, :], func=AF.Exp),
                     reads=["tmpr"], writes=["R3_2"])
                S.op("dve", lambda e: e.tensor_tensor(out=ms[:, 0:1], in0=gg[:, 127:128], in1=nb[:, 127:128],
                                                      op=ALU.subtract),
                     reads=["gg", "nb"], writes=[msk])
                S.op("dve", (lambda j: lambda e: e.tensor_scalar(dg[:, j, :], idf[0:4, 0:4], dec[:, j:j + 1], None, ALU.mult))(j),
                     reads=["idf", "dec%d" % j], writes=["dg%d" % j])
                ck_, ckey = newbank()
                for q_ in range(3):
                    S.op("pe", (lambda q_, ck_: lambda e: e.matmul(ck_[:, q_ * 4:(q_ + 1) * 4], R3[:, q_, :],
                                                                  idf[0:4, 0:4], start=True, stop=True))(q_, ck_),
                         reads=["R3_%d" % q_, "idf"], writes=[ckey], safe_same=[ckey])
                S.op("pe", (lambda j, ck_: lambda e: e.matmul(ck_[:, 12:16], ones4[:, :], dg[:, j, :],
                                                              start=True, stop=True))(j, ck_),
                     reads=["ones4", "dg%d" % j], writes=[ckey], safe_same=[ckey])
                S.op("dve", (lambda j, ck_: lambda e: e.tensor_copy(cols[:, j, :], ck_[:, 0:16]))(j, ck_),
                     reads=[ckey], writes=["cols%d" % j])
                S.op("dve", (lambda j: lambda e: e.tensor_scalar(cols[:, j, 0:4], cols[:, j, 0:4], 128.0 ** -0.5, None,
                                                                 ALU.mult))(j),
                     reads=["cols%d" % j], writes=["cols%d" % j])
            sl, skey = load_tile([(0, 8, 512, wcols(w_in, 0, 512))])
            for j in range(NBLK):
                bk, bkey = proj_block(j, sl, skey)
                for hh in range(4):
                    S.op("act", (lambda hh, bk, j: lambda e: e.activation(
                        out=rot[:, hh * 128:(hh + 1) * 128], in_=bk[:, hh * 128:(hh + 1) * 128], func=AF.Copy,
                        scale=cols[:, j, 4 + hh:5 + hh]))(hh, bk, j),
                         reads=[bkey, "cols%d" % j], writes=["rot"])
                bb, tkey = transpose4(lambda hh: rot[:, hh * 128:(hh + 1) * 128], "rot")
                S.op("act", (lambda j, bb: lambda e: e.activation(
                    out=qrT[:, j, :, :], in_=bb[:, 0:512].rearrange("p (a b) -> p a b", a=4), func=AF.Copy))(j, bb),
                     reads=[tkey], writes=["qdT%d" % j])
            sl, skey = load_tile([(0, 8, 512, wcols(w_in, 512, 512))])
            for j in range(NBLK):
                bk, bkey = proj_block(j, sl, skey)
                for hh in range(4):
                    S.op("act", (lambda hh, bk, j: lambda e: e.activation(
                        out=kdc[:, j, hh * 128:(hh + 1) * 128], in_=bk[:, hh * 128:(hh + 1) * 128], func=AF.Copy,
                        scale=cols[:, j, hh:hh + 1]))(hh, bk, j),
                         reads=[bkey, "cols%d" % j], writes=["kd%d" % j])
                bb, tkey = transpose4((lambda j: lambda hh: kdc[:, j, hh * 128:(hh + 1) * 128])(j), "kd%d" % j)
                S.op("act", (lambda j, bb: lambda e: e.activation(
                    out=kdT[:, j, :, :], in_=bb[:, 0:512].rearrange("p (a b) -> p a b", a=4), func=AF.Copy))(j, bb),
                     reads=[tkey], writes=["kT%d" % j])
            for vt in range(2):
                sl, skey = load_tile([(0, 8, 512, wcols(w_in, 1024 + vt * 512, 512))])
                for j in range(NBLK):
                    bk, bkey = proj_block(j, sl, skey)
                    S.op("act", (lambda j, bk, vt: lambda e: e.activation(
                        out=vext[:, j, 2 * vt:2 * vt + 2, 0:256], in_=bk[:, :].rearrange("p (a b) -> p a b", a=2),
                        func=AF.Copy))(j, bk, vt),
                         reads=[bkey], writes=["vb%d" % j])
            for j in range(NBLK):
                S.op("dve", (lambda j: lambda e: e.memset(vext[:, j, :, 256:257], 1.0))(j), reads=[], writes=["vb%d" % j])
            for ot in range(2):
                sl, skey = load_tile([(0, 8, 512, wcols(w_in, 2048 + ot * 512, 512))])
                for j in range(NBLK):
                    bk, bkey = proj_block(j, sl, skey)
                    S.op("act", (lambda j, bk, ot: lambda e: e.activation(
                        out=so[:, j, ot * 512:(ot + 1) * 512], in_=bk[:, :], func=AF.Sigmoid))(j, bk, ot),
                         reads=[bkey], writes=["so%d" % j])
            otiles = [load_tile([(0, 8, 512, wcols(c_w_out[jl], half * 512, 512))]) for half in range(2)]
            def S1(j):
                nd = NDs[j % 2]
                for hh in range(4):
                    chk = ck + "_%d" % hh
                    S.op("dve", lambda e: e.tensor_scalar(Cb[:, hh, 0:257], C[:, hh, 0:257],
                                                          cols[:, j, 12 + hh:13 + hh], None, ALU.mult),
                         reads=[chk, "cols%d" % j], writes=["Cb_%d" % hh])
                    pk, pkey = B(hh % 2)
                    S.op("pe", lambda e: e.matmul(pk[:, 0:128], kdT[:, j, hh, :], qrT[:, j, hh, :], start=True, stop=True),
                         reads=["kT%d" % j, "qdT%d" % j], writes=[pkey])
                    S.op("dve", lambda e: e.tensor_tensor(out=Pb[:, hh * 128:(hh + 1) * 128], in0=pk[:, 0:128],
                                                          in1=CM[:, :], op=ALU.mult),
                         reads=[pkey, "CM"], writes=["Pb_%d" % hh])
                    nk, nkey = B(2 + hh % 2)
                    S.op("pe", lambda e: e.matmul(nk[:, 0:257], Pb[:, hh * 128:(hh + 1) * 128],
                                                  vext[:, j, hh, 0:257], start=True, stop=False),
                         reads=["Pb_%d" % hh, "vb%d" % j], writes=[nkey])
                    S.op("pe", lambda e: e.matmul(nk[:, 0:257], qrT[:, j, hh, :], Cb[:, hh, 0:257], start=False, stop=True),
                         reads=["qdT%d" % j, "Cb_%d" % hh], writes=[nkey], safe_same=[nkey])
                    kk, kkey = B(4 + hh % 2)
                    S.op("pe", lambda e: e.matmul(kk[:, 0:257], kdc[:, j, hh * 128:(hh + 1) * 128],
                                                  vext[:, j, hh, 0:257], start=True, stop=True),
                         reads=["kd%d" % j, "vb%d" % j], writes=[kkey])
                    S.op("dve", lambda e: e.scalar_tensor_tensor(
                        out=C[:, hh, 0:257], in0=C[:, hh, 0:257], scalar=cols[:, j, 12 + hh:13 + hh], in1=kk[:, 0:257],
                        op0=ALU.mult, op1=ALU.add),
                         reads=[kkey, chk, "cols%d" % j], writes=[chk])
                    S.op("act", lambda e: e.activation(out=nd[:, hh, 0:257], in_=nk[:, 0:257], func=AF.Copy),
                         reads=[nkey], writes=["NDs%d_%d" % (j % 2, hh)])

            def S2(j):
                nd = NDs[j % 2]
                ndk = ["NDs%d_%d" % (j % 2, hh) for hh in range(4)]
                hyk = ["hy_%d" % hh for hh in range(4)]
                S.op("act", lambda e: e.activation(out=low4[:, :], in_=nd[:, :, 256], func=AF.Abs),
                     reads=ndk, writes=["low4"])
                S.op("dve", lambda e: e.tensor_tensor(out=low4[:, :], in0=low4[:, :], in1=cols[:, j, 8:12], op=ALU.max),
                     reads=["low4", "cols%d" % j], writes=["low4"])
                S.op("dve", lambda e: e.reciprocal(rl4[:, :], low4[:, :]), reads=["low4"], writes=["rl4"])
                for hh in range(4):
                    S.op("act", lambda e: e.activation(out=nd[:, hh, 0:256], in_=nd[:, hh, 0:256], func=AF.Copy,
                                                       scale=rl4[:, hh:hh + 1]),
                         reads=[ndk[hh], "rl4"], writes=[ndk[hh]])
                ln_stats([nd[:, hh, 0:256] for hh in range(4)], ndk, 4)
                for hh in range(4):
                    S.op("act", lambda e: e.activation(out=hy[:, hh * 256:(hh + 1) * 256], in_=nd[:, hh, 0:256],
                                                       func=AF.Identity, scale=rs4[:, hh:hh + 1], bias=nm4[:, hh:hh + 1]),
                         reads=[ndk[hh], "rs4", "nm4"], writes=["hy_%d" % hh])
                S.op("dve", lambda e: e.tensor_tensor(out=hy[:, :], in0=hy[:, :], in1=CNG[:, :], op=ALU.mult),
                     reads=hyk + ["CNG"], writes=hyk)
                S.op("dve", lambda e: e.tensor_tensor(out=mixed[:, :], in0=hy[:, :], in1=so[:, j, :], op=ALU.mult),
                     reads=hyk + ["so%d" % j], writes=["mixed"])
                out_proj_block(j, otiles, B(6), [B(7), B(6)])
                norm_block(j, Gf, "Gf", B(7))

            S1(0)
            for j in range(NBLK):
                if j + 1 < NBLK:
                    S1(j + 1)
                S2(j)

        def ffn(l, last, r0):
            if not last:
                sp_dma(Gm[:], mix_g[l + 1:l + 2, :].partition_broadcast(128), "Gm")
            hk = ["hnT%d" % j for j in range(NBLK)]
            for ft in range(NFI // 2):
                sl, skey = load_tile([(0, 8, 256, wcols(w_gate[l], ft * 256, 256)),
                                      (2048, 8, 256, wcols(w_up[l], ft * 256, 256))])
                gvw = sl[:, 0:2048].rearrange("p (a b) -> p a b", a=8)
                uvw = sl[:, 2048:4096].rearrange("p (a b) -> p a b", a=8)
                for sub in range(2):
                    fi = ft * 2 + sub
                    pg, pgk = newbank()
                    pu, puk = newbank()
                    for kc in range(8):
                        S.op("pe", (lambda kc, pg, sub, gvw: lambda e: e.matmul(pg[:, :], gvw[:, kc, sub * 128:(sub + 1) * 128],
                                                                           hnT[:, kc, :], start=(kc == 0), stop=(kc == 7)))(kc, pg, sub, gvw),
                             reads=hk + [skey], writes=[pgk], safe_same=[pgk])
                    for kc in range(8):
                        S.op("pe", (lambda kc, pu, sub, uvw: lambda e: e.matmul(pu[:, :], uvw[:, kc, sub * 128:(sub + 1) * 128],
                                                                           hnT[:, kc, :], start=(kc == 0), stop=(kc == 7)))(kc, pu, sub, uvw),
                             reads=hk + [skey], writes=[puk], safe_same=[puk])
                    S.op("act", (lambda pg: lambda e: e.activation(out=sgt[:, :], in_=pg[:, :], func=AF.Silu))(pg),
                         reads=[pgk], writes=["sgt"])
                    S.op("dve", (lambda pu, fi: lambda e: e.tensor_tensor(out=actT[:, fi, :], in0=sgt[:, :], in1=pu[:, :],
                                                                          op=ALU.mult))(pu, fi),
                         reads=["sgt", puk], writes=["actT%d" % fi])
            accs = [[newbank() for half in range(2)] for j in range(NBLK)]
            wdv = w_down[l].rearrange("(fi p) n -> p fi n", p=128)
            f0 = 0
            while f0 < NFI:
                nf = min(4, NFI - f0)
                sl, skey = load_tile([(0, nf, 1024, wdv[:, f0:f0 + nf, :])])
                wv = sl[:, 0:nf * 1024].rearrange("p (a b) -> p a b", a=nf)
                for fl in range(nf):
                    fi = f0 + fl
                    for j in range(NBLK):
                        for half in range(2):
                            ak, akey = accs[j][half]
                            S.op("pe", (lambda fi, fl, j, half, ak, wv: lambda e: e.matmul(
                                ak[:, :], actT[:, fi, j * 128:(j + 1) * 128], wv[:, fl, half * 512:(half + 1) * 512],
                                start=(fi == 0), stop=(fi == NFI - 1)))(fi, fl, j, half, ak, wv),
                                 reads=["actT%d" % fi, skey], writes=[akey], safe_same=[akey])
                f0 += nf
            for j in range(NBLK):
                for half in range(2):
                    ak, akey = accs[j][half]
                    S.op("dve", lambda e: e.tensor_tensor(
                        out=h[:, j, half * 512:(half + 1) * 512], in0=ak[:, :], in1=h[:, j, half * 512:(half + 1) * 512],
                        op=ALU.add), reads=[akey, "h%d" % j], writes=["h%d" % j])
                if last:
                    final_block(j, r0)
                else:
                    norm_block(j, Gm, "Gm", accs[j][0])

        sp_dma(Gfin[:], fin_g[0:1, :].partition_broadcast(128), "Gfin")
        for sb in range(nsb):
            r0 = sb * SBT
            for j in range(NBLK):
                S.dma("sp", lambda e: e.dma_start(out=h[:, j, :], in_=x[r0 + j * 128:r0 + (j + 1) * 128, :]),
                      write="h%d" % j)
            S.dma("sp", lambda e: e.dma_start(out=CS[:, :, :], in_=cs_d[r0:r0 + SBT, :].rearrange("(j p) n -> p j n", p=128)),
                  write="CS")
            first_mix = ('mix' in parts)
            sp_dma(Gm[:], (mix_g if first_mix else ffn_g)[0:1, :].partition_broadcast(128), "Gm")
            for j in range(NBLK):
                norm_block(j, Gm, "Gm")
            for l in range(depth):
                if 'mix' in parts:
                    if l % 2 == 0:
                        even_mixer(l)
                    else:
                        odd_mixer(l)
                if 'ffn' in parts:
                    ffn(l, l == depth - 1, r0)
        S.emit(nc)
    return nc


_NC_CACHE = {}


def _prep_inputs(inputs, b, nsb):
    seq = nsb * SBT
    tabs, _ = _const_tables(seq)
    f = lambda a: np.ascontiguousarray(np.asarray(a, dtype=np.float32))
    m = {
        "x": f(inputs["x"][b, :seq]),
        "mix_norm_g": f(inputs["mix_norm_g"]),
        "ffn_norm_g": f(inputs["ffn_norm_g"]),
        "final_norm_g": f(inputs["final_norm_g"]).reshape(1, D),
        "ab_w_in": f(inputs["ab_w_in"]),
        "ab_w_out": f(inputs["ab_w_out"]),
        "ret_norm_g": f(inputs["ret_norm_g"]).reshape(2, 512),
        "sgu_ln_g": f(inputs["sgu_ln_g"]),
        "sgu_ln_b": f(inputs["sgu_ln_b"]),
        "sgu_wT": f(np.transpose(np.asarray(inputs["sgu_w"]), (0, 1, 3, 2))),
        "sgu_bT": f(np.transpose(np.asarray(inputs["sgu_b"]), (0, 2, 1))),
        "c_w_in": f(inputs["c_w_in"]),
        "c_w_out": f(inputs["c_w_out"]),
        "c_b_i": f(inputs["c_b_i"]).reshape(2, 4, 1),
        "c_b_f": f(inputs["c_b_f"]).reshape(2, 4, 1),
        "c_norm_g": f(inputs["c_norm_g"]).reshape(2, 1024),
        "ffn_w_gate": f(inputs["ffn_w_gate"]),
        "ffn_w_up": f(inputs["ffn_w_up"]),
        "ffn_w_down": f(inputs["ffn_w_down"]),
    }
    m.update(tabs)
    return m


def run(inputs, nsb=SEQ // SBT, depth=DEPTH, ncores=8, trace=False, parts=('mix', 'ffn')):
    key = (nsb, depth, parts)
    if key not in _NC_CACHE:
        _NC_CACHE[key] = build(nsb, depth, parts)
    nc = _NC_CACHE[key]
    in_maps = [_prep_inputs(inputs, b, nsb) for b in range(ncores)]
    res = run_bass_kernel_spmd(nc, in_maps, core_ids=list(range(ncores)), **({"trace": True} if trace else {}))
    out = np.stack([np.asarray(r["y"], dtype=np.float32) for r in res.results], axis=0)
    return out, res


def kernel(**inputs):
    out, _ = run(inputs)
    return out
```

```python
import numpy as np
from contextlib import ExitStack
import concourse.bass as bass
import concourse.mybir as mybir
from concourse.bass_utils import run_bass_kernel_spmd

F32 = mybir.dt.float32
BF16 = mybir.dt.bfloat16
AF = mybir.ActivationFunctionType
ALU = mybir.AluOpType

D = 1024
SEQ = 4096
DEPTH = 4
TB = 128
NBLK = 4
SBT = TB * NBLK
DFF = 2816
NFI = DFF // 128
EPS = 1e-6
AB_IN = 3072
C_IN = 3080
NSLOT = 5
MODEL_FREE_BANKS = set()
POOL_OFFLOAD = set()
COPY_ON_DVE = {'hnT', 'mT'}
KSPLIT = 3


def _pe(tag):
    return "pool" if tag in POOL_OFFLOAD else "dve"


class _Op:
    __slots__ = ("fn", "eng", "seq", "deps", "odeps", "is_dma", "token", "dur", "xfer",
                 "signal", "sigval", "pos", "waits", "finish", "nsucc", "succ", "npend", "ready", "prio", "tag")

    def __init__(self, fn, eng, seq):
        self.fn = fn
        self.eng = eng
        self.seq = seq
        self.deps = []
        self.odeps = []
        self.is_dma = False
        self.token = None
        self.dur = 0.1
        self.xfer = 0.0
        self.signal = False
        self.sigval = None
        self.pos = None
        self.waits = []
        self.finish = 0.0
        self.succ = []
        self.npend = 0
        self.ready = 0.0


class _Rec:
    def __init__(self):
        self.calls = []

    def __getattr__(self, name):
        def f(*a, **k):
            self.calls.append((name, a, k))
            return None
        return f


def _freeze(fn, eng):
    r = _Rec()
    fn(r)
    assert len(r.calls) == 1, r.calls
    name, a, k = r.calls[0]
    out = k.get("out", a[0] if a else None)
    try:
        shp = tuple(out.shape)
        n = 1
        for d_ in shp[1:]:
            n *= int(d_)
        npart = int(shp[0])
    except Exception:
        n, npart = 64, 128
    if eng == "pe":
        dur = 0.11 if name == "transpose" else 0.012 + n / 1950.0
    elif eng == "act":
        dur = 0.22 + n * 1.0e-3
    elif eng == "dve":
        dur = 0.10 + n * 1.2e-3
    elif eng == "pool":
        dur = 0.2 + n * 2.0e-3
    else:
        dur = 0.1
    xfer = 0.0
    if name == "dma_start":
        dur = 1.0 if eng == "pool" else 0.15
        xfer = 2.0 + (n * npart * 4) / 300e3
    return (lambda h: getattr(h, name)(*a, **k)), dur, xfer


class _Buf:
    __slots__ = ("writer", "readers", "dma_sem", "dma_count")

    def __init__(self):
        self.writer = None
        self.readers = []
        self.dma_sem = None
        self.dma_count = 0


class Sched:
    ENGS = ("pe", "act", "dve", "pool", "sp")
    RESCHEDULE = True
    PRIO = "cp"

    def __init__(self):
        self.all = []
        self.bufs = {}
        self.dma_sems = []
        self.out_ops = {}
        self.cur_tag = ""

    def _buf(self, k):
        b = self.bufs.get(k)
        if b is None:
            b = self.bufs[k] = _Buf()
        return b

    def op(self, eng, fn, reads=(), writes=(), safe_same=(), dur=None):
        f, dur0, _ = _freeze(fn, eng)
        dur = dur0 if dur is None else dur
        o = _Op(f, eng, len(self.all))
        o.dur = dur
        o.tag = self.cur_tag
        seen = set()
        for k in reads:
            b = self._buf(k)
            if b.writer is not None:
                self._dep(o, b.writer, k in safe_same, seen)
        for k in writes:
            b = self._buf(k)
            if b.writer is not None:
                self._dep(o, b.writer, k in safe_same, seen)
            for r in b.readers:
                self._dep(o, r, k in safe_same, seen)
        self.all.append(o)
        for k in reads:
            self._buf(k).readers.append(o)
        for k in writes:
            b = self._buf(k)
            b.writer = o
            b.readers = []
        return o

    def _dep(self, o, d, safe, seen):
        if d is o:
            return
        key = (id(d), safe and d.eng == o.eng and not d.is_dma)
        if key in seen:
            return
        seen.add(key)
        if safe and d.eng == o.eng and not d.is_dma:
            o.odeps.append(d)
        else:
            o.deps.append(d)

    def dma(self, eng, fn, reads=(), write=None, is_output=False):
        f, dur, xfer = _freeze(fn, eng)
        o = _Op(f, eng, len(self.all))
        o.is_dma = True
        o.dur = dur
        o.xfer = xfer
        o.tag = self.cur_tag
        b = self._buf(write)
        if b.dma_sem is None:
            b.dma_sem = "dsem%d" % len(self.dma_sems)
            self.dma_sems.append(b.dma_sem)
        seen = set()
        for k in reads:
            rb = self._buf(k)
            if rb.writer is not None:
                self._dep(o, rb.writer, False, seen)
        if b.writer is not None:
            self._dep(o, b.writer, False, seen)
        for r in b.readers:
            self._dep(o, r, False, seen)
        b.dma_count += 1
        o.token = (b.dma_sem, 16 * b.dma_count)
        self.all.append(o)
        for k in reads:
            self._buf(k).readers.append(o)
        b.writer = o
        b.readers = []
        if is_output:
            self.out_ops[b.dma_sem] = o
        return o

    def _schedule(self):
        import heapq
        order = {e: [] for e in self.ENGS}
        if not self.RESCHEDULE:
            for o in self.all:
                order[o.eng].append(o)
            return order
        LAT = 0.25
        for o in self.all:
            o.succ = []
            o.npend = 0
        for o in self.all:
            for d in o.deps:
                d.succ.append((o, True))
                o.npend += 1
            for d in o.odeps:
                d.succ.append((o, False))
                o.npend += 1
        if self.PRIO == "cp":
            for o in self.all:
                o.prio = 0.0
            for o in reversed(self.all):
                b = 0.0
                for (s_, hard) in o.succ:
                    v = s_.prio + (LAT if hard else 0.0)
                    if v > b:
                        b = v
                o.prio = b + (o.xfer if o.is_dma else o.dur)
            for o in self.all:
                o.seq = -o.prio + o.seq * 1e-9
        waiting = {e: [] for e in self.ENGS}
        runnable = {e: [] for e in self.ENGS}
        tfree = {e: 0.0 for e in self.ENGS}
        dma_free = [0.0]
        for o in self.all:
            if o.npend == 0:
                heapq.heappush(waiting[o.eng], (0.0, o.seq, o))
        left = len(self.all)
        while left:
            best = None
            for e in self.ENGS:
                w, r = waiting[e], runnable[e]
                while w and w[0][0] <= tfree[e]:
                    _, sq, o = heapq.heappop(w)
                    heapq.heappush(r, (sq, o))
                if r:
                    cand = (tfree[e], r[0][0], e, True)
                elif w:
                    cand = (w[0][0], w[0][1], e, False)
                else:
                    continue
                if best is None or cand < best:
                    best = cand
            start, _, e, from_r = best
            if from_r:
                _, o = heapq.heappop(runnable[e])
            else:
                _, _, o = heapq.heappop(waiting[e])
            tfree[e] = start + o.dur
            if o.is_dma:
                t0 = max(start + o.dur, dma_free[0])
                dma_free[0] = t0 + (o.xfer - 2.0)
                o.finish = t0 + o.xfer
            else:
                o.finish = start + o.dur
            order[e].append(o)
            left -= 1
            for (s_, hard) in o.succ:
                rdy = o.finish + (LAT if hard else 0.0)
                if rdy > s_.ready:
                    s_.ready = rdy
                s_.npend -= 1
                if s_.npend == 0:
                    heapq.heappush(waiting[s_.eng], (s_.ready, s_.seq, s_))
        return order

    def emit(self, nc, final_eng="sp"):
        EPOCH = 30000
        order = self._schedule()
        for e in self.ENGS:
            for i, o in enumerate(order[e]):
                o.pos = i
        for e in self.ENGS:
            known = {}
            for o in order[e]:
                o.waits = []
                for d in sorted(o.deps, key=lambda d_: -(d_.token[1] if d_.is_dma else d_.pos)):
                    if d.is_dma:
                        sname, val = d.token
                        if known.get(sname, -1) >= val:
                            continue
                        known[sname] = val
                        o.waits.append(d)
                    else:
                        if known.get(d.eng, -1) >= d.pos:
                            continue
                        known[d.eng] = d.pos
                        d.signal = True
                        o.waits.append(d)
                for d in o.odeps:
                    assert d.eng == e and d.pos < o.pos
        with ExitStack() as st:
            nsig = {}
            for e in self.ENGS:
                c = 0
                for o in order[e]:
                    if (not o.is_dma) and o.signal:
                        c += 1
                        o.sigval = c
                nsig[e] = c
            esem = {e: [st.enter_context(nc.semaphore("s_%s_%d" % (e, i)))
                        for i in range(max(1, (nsig[e] + EPOCH - 1) // EPOCH))] for e in self.ENGS}
            dsem = {n: st.enter_context(nc.semaphore(n)) for n in self.dma_sems}
            block = st.enter_context(nc.Block())

            def run(eng_name, handle):
                for o in order[eng_name]:
                    for d in o.waits:
                        if d.is_dma:
                            handle.wait_ge(dsem[d.token[0]], d.token[1])
                        else:
                            sv = d.sigval - 1
                            handle.wait_ge(esem[d.eng][sv // EPOCH], sv % EPOCH + 1)
                    ins = o.fn(handle)
                    if o.is_dma:
                        ins.then_inc(dsem[o.token[0]], 16)
                    elif o.signal:
                        ins.then_inc(esem[eng_name][(o.sigval - 1) // EPOCH], 1)
                if eng_name == final_eng:
                    for d in self.out_ops.values():
                        handle.wait_ge(dsem[d.token[0]], d.token[1])

            @block.tensor
            def _(h):
                run("pe", h)

            @block.scalar
            def _(h):
                run("act", h)

            @block.vector
            def _(h):
                run("dve", h)

            @block.gpsimd
            def _(h):
                run("pool", h)

            @block.sync
            def _(h):
                run("sp", h)


def _const_tables(seq):
    half = 64
    inv = (10000.0 ** (-np.arange(half, dtype=np.float32) / np.float32(half))).astype(np.float32)
    pos = np.arange(seq, dtype=np.float32)
    ang = (pos[:, None] * inv[None, :]).astype(np.float32)
    cos = np.cos(ang).astype(np.float32)
    sin = np.sin(ang).astype(np.float32)
    cos4 = np.tile(cos, (1, 4))
    sin4 = np.tile(sin, (1, 4))
    cs = np.concatenate([cos4, sin4], axis=1).astype(np.float32)
    H = 4
    log_g = np.log1p(-np.power(2.0, -5.0 - np.arange(H, dtype=np.float64)))
    idx = np.arange(128, dtype=np.float64)
    s = idx[:, None]
    c = idx[None, :]
    same = (np.floor(s / 64) == np.floor(c / 64))
    lower = (np.floor(s / 64) < np.floor(c / 64))
    scale = 128.0 ** -0.5
    DQ = np.zeros((128, H, 128), np.float64)
    QDT = np.zeros((128, H, 128), np.float64)
    KD = np.zeros((128, H, 128), np.float64)
    g128 = []
    for h in range(H):
        lg = log_g[h]
        Dm = np.where(same, np.exp(lg * np.abs(c - s)), np.where(lower, np.exp(lg * (c - s)), 0.0))
        DQ[:, h, :] = scale * Dm / np.exp(lg * (c + 1.0))
        QDT[:, h, :] = np.exp(lg * (c + 1.0))
        KD[:, h, :] = scale * np.exp(lg * (127.0 - s))
        g128.append(float(np.exp(lg * 128.0)))
    CM = (s <= c).astype(np.float32)
    return dict(cs=cs, DQ=DQ.reshape(128, 512).astype(np.float32),
                QDT=QDT.reshape(128, 512).astype(np.float32),
                KD=KD.reshape(128, 512).astype(np.float32), CM=CM), g128


def build(nsb=SEQ // SBT, depth=DEPTH, parts=('mix', 'ffn')):
    seq = nsb * SBT
    _, G128 = _const_tables(128)
    nc = bass.Bass("TRN2", target_bir_lowering=False)

    def din(name, shape):
        return nc.dram_tensor(name, list(shape), F32, kind="ExternalInput").ap()

    x = din("x", [seq, D])
    mix_g = din("mix_norm_g", [4, D])
    ffn_g = din("ffn_norm_g", [4, D])
    fin_g = din("final_norm_g", [1, D])
    ab_w_in = din("ab_w_in", [2, D, AB_IN])
    ab_w_out = din("ab_w_out", [2, D, D])
    ret_g = din("ret_norm_g", [2, 512])
    sgu_g = din("sgu_ln_g", [2, 512])
    sgu_bb = din("sgu_ln_b", [2, 512])
    sgu_wT = din("sgu_wT", [2, 4, 128, 128])
    sgu_bT = din("sgu_bT", [2, 128, 4])
    c_w_in = din("c_w_in", [2, D, C_IN])
    c_w_out = din("c_w_out", [2, D, D])
    c_bi = din("c_b_i", [2, 4, 1])
    c_bf = din("c_b_f", [2, 4, 1])
    c_ng = din("c_norm_g", [2, 1024])
    w_gate = din("ffn_w_gate", [4, D, DFF])
    w_up = din("ffn_w_up", [4, D, DFF])
    w_down = din("ffn_w_down", [4, DFF, D])
    cs_d = din("cs", [seq, 512])
    DQ_d = din("DQ", [128, 512])
    QDT_d = din("QDT", [128, 512])
    KD_d = din("KD", [128, 512])
    CM_d = din("CM", [128, 128])
    y = nc.dram_tensor("y", [seq, D], F32, kind="ExternalOutput").ap()

    S = Sched()
    with ExitStack() as st:
        def T(name, shape, dt=F32):
            return st.enter_context(nc.sbuf_tensor("sb_" + name, list(shape), dt))

        h = T("h", [128, NBLK, D])
        hnT = T("hnT", [128, 8, SBT], BF16)
        hn = T("hn", [128, D], BF16)
        junk = T("junk", [128, D], BF16)
        slots = [T("slot%d" % i, [128, 4096], BF16) for i in range(NSLOT)]
        stg = T("stg", [128, 14400], BF16)
        actT = stg[:, 0:NFI * SBT].rearrange("p (a b) -> p a b", a=NFI)
        mixed = T("mixed", [128, D], BF16)
        mT = T("mT", [128, 8, 128], BF16)
        Gm = T("Gm", [128, D]); Gf = T("Gf", [128, D]); Gfin = T("Gfin", [128, D])
        NDs = [T("NDs%d" % i, [128, 4, 258]) for i in range(2)]
        low4 = T("low4", [128, 4]); rl4 = T("rl4", [128, 4])
        CS = T("CS", [128, NBLK, 512])
        DQ = T("DQ", [128, 512]); QDT = T("QDT", [128, 512]); KD = T("KD", [128, 512])
        CM = T("CM", [128, 128])
        idf = T("idf", [128, 128]); idb = T("idb", [128, 128], BF16)
        ones4 = T("ones4", [4, 128]); zrow = T("zrow", [4, 128])
        RG = T("RG", [128, 512]); SG = T("SG", [128, 512]); SBB = T("SBB", [128, 512])
        WT = T("WT", [128, 4, 128], BF16); sbias = T("sbias", [128, 4])
        CNG = T("CNG", [128, 1024])
        bi = T("bi", [4, 1]); bfm = T("bfm", [4, 1])
        Sret = [T("Sret%d" % i, [128, 512]) for i in range(2)]
        Sretb = [T("Sretb%d" % i, [128, 512], BF16) for i in range(2)]
        Cst = [T("Cst%d" % i, [128, 4, 258]) for i in range(2)]
        Cb = T("Cb", [128, 4, 258], BF16)
        mst = [T("mst%d" % i, [4, 1]) for i in range(2)]
        ss = T("ss", [128, 1]); sd = T("sd", [128, 1]); rstd = T("rstd", [128, 1])
        tA = T("tA", [128, 256]); tB = T("tB", [128, 256])
        rot = T("rot", [128, 512], BF16)
        gv = T("gv", [128, 512])
        st6 = T("st6", [128, 4, 6]); mv = T("mv", [128, 4, 2])
        sd4 = T("sd4", [128, 4]); rs4 = T("rs4", [128, 4]); nm4 = T("nm4", [128, 4])
        rn = T("rn", [128, 512])
        Pb = T("Pb", [128, 512], BF16)
        sgt = T("sgt", [128, 512])
        li = T("li", [4, SBT]); lfn = T("lfn", [4, SBT]); ex = lfn
        nb = T("nb", [4, 128]); aa = T("aa", [4, 128]); gg = T("gg", [4, 128])
        R3 = T("R3", [4, 3, 128])
        ngl = T("ngl", [4, NBLK]); dec = T("dec", [4, NBLK]); dg = T("dg", [4, NBLK, 4])
        tmpr = T("tmpr", [4, 128])
        cols = T("cols", [128, NBLK, 16])
        low = T("low", [128, 1]); rl = T("rl", [128, 1])
        hs = T("hs", [128, 256]); hy = T("hy", [128, 1024])

        banks = [st.enter_context(nc.psum_tensor("bank%d" % i, [128, 512], F32)) for i in range(8)]
        bank_ctr = [0]

        uniq = [0]

        def _bkey(i, cls):
            if cls in MODEL_FREE_BANKS:
                uniq[0] += 1
                return "bankfree%d" % uniq[0]
            return "bank%d" % i

        def newbank(cls="x"):
            i = bank_ctr[0] % 8
            bank_ctr[0] += 1
            return banks[i], _bkey(i, cls)

        def stg_view(off, shape):
            n = int(np.prod(shape))
            ap = stg[:, off:off + n]
            if len(shape) == 2:
                return ap.rearrange("p (a b) -> p a b", a=shape[0])
            if len(shape) == 3:
                return ap.rearrange("p (a b c) -> p a b c", a=shape[0], b=shape[1])
            return ap

        qdT = stg_view(0, (NBLK, 4, 128)); kT = stg_view(2048, (NBLK, 4, 128))
        kd = stg_view(4096, (NBLK, 512)); vb = stg_view(6144, (NBLK, 512))
        sgl = stg_view(8192, (NBLK, 512)); ug = stg_view(10240, (NBLK, 512))
        vsn = stg_view(12288, (NBLK, 512))
        qrT = qdT; kdT = kT; kdc = kd
        vext = stg_view(6144, (NBLK, 4, 258))
        so = stg_view(10272, (NBLK, 1024))
        assert 10272 + 4096 <= 14400

        slot_ctr = [0]

        def load_tile(dmas):
            i = slot_ctr[0] % NSLOT
            slot_ctr[0] += 1
            sl = slots[i]
            key = "slot%d" % i
            for (lo, a, b, src) in dmas:
                dst = sl[:, lo:lo + a * b].rearrange("p (a b) -> p a b", a=a)
                S.dma("pool", (lambda dst, src: lambda e: e.dma_start(out=dst, in_=src))(dst, src), write=key)
            return sl, key

        def wcols(w2d, c0, ncol):
            return w2d.rearrange("(kc p) n -> p kc n", p=128)[:, :, c0:c0 + ncol]

        sp_dma = lambda dst, src, key: S.dma("sp", (lambda e: e.dma_start(out=dst, in_=src)), write=key)
        sp_dma(DQ[:], DQ_d[:], "DQ"); sp_dma(QDT[:], QDT_d[:], "QDT"); sp_dma(KD[:], KD_d[:], "KD")
        sp_dma(CM[:], CM_d[:], "CM")
        S.op("pool", lambda e: e.memset(idf[:], 0.0), writes=["idf"])
        S.op("pool", lambda e: e.affine_select(out=idf[:], in_=idf[:], compare_op=ALU.not_equal, fill=1.0,
                                               base=0, pattern=[[-1, 128]], channel_multiplier=1),
             reads=["idf"], writes=["idf"])
        S.op("pool", lambda e: e.tensor_copy(idb[:], idf[:]), reads=["idf"], writes=["idb"])
        S.op("pool", lambda e: e.memset(ones4[:], 1.0), writes=["ones4"])
        S.op("pool", lambda e: e.memset(zrow[:], 0.0), writes=["zrow"])
        for i in range(2):
            S.op("pool", (lambda i: lambda e: e.memset(Sret[i][:], 0.0))(i), writes=["Sret%d" % i])
            S.op("pool", (lambda i: lambda e: e.memset(Sretb[i][:], 0.0))(i), writes=["Sretb%d" % i])
            S.op("pool", (lambda i: lambda e: e.memset(Cst[i][:], 0.0))(i), writes=["Cst%d" % i])
            S.op("pool", (lambda i: lambda e: e.memset(mst[i][:], 0.0))(i), writes=["mst%d" % i])

        def tcopy(tag, out_ap, in_ap, reads, writes):
            n = 1
            for d_ in tuple(out_ap.shape)[1:]:
                n *= int(d_)
            if tag in COPY_ON_DVE:
                S.op("dve", lambda e: e.tensor_copy(out_ap, in_ap), reads=reads, writes=writes, dur=0.1 + n * 0.6e-3)
            else:
                S.op("act", lambda e: e.activation(out=out_ap, in_=in_ap, func=AF.Copy), reads=reads, writes=writes)

        def B(i, cls="x"):
            return banks[i], _bkey(i, cls)

        def norm_block(j, Gt, gkey, bank=None):
            S.cur_tag = S.cur_tag.split("/")[0] + "/norm"
            hk = "h%d" % j
            S.op("act", lambda e: e.activation(out=junk[:], in_=h[:, j, :], func=AF.Square, accum_out=ss[:]),
                 reads=[hk], writes=["junk", "ss"])
            S.op("act", lambda e: e.activation(out=sd[:], in_=ss[:], func=AF.Sqrt, bias=EPS, scale=1.0 / D),
                 reads=["ss"], writes=["sd"])
            S.op("dve", lambda e: e.reciprocal(rstd[:], sd[:]), reads=["sd"], writes=["rstd"])
            S.op("dve", lambda e: e.scalar_tensor_tensor(out=hn[:], in0=h[:, j, :], scalar=rstd[:, 0:1],
                                                         in1=Gt[:], op0=ALU.mult, op1=ALU.mult),
                 reads=[hk, "rstd", gkey], writes=["hn"])
            bk, bkey = bank if bank is not None else newbank('norm')
            bb = bk[:].bitcast(BF16)
            for kc in range(8):
                S.op("pe", lambda e: e.transpose(bb[:, kc * 128:(kc + 1) * 128], hn[:, kc * 128:(kc + 1) * 128], idb[:]),
                     reads=["hn", "idb"], writes=[bkey], safe_same=[bkey])
            tcopy("hnT", hnT[:, :, j * 128:(j + 1) * 128], bb[:, 0:1024].rearrange("p (a b) -> p a b", a=8),
                  [bkey], ["hnT%d" % j])

        def final_block(j, r0):
            hk = "h%d" % j
            hyk = ["hy_%d" % q for q in range(4)]
            S.op("act", lambda e: e.activation(out=junk[:], in_=h[:, j, :], func=AF.Square, accum_out=ss[:]),
                 reads=[hk], writes=["junk", "ss"])
            S.op("act", lambda e: e.activation(out=sd[:], in_=ss[:], func=AF.Sqrt, bias=EPS, scale=1.0 / D),
                 reads=["ss"], writes=["sd"])
            S.op("dve", lambda e: e.reciprocal(rstd[:], sd[:]), reads=["sd"], writes=["rstd"])
            S.op("dve", lambda e: e.scalar_tensor_tensor(out=hy[:], in0=h[:, j, :], scalar=rstd[:, 0:1],
                                                         in1=Gfin[:], op0=ALU.mult, op1=ALU.mult),
                 reads=[hk, "rstd", "Gfin"], writes=hyk)
            S.dma("sp", lambda e: e.dma_start(out=y[r0 + j * 128:r0 + (j + 1) * 128, :], in_=hy[:]),
                  reads=hyk, write="y", is_output=True)

        def proj_block(j, sl, skey, ncol=512, coff=0, stride=None):
            stride = stride or ncol
            bk, bkey = newbank('proj')
            wv = sl[:, 0:8 * stride].rearrange("p (a b) -> p a b", a=8)
            for kc in range(8):
                S.op("pe", (lambda kc: lambda e: e.matmul(bk[:, 0:ncol], hnT[:, kc, j * 128:(j + 1) * 128],
                                                          wv[:, kc, coff:coff + ncol],
                                                          start=(kc == 0), stop=(kc == 7)))(kc),
                     reads=["hnT%d" % j, skey], writes=[bkey], safe_same=[bkey])
            return bk, bkey

        def rope_block(j, bk, bkey, outT):
            pv = bk[:, 0:512].rearrange("p (h t i) -> p h t i", h=4, t=2)
            t1 = pv[:, :, 0, :]
            t2 = pv[:, :, 1, :]
            cos = CS[:, j, 0:256].rearrange("p (h i) -> p h i", h=4)
            sin = CS[:, j, 256:512].rearrange("p (h i) -> p h i", h=4)
            rv = rot[:, :].rearrange("p (h t i) -> p h t i", h=4, t=2)
            A = tA[:, :].rearrange("p (h i) -> p h i", h=4)
            B = tB[:, :].rearrange("p (h i) -> p h i", h=4)
            ck = "CS"
            S.op("dve", lambda e: e.tensor_tensor(out=A, in0=t1, in1=cos, op=ALU.mult), reads=[bkey, ck], writes=["tA"])
            S.op("dve", lambda e: e.tensor_tensor(out=B, in0=t2, in1=sin, op=ALU.mult), reads=[bkey, ck], writes=["tB"])
            S.op(_pe("rope2"), lambda e: e.tensor_tensor(out=rv[:, :, 0, :], in0=A, in1=B, op=ALU.subtract),
                 reads=["tA", "tB"], writes=["rot"])
            S.op("dve", lambda e: e.tensor_tensor(out=A, in0=t1, in1=sin, op=ALU.mult), reads=[bkey, ck], writes=["tA"])
            S.op("dve", lambda e: e.tensor_tensor(out=B, in0=t2, in1=cos, op=ALU.mult), reads=[bkey, ck], writes=["tB"])
            S.op(_pe("rope2"), lambda e: e.tensor_tensor(out=rv[:, :, 1, :], in0=A, in1=B, op=ALU.add),
                 reads=["tA", "tB"], writes=["rot"])

        def transpose4(src_ap_fn, src_key):
            bk, bkey = newbank('tr4')
            bb = bk[:].bitcast(BF16)
            for hh in range(4):
                S.op("pe", (lambda hh: lambda e: e.transpose(bb[:, hh * 128:(hh + 1) * 128], src_ap_fn(hh), idb[:]))(hh),
                     reads=[src_key, "idb"], writes=[bkey], safe_same=[bkey])
            return bb, bkey

        def out_proj_block(j, tiles, tb, pbs):
            bk, bkey = tb
            bb = bk[:].bitcast(BF16)
            for kc in range(8):
                S.op("pe", lambda e: e.transpose(bb[:, kc * 128:(kc + 1) * 128], mixed[:, kc * 128:(kc + 1) * 128], idb[:]),
                     reads=["mixed", "idb"], writes=[bkey], safe_same=[bkey])
            tcopy("mT", mT[:, :, :], bb[:, 0:1024].rearrange("p (a b) -> p a b", a=8), [bkey], ["mT"])
            for half in range(2):
                sl, skey = tiles[half]
                wv = sl[:, 0:4096].rearrange("p (a b) -> p a b", a=8)
                pk, pkey = pbs[half]
                for kc in range(8):
                    S.op("pe", lambda e: e.matmul(pk[:, :], mT[:, kc, :], wv[:, kc, :], start=(kc == 0), stop=(kc == 7)),
                         reads=["mT", skey], writes=[pkey], safe_same=[pkey])
                S.op("dve", lambda e: e.tensor_tensor(out=h[:, j, half * 512:(half + 1) * 512], in0=pk[:, :],
                                                      in1=h[:, j, half * 512:(half + 1) * 512], op=ALU.add),
                     reads=[pkey, "h%d" % j], writes=["h%d" % j])

        def ln_stats(src_aps, skeys, n):
            for g_ in range(n):
                S.op("dve", (lambda g_: lambda e: e.bn_stats(st6[:, g_, :], src_aps[g_]))(g_),
                     reads=[skeys[g_]], writes=["st6_%d" % g_])
                S.op("dve", (lambda g_: lambda e: e.bn_aggr(mv[:, g_, :], st6[:, g_, :]))(g_),
                     reads=["st6_%d" % g_], writes=["mv_%d" % g_])
            mvk = ["mv_%d" % g_ for g_ in range(n)]
            S.op("act", lambda e: e.activation(out=sd4[:, 0:n], in_=mv[:, 0:n, 1], func=AF.Sqrt, bias=EPS, scale=1.0),
                 reads=mvk, writes=["sd4"])
            S.op("dve", lambda e: e.reciprocal(rs4[:, 0:n], sd4[:, 0:n]), reads=["sd4"], writes=["rs4"])
            S.op("dve", lambda e: e.scalar_tensor_tensor(out=nm4[:, 0:n], in0=mv[:, 0:n, 0], scalar=-1.0,
                                                         in1=rs4[:, 0:n], op0=ALU.mult, op1=ALU.mult),
                 reads=mvk + ["rs4"], writes=["nm4"])

        def even_mixer(l):
            S.cur_tag = "L%d/A" % l
            jl = l // 2
            w_in = ab_w_in[jl]
            sp_dma(Gm[:], mix_g[l:l + 1, :].partition_broadcast(128), "Gm")
            sp_dma(RG[:], ret_g[jl:jl + 1, :].partition_broadcast(128), "RG")
            sp_dma(SG[:], sgu_g[jl:jl + 1, :].partition_broadcast(128), "SG")
            sp_dma(SBB[:], sgu_bb[jl:jl + 1, :].partition_broadcast(128), "SBB")
            sp_dma(sbias[:], sgu_bT[jl], "sbias")
            S.dma("pool", lambda e: e.dma_start(out=WT[:], in_=sgu_wT[jl].rearrange("g q p -> q g p")), write="WT")
            S.op("pool", lambda e: e.memset(WT[64:128, :, 0:64], 0.0), reads=["WT"], writes=["WT"])
            sp_dma(Gf[:], ffn_g[l:l + 1, :].partition_broadcast(128), "Gf")
            for ct in range(6):
                sl, skey = load_tile([(0, 8, 512, wcols(w_in, ct * 512, 512))])
                for j in range(NBLK):
                    bk, bkey = proj_block(j, sl, skey)
                    if ct == 0:
                        rope_block(j, bk, bkey, None)
                        bb, tkey = transpose4(lambda hh: rot[:, hh * 128:(hh + 1) * 128], "rot")
                        S.op("dve", (lambda j, bb: lambda e: e.tensor_tensor(
                            out=qdT[:, j, :, :], in0=bb[:, 0:512].rearrange("p (a b) -> p a b", a=4),
                            in1=QDT[:, :].rearrange("p (a b) -> p a b", a=4), op=ALU.mult))(j, bb),
                             reads=[tkey, "QDT"], writes=["qdT%d" % j])
                    elif ct == 1:
                        rope_block(j, bk, bkey, None)
                        S.op(_pe("kd"), (lambda j: lambda e: e.tensor_tensor(out=kd[:, j, :], in0=rot[:, :], in1=KD[:, :],
                                                                         op=ALU.mult))(j),
                             reads=["rot", "KD"], writes=["kd%d" % j])
                        bb, tkey = transpose4(lambda hh: rot[:, hh * 128:(hh + 1) * 128], "rot")
                        tcopy("kT", kT[:, j, :, :], bb[:, 0:512].rearrange("p (a b) -> p a b", a=4), [tkey], ["kT%d" % j])
                    elif ct == 2:
                        S.op("act", (lambda j, bk: lambda e: e.activation(out=vb[:, j, :], in_=bk[:, :], func=AF.Copy))(j, bk),
                             reads=[bkey], writes=["vb%d" % j])
                    elif ct == 3:
                        S.op("act", (lambda j, bk: lambda e: e.activation(out=sgl[:, j, :], in_=bk[:, :], func=AF.Silu))(j, bk),
                             reads=[bkey], writes=["sgl%d" % j])
                    elif ct == 4:
                        S.op("act", (lambda j, bk: lambda e: e.activation(out=ug[:, j, :], in_=bk[:, :],
                                                                          func=AF.Gelu_apprx_tanh))(j, bk),
                             reads=[bkey], writes=["ug%d" % j])
                    else:
                        S.op("act", (lambda bk: lambda e: e.activation(out=gv[:, :], in_=bk[:, :],
                                                                       func=AF.Gelu_apprx_tanh))(bk),
                             reads=[bkey], writes=["gv"])
                        ln_stats([gv[:, :]], ["gv"], 1)
                        S.op("act", lambda e: e.activation(out=gv[:, :], in_=gv[:, :], func=AF.Identity,
                                                           scale=rs4[:, 0:1], bias=nm4[:, 0:1]),
                             reads=["gv", "rs4", "nm4"], writes=["gv"])
                        S.op(_pe("vs"), lambda e: e.tensor_tensor(out=gv[:, :], in0=gv[:, :], in1=SG[:, :], op=ALU.mult),
                             reads=["gv", "SG"], writes=["gv"])
                        S.op(_pe("vs"), (lambda j: lambda e: e.tensor_tensor(out=vsn[:, j, :], in0=gv[:, :], in1=SBB[:, :],
                                                                         op=ALU.add))(j),
                             reads=["gv", "SBB"], writes=["vsn%d" % j])
            otiles = [load_tile([(0, 8, 512, wcols(ab_w_out[jl], half * 512, 512))]) for half in range(2)]
            Sr, Srb = Sret[jl], Sretb[jl]
            sk, sbk = "Sret%d" % jl, "Sretb%d" % jl

            def S1(j):
                S.cur_tag = "L%d/S1" % l
                pk, pkey = B(0, 'eS1')
                for hh in range(4):
                    S.op("pe", lambda e: e.matmul(pk[:, hh * 128:(hh + 1) * 128], kT[:, j, hh, :], qdT[:, j, hh, :],
                                                  start=True, stop=True),
                         reads=["kT%d" % j, "qdT%d" % j], writes=[pkey], safe_same=[pkey])
                S.op("dve", lambda e: e.tensor_tensor(out=Pb[:, :], in0=pk[:, :], in1=DQ[:, :], op=ALU.mult),
                     reads=[pkey, "DQ"], writes=["Pb"])
                ok_, okey = B(1 + (j % 2), 'eOK')
                for hh in range(4):
                    S.op("pe", lambda e: e.matmul(ok_[:, hh * 128:(hh + 1) * 128], Pb[:, hh * 128:(hh + 1) * 128],
                                                  vb[:, j, hh * 128:(hh + 1) * 128], start=True, stop=False),
                         reads=["Pb", "vb%d" % j], writes=[okey], safe_same=[okey])
                    S.op("pe", lambda e: e.matmul(ok_[:, hh * 128:(hh + 1) * 128], qdT[:, j, hh, :],
                                                  Srb[:, hh * 128:(hh + 1) * 128], start=False, stop=True),
                         reads=["qdT%d" % j, sbk], writes=[okey], safe_same=[okey])
                kk, kkey = B(0, 'eS1')
                for hh in range(4):
                    S.op("pe", lambda e: e.matmul(kk[:, hh * 128:(hh + 1) * 128], kd[:, j, hh * 128:(hh + 1) * 128],
                                                  vb[:, j, hh * 128:(hh + 1) * 128], start=True, stop=True),
                         reads=["kd%d" % j, "vb%d" % j], writes=[kkey], safe_same=[kkey])
                for hh in range(4):
                    S.op("dve", lambda e: e.scalar_tensor_tensor(
                        out=Sr[:, hh * 128:(hh + 1) * 128], in0=Sr[:, hh * 128:(hh + 1) * 128], scalar=G128[hh],
                        in1=kk[:, hh * 128:(hh + 1) * 128], op0=ALU.mult, op1=ALU.add),
                         reads=[kkey, sk + "_%d" % hh], writes=[sk + "_%d" % hh])
                S.op("act", lambda e: e.activation(out=Srb[:, :], in_=Sr[:, :], func=AF.Copy),
                     reads=[sk + "_%d" % hh for hh in range(4)], writes=[sbk])

            def S2(j):
                S.cur_tag = "L%d/S2" % l
                ok_, okey = B(1 + (j % 2), 'eOK')
                rnk = ["rn_%d" % hh for hh in range(4)]
                ln_stats([ok_[:, hh * 128:(hh + 1) * 128] for hh in range(4)], [okey] * 4, 4)
                for hh in range(4):
                    S.op("act", lambda e: e.activation(
                        out=rn[:, hh * 128:(hh + 1) * 128], in_=ok_[:, hh * 128:(hh + 1) * 128], func=AF.Identity,
                        scale=rs4[:, hh:hh + 1], bias=nm4[:, hh:hh + 1]),
                         reads=[okey, "rs4", "nm4"], writes=["rn_%d" % hh])
                S.op(_pe("rn"), lambda e: e.tensor_tensor(out=rn[:, :], in0=rn[:, :], in1=RG[:, :], op=ALU.mult),
                     reads=rnk + ["RG"], writes=rnk)
                S.op(_pe("rn2"), lambda e: e.tensor_tensor(out=mixed[:, 0:512], in0=rn[:, :], in1=sgl[:, j, :], op=ALU.mult),
                     reads=rnk + ["sgl%d" % j], writes=["mixed"])
                gk, gkey = B(4, 'eS2')
                for g_ in range(4):
                    S.op("pe", lambda e: e.matmul(gk[:, g_ * 128:(g_ + 1) * 128], WT[:, g_, :],
                                                  vsn[:, j, g_ * 128:(g_ + 1) * 128], start=True, stop=True),
                         reads=["WT", "vsn%d" % j], writes=[gkey], safe_same=[gkey])
                for g_ in range(4):
                    S.op("dve", lambda e: e.scalar_tensor_tensor(
                        out=mixed[:, 512 + g_ * 128:512 + (g_ + 1) * 128], in0=gk[:, g_ * 128:(g_ + 1) * 128],
                        scalar=sbias[:, g_:g_ + 1], in1=ug[:, j, g_ * 128:(g_ + 1) * 128],
                        op0=ALU.add, op1=ALU.mult),
                         reads=[gkey, "sbias", "ug%d" % j], writes=["mixed"])
                out_proj_block(j, otiles, B(5, 'op'), [B(6, 'op'), B(7, 'op')])
                norm_block(j, Gf, "Gf", B(3, 'op'))

            S1(0)
            for j in range(NBLK):
                if j + 1 < NBLK:
                    S1(j + 1)
                S2(j)

        def odd_mixer(l):
            S.cur_tag = "L%d/A" % l
            jl = l // 2
            w_in = c_w_in[jl]
            C = Cst[jl]
            ck = "Cst%d" % jl
            msk = "mst%d" % jl
            ms = mst[jl]
            sp_dma(Gm[:], mix_g[l:l + 1, :].partition_broadcast(128), "Gm")
            sp_dma(CNG[:], c_ng[jl:jl + 1, :].partition_broadcast(128), "CNG")
            sp_dma(bi[:], c_bi[jl], "bi")
            sp_dma(bfm[:], c_bf[jl], "bfm")
            S.op("dve", lambda e: e.tensor_scalar(bfm[:], bfm[:], -1.0, None, ALU.mult), reads=["bfm"], writes=["bfm"])
            sp_dma(Gf[:], ffn_g[l:l + 1, :].partition_broadcast(128), "Gf")
            sl, skey = load_tile([(0, 8, 8, wcols(w_in, 3072, 8))])
            wv = sl[:, 0:64].rearrange("p (a b) -> p a b", a=8)
            pi, pikey = newbank('gate')
            pf, pfkey = newbank('gate')
            for kc in range(8):
                S.op("pe", (lambda kc: lambda e: e.matmul(pi[0:4, :], wv[:, kc, 0:4], hnT[:, kc, :],
                                                          start=(kc == 0), stop=(kc == 7)))(kc),
                     reads=["hnT%d" % j for j in range(NBLK)] + [skey], writes=[pikey], safe_same=[pikey])
            for kc in range(8):
                S.op("pe", (lambda kc: lambda e: e.matmul(pf[0:4, :], wv[:, kc, 4:8], hnT[:, kc, :],
                                                          start=(kc == 0), stop=(kc == 7)))(kc),
                     reads=["hnT%d" % j for j in range(NBLK)] + [skey], writes=[pfkey], safe_same=[pfkey])
            S.op("act", lambda e: e.activation(out=li[:, :], in_=pi[0:4, :], func=AF.Identity, bias=bi[:, 0:1], scale=1.0),
                 reads=[pikey, "bi"], writes=["li"])
            S.op("act", lambda e: e.activation(out=ex[:, :], in_=pf[0:4, :], func=AF.Exp, bias=bfm[:, 0:1], scale=-1.0),
                 reads=[pfkey, "bfm"], writes=["lfn"])
            S.op("act", lambda e: e.activation(out=lfn[:, :], in_=ex[:, :], func=AF.Ln, bias=1.0, scale=1.0),
                 reads=["lfn"], writes=["lfn"])
            for j in range(NBLK):
                cs_ = slice(j * 128, (j + 1) * 128)
                S.op("dve", (lambda cs_: lambda e: e.tensor_tensor_scan(nb[:, :], lfn[:, cs_], zrow[:, :], 0.0,
                                                                        ALU.add, ALU.add))(cs_),
                     reads=["lfn", "zrow"], writes=["nb"])
                S.op("dve", (lambda cs_: lambda e: e.tensor_tensor(out=aa[:, :], in0=li[:, cs_], in1=nb[:, :],
                                                                   op=ALU.add))(cs_),
                     reads=["li", "nb"], writes=["aa"])
                S.op("dve", lambda e: e.tensor_tensor_scan(gg[:, :], aa[:, :], aa[:, :], ms[:, 0:1], ALU.max, ALU.max),
                     reads=["aa", msk], writes=["gg"])
                S.op("dve", (lambda j: lambda e: e.tensor_scalar(ngl[:, j:j + 1], gg[:, 127:128], -1.0, None, ALU.mult))(j),
                     reads=["gg"], writes=["ngl%d" % j])
                S.op("act", (lambda j: lambda e: e.activation(out=dec[:, j:j + 1], in_=ms[:, 0:1], func=AF.Exp,
                                                              bias=ngl[:, j:j + 1], scale=1.0))(j),
                     reads=[msk, "ngl%d" % j], writes=["dec%d" % j])
                S.op("act", (lambda j: lambda e: e.activation(out=R3[:, 0, :], in_=aa[:, :], func=AF.Exp,
                                                              bias=ngl[:, j:j + 1], scale=1.0))(j),
                     reads=["aa", "ngl%d" % j], writes=["R3_0"])
                S.op("act", lambda e: e.activation(out=R3[:, 1, :], in_=gg[:, :], func=AF.Exp,
                                                   bias=gg[:, 127:128], scale=-1.0),
                     reads=["gg"], writes=["R3_1"])
                S.op("dve", lambda e: e.tensor_tensor(out=tmpr[:, :], in0=nb[:, :], in1=gg[:, :], op=ALU.subtract),
                     reads=["nb", "gg"], writes=["tmpr"])
                S.op("act", lambda e: e.activation(out=R3[:, 2, :], in_=tmpr[:, :], func=AF.Exp),
                     reads=["tmpr"], writes=["R3_2"])
                S.op("dve", lambda e: e.tensor_tensor(out=ms[:, 0:1], in0=gg[:, 127:128], in1=nb[:, 127:128],
                                                      op=ALU.subtract),
                     reads=["gg", "nb"], writes=[msk])
                S.op("dve", (lambda j: lambda e: e.tensor_scalar(dg[:, j, :], idf[0:4, 0:4], dec[:, j:j + 1], None, ALU.mult))(j),
                     reads=["idf", "dec%d" % j], writes=["dg%d" % j])
                ck_, ckey = newbank('gate')
                for q_ in range(3):
                    S.op("pe", (lambda q_, ck_: lambda e: e.matmul(ck_[:, q_ * 4:(q_ + 1) * 4], R3[:, q_, :],
                                                                  idf[0:4, 0:4], start=True, stop=True))(q_, ck_),
                         reads=["R3_%d" % q_, "idf"], writes=[ckey], safe_same=[ckey])
                S.op("pe", (lambda j, ck_: lambda e: e.matmul(ck_[:, 12:16], ones4[:, :], dg[:, j, :],
                                                              start=True, stop=True))(j, ck_),
                     reads=["ones4", "dg%d" % j], writes=[ckey], safe_same=[ckey])
                S.op("dve", (lambda j, ck_: lambda e: e.tensor_copy(cols[:, j, :], ck_[:, 0:16]))(j, ck_),
                     reads=[ckey], writes=["cols%d" % j])
                S.op("dve", (lambda j: lambda e: e.tensor_scalar(cols[:, j, 0:4], cols[:, j, 0:4], 128.0 ** -0.5, None,
                                                                 ALU.mult))(j),
                     reads=["cols%d" % j], writes=["cols%d" % j])
            sl, skey = load_tile([(0, 8, 512, wcols(w_in, 0, 512))])
            for j in range(NBLK):
                bk, bkey = proj_block(j, sl, skey)
                for hh in range(4):
                    S.op("act", (lambda hh, bk, j: lambda e: e.activation(
                        out=rot[:, hh * 128:(hh + 1) * 128], in_=bk[:, hh * 128:(hh + 1) * 128], func=AF.Copy,
                        scale=cols[:, j, 4 + hh:5 + hh]))(hh, bk, j),
                         reads=[bkey, "cols%d" % j], writes=["rot"])
                bb, tkey = transpose4(lambda hh: rot[:, hh * 128:(hh + 1) * 128], "rot")
                tcopy("qrT", qrT[:, j, :, :], bb[:, 0:512].rearrange("p (a b) -> p a b", a=4), [tkey], ["qdT%d" % j])
            sl, skey = load_tile([(0, 8, 512, wcols(w_in, 512, 512))])
            for j in range(NBLK):
                bk, bkey = proj_block(j, sl, skey)
                for hh in range(4):
                    S.op("act", (lambda hh, bk, j: lambda e: e.activation(
                        out=kdc[:, j, hh * 128:(hh + 1) * 128], in_=bk[:, hh * 128:(hh + 1) * 128], func=AF.Copy,
                        scale=cols[:, j, hh:hh + 1]))(hh, bk, j),
                         reads=[bkey, "cols%d" % j], writes=["kd%d" % j])
                bb, tkey = transpose4((lambda j: lambda hh: kdc[:, j, hh * 128:(hh + 1) * 128])(j), "kd%d" % j)
                tcopy("kdT", kdT[:, j, :, :], bb[:, 0:512].rearrange("p (a b) -> p a b", a=4), [tkey], ["kT%d" % j])
            for vt in range(2):
                sl, skey = load_tile([(0, 8, 512, wcols(w_in, 1024 + vt * 512, 512))])
                for j in range(NBLK):
                    bk, bkey = proj_block(j, sl, skey)
                    S.op("act", (lambda j, bk, vt: lambda e: e.activation(
                        out=vext[:, j, 2 * vt:2 * vt + 2, 0:256], in_=bk[:, :].rearrange("p (a b) -> p a b", a=2),
                        func=AF.Copy))(j, bk, vt),
                         reads=[bkey], writes=["vb%d" % j])
            for j in range(NBLK):
                S.op(_pe("vmem"), (lambda j: lambda e: e.memset(vext[:, j, :, 256:257], 1.0))(j), reads=[], writes=["vb%d" % j])
            for ot in range(2):
                sl, skey = load_tile([(0, 8, 512, wcols(w_in, 2048 + ot * 512, 512))])
                for j in range(NBLK):
                    bk, bkey = proj_block(j, sl, skey)
                    S.op("act", (lambda j, bk, ot: lambda e: e.activation(
                        out=so[:, j, ot * 512:(ot + 1) * 512], in_=bk[:, :], func=AF.Sigmoid))(j, bk, ot),
                         reads=[bkey], writes=["so%d" % j])
            otiles = [load_tile([(0, 8, 512, wcols(c_w_out[jl], half * 512, 512))]) for half in range(2)]
            def S1(j):
                S.cur_tag = "L%d/S1" % l
                nd = NDs[j % 2]
                for hh in range(4):
                    chk = ck + "_%d" % hh
                    S.op("dve", lambda e: e.tensor_scalar(Cb[:, hh, 0:257], C[:, hh, 0:257],
                                                          cols[:, j, 12 + hh:13 + hh], None, ALU.mult),
                         reads=[chk, "cols%d" % j], writes=["Cb_%d" % hh])
                    pk, pkey = B(hh % 2, 'oS1')
                    S.op("pe", lambda e: e.matmul(pk[:, 0:128], kdT[:, j, hh, :], qrT[:, j, hh, :], start=True, stop=True),
                         reads=["kT%d" % j, "qdT%d" % j], writes=[pkey])
                    S.op("dve", lambda e: e.tensor_tensor(out=Pb[:, hh * 128:(hh + 1) * 128], in0=pk[:, 0:128],
                                                          in1=CM[:, :], op=ALU.mult),
                         reads=[pkey, "CM"], writes=["Pb_%d" % hh])
                    nk, nkey = B(2 + hh % 2, 'oS1')
                    S.op("pe", lambda e: e.matmul(nk[:, 0:257], Pb[:, hh * 128:(hh + 1) * 128],
                                                  vext[:, j, hh, 0:257], start=True, stop=False),
                         reads=["Pb_%d" % hh, "vb%d" % j], writes=[nkey])
                    S.op("pe", lambda e: e.matmul(nk[:, 0:257], qrT[:, j, hh, :], Cb[:, hh, 0:257], start=False, stop=True),
                         reads=["qdT%d" % j, "Cb_%d" % hh], writes=[nkey], safe_same=[nkey])
                    kk, kkey = B(hh % 2, 'oS1')
                    S.op("pe", lambda e: e.matmul(kk[:, 128:385], kdc[:, j, hh * 128:(hh + 1) * 128],
                                                  vext[:, j, hh, 0:257], start=True, stop=True),
                         reads=["kd%d" % j, "vb%d" % j], writes=[kkey])
                    S.op("dve", lambda e: e.scalar_tensor_tensor(
                        out=C[:, hh, 0:257], in0=C[:, hh, 0:257], scalar=cols[:, j, 12 + hh:13 + hh], in1=kk[:, 128:385],
                        op0=ALU.mult, op1=ALU.add),
                         reads=[kkey, chk, "cols%d" % j], writes=[chk])
                    S.op("act", lambda e: e.activation(out=nd[:, hh, 0:257], in_=nk[:, 0:257], func=AF.Copy),
                         reads=[nkey], writes=["NDs%d_%d" % (j % 2, hh)])

            def S2(j):
                S.cur_tag = "L%d/S2" % l
                nd = NDs[j % 2]
                ndk = ["NDs%d_%d" % (j % 2, hh) for hh in range(4)]
                hyk = ["hy_%d" % hh for hh in range(4)]
                S.op("act", lambda e: e.activation(out=low4[:, :], in_=nd[:, :, 256], func=AF.Abs),
                     reads=ndk, writes=["low4"])
                S.op("dve", lambda e: e.tensor_tensor(out=low4[:, :], in0=low4[:, :], in1=cols[:, j, 8:12], op=ALU.max),
                     reads=["low4", "cols%d" % j], writes=["low4"])
                S.op("dve", lambda e: e.reciprocal(rl4[:, :], low4[:, :]), reads=["low4"], writes=["rl4"])
                for hh in range(4):
                    S.op("act", lambda e: e.activation(out=nd[:, hh, 0:256], in_=nd[:, hh, 0:256], func=AF.Copy,
                                                       scale=rl4[:, hh:hh + 1]),
                         reads=[ndk[hh], "rl4"], writes=[ndk[hh]])
                ln_stats([nd[:, hh, 0:256] for hh in range(4)], ndk, 4)
                for hh in range(4):
                    S.op("act", lambda e: e.activation(out=hy[:, hh * 256:(hh + 1) * 256], in_=nd[:, hh, 0:256],
                                                       func=AF.Identity, scale=rs4[:, hh:hh + 1], bias=nm4[:, hh:hh + 1]),
                         reads=[ndk[hh], "rs4", "nm4"], writes=["hy_%d" % hh])
                S.op(_pe("hy"), lambda e: e.tensor_tensor(out=hy[:, :], in0=hy[:, :], in1=CNG[:, :], op=ALU.mult),
                     reads=hyk + ["CNG"], writes=hyk)
                S.op(_pe("hy2"), lambda e: e.tensor_tensor(out=mixed[:, :], in0=hy[:, :], in1=so[:, j, :], op=ALU.mult),
                     reads=hyk + ["so%d" % j], writes=["mixed"])
                out_proj_block(j, otiles, B(4, 'op'), [B(6, 'op'), B(7, 'op')])
                norm_block(j, Gf, "Gf", B(5, 'op'))

            S1(0)
            for j in range(NBLK):
                if j + 1 < NBLK:
                    S1(j + 1)
                S2(j)

        def ffn(l, last, r0):
            S.cur_tag = "L%d/F1" % l
            if not last:
                sp_dma(Gm[:], mix_g[l + 1:l + 2, :].partition_broadcast(128), "Gm")
            hk = ["hnT%d" % j for j in range(NBLK)]
            for ft in range(NFI // 2):
                sl, skey = load_tile([(0, 8, 256, wcols(w_gate[l], ft * 256, 256)),
                                      (2048, 8, 256, wcols(w_up[l], ft * 256, 256))])
                gvw = sl[:, 0:2048].rearrange("p (a b) -> p a b", a=8)
                uvw = sl[:, 2048:4096].rearrange("p (a b) -> p a b", a=8)
                for sub in range(2):
                    fi = ft * 2 + sub
                    pg, pgk = newbank('ffn1')
                    pu, puk = newbank('ffn1')
                    nhb = 2 if ft < KSPLIT else 1
                    wtok = SBT // nhb
                    for (pp, pkk, wvv) in ((pg, pgk, gvw), (pu, puk, uvw)):
                        for hb in range(nhb):
                            rk = ["hnT%d" % q for q in range(hb * NBLK // nhb, (hb + 1) * NBLK // nhb)]
                            for kc in range(8):
                                S.op("pe", lambda e: e.matmul(pp[:, hb * wtok:(hb + 1) * wtok],
                                                              wvv[:, kc, sub * 128:(sub + 1) * 128],
                                                              hnT[:, kc, hb * wtok:(hb + 1) * wtok],
                                                              start=(kc == 0), stop=(kc == 7)),
                                     reads=rk + [skey], writes=[pkk], safe_same=[pkk])
                    S.op("act", (lambda pg: lambda e: e.activation(out=sgt[:, :], in_=pg[:, :], func=AF.Silu))(pg),
                         reads=[pgk], writes=["sgt"])
                    S.op("dve", (lambda pu, fi: lambda e: e.tensor_tensor(out=actT[:, fi, :], in0=sgt[:, :], in1=pu[:, :],
                                                                          op=ALU.mult))(pu, fi),
                         reads=["sgt", puk], writes=["actT%d" % fi])
            S.cur_tag = "L%d/F2" % l
            accs = [[newbank('ffn2') for half in range(2)] for j in range(NBLK)]
            wdv = w_down[l].rearrange("(fi p) n -> p fi n", p=128)
            f0 = 0
            while f0 < NFI:
                nf = min(4, NFI - f0)
                sl, skey = load_tile([(0, nf, 1024, wdv[:, f0:f0 + nf, :])])
                wv = sl[:, 0:nf * 1024].rearrange("p (a b) -> p a b", a=nf)
                for fl in range(nf):
                    fi = f0 + fl
                    for j in range(NBLK):
                        for half in range(2):
                            ak, akey = accs[j][half]
                            S.op("pe", (lambda fi, fl, j, half, ak, wv: lambda e: e.matmul(
                                ak[:, :], actT[:, fi, j * 128:(j + 1) * 128], wv[:, fl, half * 512:(half + 1) * 512],
                                start=(fi == 0), stop=(fi == NFI - 1)))(fi, fl, j, half, ak, wv),
                                 reads=["actT%d" % fi, skey], writes=[akey], safe_same=[akey])
                f0 += nf
            for j in range(NBLK):
                S.cur_tag = "L%d/F3" % l
                for half in range(2):
                    ak, akey = accs[j][half]
                    S.op("dve", lambda e: e.tensor_tensor(
                        out=h[:, j, half * 512:(half + 1) * 512], in0=ak[:, :], in1=h[:, j, half * 512:(half + 1) * 512],
                        op=ALU.add), reads=[akey, "h%d" % j], writes=["h%d" % j])
                if last:
                    final_block(j, r0)
                else:
                    norm_block(j, Gm, "Gm", accs[j][0])

        sp_dma(Gfin[:], fin_g[0:1, :].partition_broadcast(128), "Gfin")
        for sb in range(nsb):
            r0 = sb * SBT
            for j in range(NBLK):
                S.dma("sp", lambda e: e.dma_start(out=h[:, j, :], in_=x[r0 + j * 128:r0 + (j + 1) * 128, :]),
                      write="h%d" % j)
            S.dma("sp", lambda e: e.dma_start(out=CS[:, :, :], in_=cs_d[r0:r0 + SBT, :].rearrange("(j p) n -> p j n", p=128)),
                  write="CS")
            first_mix = ('mix' in parts)
            sp_dma(Gm[:], (mix_g if first_mix else ffn_g)[0:1, :].partition_broadcast(128), "Gm")
            for j in range(NBLK):
                norm_block(j, Gm, "Gm")
            for l in range(depth):
                if 'mix' in parts:
                    if l % 2 == 0:
                        even_mixer(l)
                    else:
                        odd_mixer(l)
                if 'ffn' in parts:
                    ffn(l, l == depth - 1, r0)
        S.emit(nc)
    return nc


_NC_CACHE = {}


def _prep_inputs(inputs, b, nsb):
    seq = nsb * SBT
    tabs, _ = _const_tables(seq)
    f = lambda a: np.ascontiguousarray(np.asarray(a, dtype=np.float32))
    m = {
        "x": f(inputs["x"][b, :seq]),
        "mix_norm_g": f(inputs["mix_norm_g"]),
        "ffn_norm_g": f(inputs["ffn_norm_g"]),
        "final_norm_g": f(inputs["final_norm_g"]).reshape(1, D),
        "ab_w_in": f(inputs["ab_w_in"]),
        "ab_w_out": f(inputs["ab_w_out"]),
        "ret_norm_g": f(inputs["ret_norm_g"]).reshape(2, 512),
        "sgu_ln_g": f(inputs["sgu_ln_g"]),
        "sgu_ln_b": f(inputs["sgu_ln_b"]),
        "sgu_wT": f(np.transpose(np.asarray(inputs["sgu_w"]), (0, 1, 3, 2))),
        "sgu_bT": f(np.transpose(np.asarray(inputs["sgu_b"]), (0, 2, 1))),
        "c_w_in": f(inputs["c_w_in"]),
        "c_w_out": f(inputs["c_w_out"]),
        "c_b_i": f(inputs["c_b_i"]).reshape(2, 4, 1),
        "c_b_f": f(inputs["c_b_f"]).reshape(2, 4, 1),
        "c_norm_g": f(inputs["c_norm_g"]).reshape(2, 1024),
        "ffn_w_gate": f(inputs["ffn_w_gate"]),
        "ffn_w_up": f(inputs["ffn_w_up"]),
        "ffn_w_down": f(inputs["ffn_w_down"]),
    }
    m.update(tabs)
    return m


def run(inputs, nsb=SEQ // SBT, depth=DEPTH, ncores=8, trace=False, parts=('mix', 'ffn')):
    key = (nsb, depth, parts)
    if key not in _NC_CACHE:
        _NC_CACHE[key] = build(nsb, depth, parts)
    nc = _NC_CACHE[key]
    in_maps = [_prep_inputs(inputs, b, nsb) for b in range(ncores)]
    res = run_bass_kernel_spmd(nc, in_maps, core_ids=list(range(ncores)), **({"trace": True} if trace else {}))
    out = np.stack([np.asarray(r["y"], dtype=np.float32) for r in res.results], axis=0)
    return out, res


def kernel(**inputs):
    out, _ = run(inputs)
    return out
```

```python
import numpy as np
from contextlib import ExitStack
import concourse.bass as bass
import concourse.mybir as mybir
from concourse.bass_utils import run_bass_kernel_spmd

F32 = mybir.dt.float32
BF16 = mybir.dt.bfloat16
AF = mybir.ActivationFunctionType
ALU = mybir.AluOpType

D = 1024
SEQ = 4096
DEPTH = 4
TB = 128
NBLK = 4
SBT = TB * NBLK
DFF = 2816
NFI = DFF // 128
EPS = 1e-6
AB_IN = 3072
C_IN = 3080
NSLOT = 5
MODEL_FREE_BANKS = set()
POOL_OFFLOAD = set()
COPY_ON_DVE = {'hnT', 'mT'}
KSPLIT = 3
HDOUBLE = True


def _pe(tag):
    return "pool" if tag in POOL_OFFLOAD else "dve"


class _Op:
    __slots__ = ("fn", "eng", "seq", "deps", "odeps", "is_dma", "token", "dur", "xfer",
                 "signal", "sigval", "pos", "waits", "finish", "nsucc", "succ", "npend", "ready", "prio", "tag")

    def __init__(self, fn, eng, seq):
        self.fn = fn
        self.eng = eng
        self.seq = seq
        self.deps = []
        self.odeps = []
        self.is_dma = False
        self.token = None
        self.dur = 0.1
        self.xfer = 0.0
        self.signal = False
        self.sigval = None
        self.pos = None
        self.waits = []
        self.finish = 0.0
        self.succ = []
        self.npend = 0
        self.ready = 0.0


class _Rec:
    def __init__(self):
        self.calls = []

    def __getattr__(self, name):
        def f(*a, **k):
            self.calls.append((name, a, k))
            return None
        return f


def _freeze(fn, eng):
    r = _Rec()
    fn(r)
    assert len(r.calls) == 1, r.calls
    name, a, k = r.calls[0]
    out = k.get("out", a[0] if a else None)
    try:
        shp = tuple(out.shape)
        n = 1
        for d_ in shp[1:]:
            n *= int(d_)
        npart = int(shp[0])
    except Exception:
        n, npart = 64, 128
    if eng == "pe":
        dur = 0.11 if name == "transpose" else 0.012 + n / 1950.0
    elif eng == "act":
        dur = 0.22 + n * 1.0e-3
    elif eng == "dve":
        dur = 0.10 + n * 1.2e-3
    elif eng == "pool":
        dur = 0.2 + n * 2.0e-3
    else:
        dur = 0.1
    xfer = 0.0
    if name == "dma_start":
        dur = 1.0 if eng == "pool" else 0.15
        xfer = 2.0 + (n * npart * 4) / 300e3
    return (lambda h: getattr(h, name)(*a, **k)), dur, xfer


class _Buf:
    __slots__ = ("writer", "readers", "dma_sem", "dma_count")

    def __init__(self):
        self.writer = None
        self.readers = []
        self.dma_sem = None
        self.dma_count = 0


class Sched:
    ENGS = ("pe", "act", "dve", "pool", "sp")
    RESCHEDULE = True
    PRIO = "cp"

    def __init__(self):
        self.all = []
        self.bufs = {}
        self.dma_sems = []
        self.out_ops = {}
        self.cur_tag = ""

    def _buf(self, k):
        b = self.bufs.get(k)
        if b is None:
            b = self.bufs[k] = _Buf()
        return b

    def op(self, eng, fn, reads=(), writes=(), safe_same=(), dur=None):
        f, dur0, _ = _freeze(fn, eng)
        dur = dur0 if dur is None else dur
        o = _Op(f, eng, len(self.all))
        o.dur = dur
        o.tag = self.cur_tag
        seen = set()
        for k in reads:
            b = self._buf(k)
            if b.writer is not None:
                self._dep(o, b.writer, k in safe_same, seen)
        for k in writes:
            b = self._buf(k)
            if b.writer is not None:
                self._dep(o, b.writer, k in safe_same, seen)
            for r in b.readers:
                self._dep(o, r, k in safe_same, seen)
        self.all.append(o)
        for k in reads:
            self._buf(k).readers.append(o)
        for k in writes:
            b = self._buf(k)
            b.writer = o
            b.readers = []
        return o

    def _dep(self, o, d, safe, seen):
        if d is o:
            return
        key = (id(d), safe and d.eng == o.eng and not d.is_dma)
        if key in seen:
            return
        seen.add(key)
        if safe and d.eng == o.eng and not d.is_dma:
            o.odeps.append(d)
        else:
            o.deps.append(d)

    def dma(self, eng, fn, reads=(), write=None, is_output=False):
        f, dur, xfer = _freeze(fn, eng)
        o = _Op(f, eng, len(self.all))
        o.is_dma = True
        o.dur = dur
        o.xfer = xfer
        o.tag = self.cur_tag
        b = self._buf(write)
        if b.dma_sem is None:
            b.dma_sem = "dsem%d" % len(self.dma_sems)
            self.dma_sems.append(b.dma_sem)
        seen = set()
        for k in reads:
            rb = self._buf(k)
            if rb.writer is not None:
                self._dep(o, rb.writer, False, seen)
        if b.writer is not None:
            self._dep(o, b.writer, False, seen)
        for r in b.readers:
            self._dep(o, r, False, seen)
        b.dma_count += 1
        o.token = (b.dma_sem, 16 * b.dma_count)
        self.all.append(o)
        for k in reads:
            self._buf(k).readers.append(o)
        b.writer = o
        b.readers = []
        if is_output:
            self.out_ops[b.dma_sem] = o
        return o

    def _schedule(self):
        import heapq
        order = {e: [] for e in self.ENGS}
        if not self.RESCHEDULE:
            for o in self.all:
                order[o.eng].append(o)
            return order
        LAT = 0.25
        for o in self.all:
            o.succ = []
            o.npend = 0
        for o in self.all:
            for d in o.deps:
                d.succ.append((o, True))
                o.npend += 1
            for d in o.odeps:
                d.succ.append((o, False))
                o.npend += 1
        if self.PRIO == "cp":
            for o in self.all:
                o.prio = 0.0
            for o in reversed(self.all):
                b = 0.0
                for (s_, hard) in o.succ:
                    v = s_.prio + (LAT if hard else 0.0)
                    if v > b:
                        b = v
                o.prio = b + (o.xfer if o.is_dma else o.dur)
            for o in self.all:
                o.seq = -o.prio + o.seq * 1e-9
        waiting = {e: [] for e in self.ENGS}
        runnable = {e: [] for e in self.ENGS}
        tfree = {e: 0.0 for e in self.ENGS}
        dma_free = [0.0]
        for o in self.all:
            if o.npend == 0:
                heapq.heappush(waiting[o.eng], (0.0, o.seq, o))
        left = len(self.all)
        while left:
            best = None
            for e in self.ENGS:
                w, r = waiting[e], runnable[e]
                while w and w[0][0] <= tfree[e]:
                    _, sq, o = heapq.heappop(w)
                    heapq.heappush(r, (sq, o))
                if r:
                    cand = (tfree[e], r[0][0], e, True)
                elif w:
                    cand = (w[0][0], w[0][1], e, False)
                else:
                    continue
                if best is None or cand < best:
                    best = cand
            start, _, e, from_r = best
            if from_r:
                _, o = heapq.heappop(runnable[e])
            else:
                _, _, o = heapq.heappop(waiting[e])
            tfree[e] = start + o.dur
            if o.is_dma:
                t0 = max(start + o.dur, dma_free[0])
                dma_free[0] = t0 + (o.xfer - 2.0)
                o.finish = t0 + o.xfer
            else:
                o.finish = start + o.dur
            order[e].append(o)
            left -= 1
            for (s_, hard) in o.succ:
                rdy = o.finish + (LAT if hard else 0.0)
                if rdy > s_.ready:
                    s_.ready = rdy
                s_.npend -= 1
                if s_.npend == 0:
                    heapq.heappush(waiting[s_.eng], (s_.ready, s_.seq, s_))
        return order

    def emit(self, nc, final_eng="sp"):
        EPOCH = 30000
        order = self._schedule()
        for e in self.ENGS:
            for i, o in enumerate(order[e]):
                o.pos = i
        for e in self.ENGS:
            known = {}
            for o in order[e]:
                o.waits = []
                for d in sorted(o.deps, key=lambda d_: -(d_.token[1] if d_.is_dma else d_.pos)):
                    if d.is_dma:
                        sname, val = d.token
                        if known.get(sname, -1) >= val:
                            continue
                        known[sname] = val
                        o.waits.append(d)
                    else:
                        if known.get(d.eng, -1) >= d.pos:
                            continue
                        known[d.eng] = d.pos
                        d.signal = True
                        o.waits.append(d)
                for d in o.odeps:
                    assert d.eng == e and d.pos < o.pos
        with ExitStack() as st:
            nsig = {}
            for e in self.ENGS:
                c = 0
                for o in order[e]:
                    if (not o.is_dma) and o.signal:
                        c += 1
                        o.sigval = c
                nsig[e] = c
            esem = {e: [st.enter_context(nc.semaphore("s_%s_%d" % (e, i)))
                        for i in range(max(1, (nsig[e] + EPOCH - 1) // EPOCH))] for e in self.ENGS}
            dsem = {n: st.enter_context(nc.semaphore(n)) for n in self.dma_sems}
            block = st.enter_context(nc.Block())

            def run(eng_name, handle):
                for o in order[eng_name]:
                    for d in o.waits:
                        if d.is_dma:
                            handle.wait_ge(dsem[d.token[0]], d.token[1])
                        else:
                            sv = d.sigval - 1
                            handle.wait_ge(esem[d.eng][sv // EPOCH], sv % EPOCH + 1)
                    ins = o.fn(handle)
                    if o.is_dma:
                        ins.then_inc(dsem[o.token[0]], 16)
                    elif o.signal:
                        ins.then_inc(esem[eng_name][(o.sigval - 1) // EPOCH], 1)
                if eng_name == final_eng:
                    for d in self.out_ops.values():
                        handle.wait_ge(dsem[d.token[0]], d.token[1])

            @block.tensor
            def _(h):
                run("pe", h)

            @block.scalar
            def _(h):
                run("act", h)

            @block.vector
            def _(h):
                run("dve", h)

            @block.gpsimd
            def _(h):
                run("pool", h)

            @block.sync
            def _(h):
                run("sp", h)


def _const_tables(seq):
    half = 64
    inv = (10000.0 ** (-np.arange(half, dtype=np.float32) / np.float32(half))).astype(np.float32)
    pos = np.arange(seq, dtype=np.float32)
    ang = (pos[:, None] * inv[None, :]).astype(np.float32)
    cos = np.cos(ang).astype(np.float32)
    sin = np.sin(ang).astype(np.float32)
    cos4 = np.tile(cos, (1, 4))
    sin4 = np.tile(sin, (1, 4))
    cs = np.concatenate([cos4, sin4], axis=1).astype(np.float32)
    H = 4
    log_g = np.log1p(-np.power(2.0, -5.0 - np.arange(H, dtype=np.float64)))
    idx = np.arange(128, dtype=np.float64)
    s = idx[:, None]
    c = idx[None, :]
    same = (np.floor(s / 64) == np.floor(c / 64))
    lower = (np.floor(s / 64) < np.floor(c / 64))
    scale = 128.0 ** -0.5
    DQ = np.zeros((128, H, 128), np.float64)
    QDT = np.zeros((128, H, 128), np.float64)
    KD = np.zeros((128, H, 128), np.float64)
    g128 = []
    for h in range(H):
        lg = log_g[h]
        Dm = np.where(same, np.exp(lg * np.abs(c - s)), np.where(lower, np.exp(lg * (c - s)), 0.0))
        DQ[:, h, :] = scale * Dm / np.exp(lg * (c + 1.0))
        QDT[:, h, :] = np.exp(lg * (c + 1.0))
        KD[:, h, :] = scale * np.exp(lg * (127.0 - s))
        g128.append(float(np.exp(lg * 128.0)))
    CM = (s <= c).astype(np.float32)
    return dict(cs=cs, DQ=DQ.reshape(128, 512).astype(np.float32),
                QDT=QDT.reshape(128, 512).astype(np.float32),
                KD=KD.reshape(128, 512).astype(np.float32), CM=CM), g128


def build(nsb=SEQ // SBT, depth=DEPTH, parts=('mix', 'ffn')):
    seq = nsb * SBT
    _, G128 = _const_tables(128)
    nc = bass.Bass("TRN2", target_bir_lowering=False)

    def din(name, shape):
        return nc.dram_tensor(name, list(shape), F32, kind="ExternalInput").ap()

    x = din("x", [seq, D])
    mix_g = din("mix_norm_g", [4, D])
    ffn_g = din("ffn_norm_g", [4, D])
    fin_g = din("final_norm_g", [1, D])
    ab_w_in = din("ab_w_in", [2, D, AB_IN])
    ab_w_out = din("ab_w_out", [2, D, D])
    ret_g = din("ret_norm_g", [2, 512])
    sgu_g = din("sgu_ln_g", [2, 512])
    sgu_bb = din("sgu_ln_b", [2, 512])
    sgu_wT = din("sgu_wT", [2, 4, 128, 128])
    sgu_bT = din("sgu_bT", [2, 128, 4])
    c_w_in = din("c_w_in", [2, D, C_IN])
    c_w_out = din("c_w_out", [2, D, D])
    c_bi = din("c_b_i", [2, 4, 1])
    c_bf = din("c_b_f", [2, 4, 1])
    c_ng = din("c_norm_g", [2, 1024])
    w_gate = din("ffn_w_gate", [4, D, DFF])
    w_up = din("ffn_w_up", [4, D, DFF])
    w_down = din("ffn_w_down", [4, DFF, D])
    cs_d = din("cs", [seq, 512])
    DQ_d = din("DQ", [128, 512])
    QDT_d = din("QDT", [128, 512])
    KD_d = din("KD", [128, 512])
    CM_d = din("CM", [128, 128])
    y = nc.dram_tensor("y", [seq, D], F32, kind="ExternalOutput").ap()

    S = Sched()
    with ExitStack() as st:
        def T(name, shape, dt=F32):
            return st.enter_context(nc.sbuf_tensor("sb_" + name, list(shape), dt))

        hbufs = [T("h_a", [128, NBLK, D]), T("h_b", [128, NBLK, D])] if HDOUBLE else [T("h_a", [128, NBLK, D])]
        h = hbufs[0]
        hpar = [0]

        def hkey(j):
            return "h%d_%d" % (hpar[0], j)

        hnT = T("hnT", [128, 8, SBT], BF16)
        hn = T("hn", [128, D], BF16)
        junk = T("junk", [128, D], BF16)
        slots = [T("slot%d" % i, [128, 4096], BF16) for i in range(NSLOT)]
        stg = T("stg", [128, 14400], BF16)
        actT = stg[:, 0:NFI * SBT].rearrange("p (a b) -> p a b", a=NFI)
        mixed = T("mixed", [128, D], BF16)
        mT = T("mT", [128, 8, 128], BF16)
        Gm = T("Gm", [128, D]); Gf = T("Gf", [128, D]); Gfin = T("Gfin", [128, D])
        NDs = [T("NDs%d" % i, [128, 4, 258]) for i in range(2)]
        low4 = T("low4", [128, 4]); rl4 = T("rl4", [128, 4])
        CS = T("CS", [128, NBLK, 512])
        DQ = T("DQ", [128, 512]); QDT = T("QDT", [128, 512]); KD = T("KD", [128, 512])
        CM = T("CM", [128, 128])
        idf = T("idf", [128, 128]); idb = T("idb", [128, 128], BF16)
        ones4 = T("ones4", [4, 128]); zrow = T("zrow", [4, 128])
        RG = T("RG", [128, 512]); SG = T("SG", [128, 512]); SBB = T("SBB", [128, 512])
        WT = T("WT", [128, 4, 128], BF16); sbias = T("sbias", [128, 4])
        CNG = T("CNG", [128, 1024])
        bi = T("bi", [4, 1]); bfm = T("bfm", [4, 1])
        Sret = [T("Sret%d" % i, [128, 512]) for i in range(2)]
        Sretb = [T("Sretb%d" % i, [128, 512], BF16) for i in range(2)]
        Cst = [T("Cst%d" % i, [128, 4, 258]) for i in range(2)]
        Cb = T("Cb", [128, 4, 258], BF16)
        mst = [T("mst%d" % i, [4, 1]) for i in range(2)]
        ss = T("ss", [128, 1]); sd = T("sd", [128, 1]); rstd = T("rstd", [128, 1])
        ss2 = T("ss2", [128, 1]); sd2 = T("sd2", [128, 1]); rstd2 = T("rstd2", [128, 1])
        junk2 = T("junk2", [128, D], BF16)
        tA = T("tA", [128, 256]); tB = T("tB", [128, 256])
        rot = T("rot", [128, 512], BF16)
        gv = T("gv", [128, 512])
        st6 = T("st6", [128, 4, 6]); mv = T("mv", [128, 4, 2])
        sd4 = T("sd4", [128, 4]); rs4 = T("rs4", [128, 4]); nm4 = T("nm4", [128, 4])
        rn = T("rn", [128, 512])
        Pb = T("Pb", [128, 512], BF16)
        sgt = T("sgt", [128, 512])
        li = T("li", [4, SBT]); lfn = T("lfn", [4, SBT]); ex = lfn
        nb = T("nb", [4, 128]); aa = T("aa", [4, 128]); gg = T("gg", [4, 128])
        R3 = T("R3", [4, 3, 128])
        ngl = T("ngl", [4, NBLK]); dec = T("dec", [4, NBLK]); dg = T("dg", [4, NBLK, 4])
        tmpr = T("tmpr", [4, 128])
        cols = T("cols", [128, NBLK, 16])
        low = T("low", [128, 1]); rl = T("rl", [128, 1])
        hs = T("hs", [128, 256]); hy = T("hy", [128, 1024])

        banks = [st.enter_context(nc.psum_tensor("bank%d" % i, [128, 512], F32)) for i in range(8)]
        bank_ctr = [0]

        uniq = [0]

        def _bkey(i, cls):
            if cls in MODEL_FREE_BANKS:
                uniq[0] += 1
                return "bankfree%d" % uniq[0]
            return "bank%d" % i

        def newbank(cls="x"):
            i = bank_ctr[0] % 8
            bank_ctr[0] += 1
            return banks[i], _bkey(i, cls)

        def stg_view(off, shape):
            n = int(np.prod(shape))
            ap = stg[:, off:off + n]
            if len(shape) == 2:
                return ap.rearrange("p (a b) -> p a b", a=shape[0])
            if len(shape) == 3:
                return ap.rearrange("p (a b c) -> p a b c", a=shape[0], b=shape[1])
            return ap

        qdT = stg_view(0, (NBLK, 4, 128)); kT = stg_view(2048, (NBLK, 4, 128))
        kd = stg_view(4096, (NBLK, 512)); vb = stg_view(6144, (NBLK, 512))
        sgl = stg_view(8192, (NBLK, 512)); ug = stg_view(10240, (NBLK, 512))
        vsn = stg_view(12288, (NBLK, 512))
        qrT = qdT; kdT = kT; kdc = kd
        vext = stg_view(6144, (NBLK, 4, 258))
        so = stg_view(10272, (NBLK, 1024))
        assert 10272 + 4096 <= 14400

        slot_ctr = [0]

        def load_tile(dmas):
            i = slot_ctr[0] % NSLOT
            slot_ctr[0] += 1
            sl = slots[i]
            key = "slot%d" % i
            for (lo, a, b, src) in dmas:
                dst = sl[:, lo:lo + a * b].rearrange("p (a b) -> p a b", a=a)
                S.dma("pool", (lambda dst, src: lambda e: e.dma_start(out=dst, in_=src))(dst, src), write=key)
            return sl, key

        def wcols(w2d, c0, ncol):
            return w2d.rearrange("(kc p) n -> p kc n", p=128)[:, :, c0:c0 + ncol]

        sp_dma = lambda dst, src, key: S.dma("sp", (lambda e: e.dma_start(out=dst, in_=src)), write=key)
        sp_dma(DQ[:], DQ_d[:], "DQ"); sp_dma(QDT[:], QDT_d[:], "QDT"); sp_dma(KD[:], KD_d[:], "KD")
        sp_dma(CM[:], CM_d[:], "CM")
        S.op("pool", lambda e: e.memset(idf[:], 0.0), writes=["idf"])
        S.op("pool", lambda e: e.affine_select(out=idf[:], in_=idf[:], compare_op=ALU.not_equal, fill=1.0,
                                               base=0, pattern=[[-1, 128]], channel_multiplier=1),
             reads=["idf"], writes=["idf"])
        S.op("pool", lambda e: e.tensor_copy(idb[:], idf[:]), reads=["idf"], writes=["idb"])
        S.op("pool", lambda e: e.memset(ones4[:], 1.0), writes=["ones4"])
        S.op("pool", lambda e: e.memset(zrow[:], 0.0), writes=["zrow"])
        for i in range(2):
            S.op("pool", (lambda i: lambda e: e.memset(Sret[i][:], 0.0))(i), writes=["Sret%d" % i])
            S.op("pool", (lambda i: lambda e: e.memset(Sretb[i][:], 0.0))(i), writes=["Sretb%d" % i])
            S.op("pool", (lambda i: lambda e: e.memset(Cst[i][:], 0.0))(i), writes=["Cst%d" % i])
            S.op("pool", (lambda i: lambda e: e.memset(mst[i][:], 0.0))(i), writes=["mst%d" % i])

        def tcopy(tag, out_ap, in_ap, reads, writes):
            n = 1
            for d_ in tuple(out_ap.shape)[1:]:
                n *= int(d_)
            if tag in COPY_ON_DVE:
                S.op("dve", lambda e: e.tensor_copy(out_ap, in_ap), reads=reads, writes=writes, dur=0.1 + n * 0.6e-3)
            else:
                S.op("act", lambda e: e.activation(out=out_ap, in_=in_ap, func=AF.Copy), reads=reads, writes=writes)

        def B(i, cls="x"):
            return banks[i], _bkey(i, cls)

        def norm_block(j, Gt, gkey, bank=None):
            S.cur_tag = S.cur_tag.split("/")[0] + "/norm"
            hk = hkey(j)
            S.op("act", lambda e: e.activation(out=junk[:], in_=h[:, j, :], func=AF.Square, accum_out=ss[:]),
                 reads=[hk], writes=["junk", "ss"])
            S.op("act", lambda e: e.activation(out=sd[:], in_=ss[:], func=AF.Sqrt, bias=EPS, scale=1.0 / D),
                 reads=["ss"], writes=["sd"])
            S.op("dve", lambda e: e.reciprocal(rstd[:], sd[:]), reads=["sd"], writes=["rstd"])
            S.op("dve", lambda e: e.scalar_tensor_tensor(out=hn[:], in0=h[:, j, :], scalar=rstd[:, 0:1],
                                                         in1=Gt[:], op0=ALU.mult, op1=ALU.mult),
                 reads=[hk, "rstd", gkey], writes=["hn"])
            bk, bkey = bank if bank is not None else newbank('norm')
            bb = bk[:].bitcast(BF16)
            for kc in range(8):
                S.op("pe", lambda e: e.transpose(bb[:, kc * 128:(kc + 1) * 128], hn[:, kc * 128:(kc + 1) * 128], idb[:]),
                     reads=["hn", "idb"], writes=[bkey], safe_same=[bkey])
            tcopy("hnT", hnT[:, :, j * 128:(j + 1) * 128], bb[:, 0:1024].rearrange("p (a b) -> p a b", a=8),
                  [bkey], ["hnT%d" % j])

        def final_block(j, r0):
            hk = hkey(j)
            hyk = ["hy_%d" % q for q in range(4)]
            S.op("act", lambda e: e.activation(out=junk2[:], in_=h[:, j, :], func=AF.Square, accum_out=ss2[:]),
                 reads=[hk], writes=["junk2", "ss2"])
            S.op("act", lambda e: e.activation(out=sd2[:], in_=ss2[:], func=AF.Sqrt, bias=EPS, scale=1.0 / D),
                 reads=["ss2"], writes=["sd2"])
            S.op("dve", lambda e: e.reciprocal(rstd2[:], sd2[:]), reads=["sd2"], writes=["rstd2"])
            S.op("dve", lambda e: e.scalar_tensor_tensor(out=hy[:], in0=h[:, j, :], scalar=rstd2[:, 0:1],
                                                         in1=Gfin[:], op0=ALU.mult, op1=ALU.mult),
                 reads=[hk, "rstd2", "Gfin"], writes=hyk)
            S.dma("sp", lambda e: e.dma_start(out=y[r0 + j * 128:r0 + (j + 1) * 128, :], in_=hy[:]),
                  reads=hyk, write="y", is_output=True)

        def proj_block(j, sl, skey, ncol=512, coff=0, stride=None):
            stride = stride or ncol
            bk, bkey = newbank('proj')
            wv = sl[:, 0:8 * stride].rearrange("p (a b) -> p a b", a=8)
            for kc in range(8):
                S.op("pe", (lambda kc: lambda e: e.matmul(bk[:, 0:ncol], hnT[:, kc, j * 128:(j + 1) * 128],
                                                          wv[:, kc, coff:coff + ncol],
                                                          start=(kc == 0), stop=(kc == 7)))(kc),
                     reads=["hnT%d" % j, skey], writes=[bkey], safe_same=[bkey])
            return bk, bkey

        def rope_block(j, bk, bkey, outT):
            pv = bk[:, 0:512].rearrange("p (h t i) -> p h t i", h=4, t=2)
            t1 = pv[:, :, 0, :]
            t2 = pv[:, :, 1, :]
            cos = CS[:, j, 0:256].rearrange("p (h i) -> p h i", h=4)
            sin = CS[:, j, 256:512].rearrange("p (h i) -> p h i", h=4)
            rv = rot[:, :].rearrange("p (h t i) -> p h t i", h=4, t=2)
            A = tA[:, :].rearrange("p (h i) -> p h i", h=4)
            B = tB[:, :].rearrange("p (h i) -> p h i", h=4)
            ck = "CS"
            S.op("dve", lambda e: e.tensor_tensor(out=A, in0=t1, in1=cos, op=ALU.mult), reads=[bkey, ck], writes=["tA"])
            S.op("dve", lambda e: e.tensor_tensor(out=B, in0=t2, in1=sin, op=ALU.mult), reads=[bkey, ck], writes=["tB"])
            S.op(_pe("rope2"), lambda e: e.tensor_tensor(out=rv[:, :, 0, :], in0=A, in1=B, op=ALU.subtract),
                 reads=["tA", "tB"], writes=["rot"])
            S.op("dve", lambda e: e.tensor_tensor(out=A, in0=t1, in1=sin, op=ALU.mult), reads=[bkey, ck], writes=["tA"])
            S.op("dve", lambda e: e.tensor_tensor(out=B, in0=t2, in1=cos, op=ALU.mult), reads=[bkey, ck], writes=["tB"])
            S.op(_pe("rope2"), lambda e: e.tensor_tensor(out=rv[:, :, 1, :], in0=A, in1=B, op=ALU.add),
                 reads=["tA", "tB"], writes=["rot"])

        def transpose4(src_ap_fn, src_key):
            bk, bkey = newbank('tr4')
            bb = bk[:].bitcast(BF16)
            for hh in range(4):
                S.op("pe", (lambda hh: lambda e: e.transpose(bb[:, hh * 128:(hh + 1) * 128], src_ap_fn(hh), idb[:]))(hh),
                     reads=[src_key, "idb"], writes=[bkey], safe_same=[bkey])
            return bb, bkey

        def out_proj_block(j, tiles, tb, pbs):
            bk, bkey = tb
            bb = bk[:].bitcast(BF16)
            for kc in range(8):
                S.op("pe", lambda e: e.transpose(bb[:, kc * 128:(kc + 1) * 128], mixed[:, kc * 128:(kc + 1) * 128], idb[:]),
                     reads=["mixed", "idb"], writes=[bkey], safe_same=[bkey])
            tcopy("mT", mT[:, :, :], bb[:, 0:1024].rearrange("p (a b) -> p a b", a=8), [bkey], ["mT"])
            for half in range(2):
                sl, skey = tiles[half]
                wv = sl[:, 0:4096].rearrange("p (a b) -> p a b", a=8)
                pk, pkey = pbs[half]
                for kc in range(8):
                    S.op("pe", lambda e: e.matmul(pk[:, :], mT[:, kc, :], wv[:, kc, :], start=(kc == 0), stop=(kc == 7)),
                         reads=["mT", skey], writes=[pkey], safe_same=[pkey])
                S.op("dve", lambda e: e.tensor_tensor(out=h[:, j, half * 512:(half + 1) * 512], in0=pk[:, :],
                                                      in1=h[:, j, half * 512:(half + 1) * 512], op=ALU.add),
                     reads=[pkey, hkey(j)], writes=[hkey(j)])

        def ln_stats(src_aps, skeys, n):
            for g_ in range(n):
                S.op("dve", (lambda g_: lambda e: e.bn_stats(st6[:, g_, :], src_aps[g_]))(g_),
                     reads=[skeys[g_]], writes=["st6_%d" % g_])
                S.op("dve", (lambda g_: lambda e: e.bn_aggr(mv[:, g_, :], st6[:, g_, :]))(g_),
                     reads=["st6_%d" % g_], writes=["mv_%d" % g_])
            mvk = ["mv_%d" % g_ for g_ in range(n)]
            S.op("act", lambda e: e.activation(out=sd4[:, 0:n], in_=mv[:, 0:n, 1], func=AF.Sqrt, bias=EPS, scale=1.0),
                 reads=mvk, writes=["sd4"])
            S.op("dve", lambda e: e.reciprocal(rs4[:, 0:n], sd4[:, 0:n]), reads=["sd4"], writes=["rs4"])
            S.op("dve", lambda e: e.scalar_tensor_tensor(out=nm4[:, 0:n], in0=mv[:, 0:n, 0], scalar=-1.0,
                                                         in1=rs4[:, 0:n], op0=ALU.mult, op1=ALU.mult),
                 reads=mvk + ["rs4"], writes=["nm4"])

        def even_mixer(l):
            S.cur_tag = "L%d/A" % l
            jl = l // 2
            w_in = ab_w_in[jl]
            sp_dma(Gm[:], mix_g[l:l + 1, :].partition_broadcast(128), "Gm")
            sp_dma(RG[:], ret_g[jl:jl + 1, :].partition_broadcast(128), "RG")
            sp_dma(SG[:], sgu_g[jl:jl + 1, :].partition_broadcast(128), "SG")
            sp_dma(SBB[:], sgu_bb[jl:jl + 1, :].partition_broadcast(128), "SBB")
            sp_dma(sbias[:], sgu_bT[jl], "sbias")
            S.dma("pool", lambda e: e.dma_start(out=WT[:], in_=sgu_wT[jl].rearrange("g q p -> q g p")), write="WT")
            S.op("pool", lambda e: e.memset(WT[64:128, :, 0:64], 0.0), reads=["WT"], writes=["WT"])
            sp_dma(Gf[:], ffn_g[l:l + 1, :].partition_broadcast(128), "Gf")
            for ct in range(6):
                sl, skey = load_tile([(0, 8, 512, wcols(w_in, ct * 512, 512))])
                for j in range(NBLK):
                    bk, bkey = proj_block(j, sl, skey)
                    if ct == 0:
                        rope_block(j, bk, bkey, None)
                        bb, tkey = transpose4(lambda hh: rot[:, hh * 128:(hh + 1) * 128], "rot")
                        S.op("dve", (lambda j, bb: lambda e: e.tensor_tensor(
                            out=qdT[:, j, :, :], in0=bb[:, 0:512].rearrange("p (a b) -> p a b", a=4),
                            in1=QDT[:, :].rearrange("p (a b) -> p a b", a=4), op=ALU.mult))(j, bb),
                             reads=[tkey, "QDT"], writes=["qdT%d" % j])
                    elif ct == 1:
                        rope_block(j, bk, bkey, None)
                        S.op(_pe("kd"), (lambda j: lambda e: e.tensor_tensor(out=kd[:, j, :], in0=rot[:, :], in1=KD[:, :],
                                                                         op=ALU.mult))(j),
                             reads=["rot", "KD"], writes=["kd%d" % j])
                        bb, tkey = transpose4(lambda hh: rot[:, hh * 128:(hh + 1) * 128], "rot")
                        tcopy("kT", kT[:, j, :, :], bb[:, 0:512].rearrange("p (a b) -> p a b", a=4), [tkey], ["kT%d" % j])
                    elif ct == 2:
                        S.op("act", (lambda j, bk: lambda e: e.activation(out=vb[:, j, :], in_=bk[:, :], func=AF.Copy))(j, bk),
                             reads=[bkey], writes=["vb%d" % j])
                    elif ct == 3:
                        S.op("act", (lambda j, bk: lambda e: e.activation(out=sgl[:, j, :], in_=bk[:, :], func=AF.Silu))(j, bk),
                             reads=[bkey], writes=["sgl%d" % j])
                    elif ct == 4:
                        S.op("act", (lambda j, bk: lambda e: e.activation(out=ug[:, j, :], in_=bk[:, :],
                                                                          func=AF.Gelu_apprx_tanh))(j, bk),
                             reads=[bkey], writes=["ug%d" % j])
                    else:
                        S.op("act", (lambda bk: lambda e: e.activation(out=gv[:, :], in_=bk[:, :],
                                                                       func=AF.Gelu_apprx_tanh))(bk),
                             reads=[bkey], writes=["gv"])
                        ln_stats([gv[:, :]], ["gv"], 1)
                        S.op("act", lambda e: e.activation(out=gv[:, :], in_=gv[:, :], func=AF.Identity,
                                                           scale=rs4[:, 0:1], bias=nm4[:, 0:1]),
                             reads=["gv", "rs4", "nm4"], writes=["gv"])
                        S.op(_pe("vs"), lambda e: e.tensor_tensor(out=gv[:, :], in0=gv[:, :], in1=SG[:, :], op=ALU.mult),
                             reads=["gv", "SG"], writes=["gv"])
                        S.op(_pe("vs"), (lambda j: lambda e: e.tensor_tensor(out=vsn[:, j, :], in0=gv[:, :], in1=SBB[:, :],
                                                                         op=ALU.add))(j),
                             reads=["gv", "SBB"], writes=["vsn%d" % j])
            otiles = [load_tile([(0, 8, 512, wcols(ab_w_out[jl], half * 512, 512))]) for half in range(2)]
            Sr, Srb = Sret[jl], Sretb[jl]
            sk, sbk = "Sret%d" % jl, "Sretb%d" % jl

            def S1(j):
                S.cur_tag = "L%d/S1" % l
                pk, pkey = B(0, 'eS1')
                for hh in range(4):
                    S.op("pe", lambda e: e.matmul(pk[:, hh * 128:(hh + 1) * 128], kT[:, j, hh, :], qdT[:, j, hh, :],
                                                  start=True, stop=True),
                         reads=["kT%d" % j, "qdT%d" % j], writes=[pkey], safe_same=[pkey])
                S.op("dve", lambda e: e.tensor_tensor(out=Pb[:, :], in0=pk[:, :], in1=DQ[:, :], op=ALU.mult),
                     reads=[pkey, "DQ"], writes=["Pb"])
                ok_, okey = B(1 + (j % 2), 'eOK')
                for hh in range(4):
                    S.op("pe", lambda e: e.matmul(ok_[:, hh * 128:(hh + 1) * 128], Pb[:, hh * 128:(hh + 1) * 128],
                                                  vb[:, j, hh * 128:(hh + 1) * 128], start=True, stop=False),
                         reads=["Pb", "vb%d" % j], writes=[okey], safe_same=[okey])
                    S.op("pe", lambda e: e.matmul(ok_[:, hh * 128:(hh + 1) * 128], qdT[:, j, hh, :],
                                                  Srb[:, hh * 128:(hh + 1) * 128], start=False, stop=True),
                         reads=["qdT%d" % j, sbk], writes=[okey], safe_same=[okey])
                kk, kkey = B(0, 'eS1')
                for hh in range(4):
                    S.op("pe", lambda e: e.matmul(kk[:, hh * 128:(hh + 1) * 128], kd[:, j, hh * 128:(hh + 1) * 128],
                                                  vb[:, j, hh * 128:(hh + 1) * 128], start=True, stop=True),
                         reads=["kd%d" % j, "vb%d" % j], writes=[kkey], safe_same=[kkey])
                for hh in range(4):
                    S.op("dve", lambda e: e.scalar_tensor_tensor(
                        out=Sr[:, hh * 128:(hh + 1) * 128], in0=Sr[:, hh * 128:(hh + 1) * 128], scalar=G128[hh],
                        in1=kk[:, hh * 128:(hh + 1) * 128], op0=ALU.mult, op1=ALU.add),
                         reads=[kkey, sk + "_%d" % hh], writes=[sk + "_%d" % hh])
                S.op("act", lambda e: e.activation(out=Srb[:, :], in_=Sr[:, :], func=AF.Copy),
                     reads=[sk + "_%d" % hh for hh in range(4)], writes=[sbk])

            def S2(j):
                S.cur_tag = "L%d/S2" % l
                ok_, okey = B(1 + (j % 2), 'eOK')
                rnk = ["rn_%d" % hh for hh in range(4)]
                ln_stats([ok_[:, hh * 128:(hh + 1) * 128] for hh in range(4)], [okey] * 4, 4)
                for hh in range(4):
                    S.op("act", lambda e: e.activation(
                        out=rn[:, hh * 128:(hh + 1) * 128], in_=ok_[:, hh * 128:(hh + 1) * 128], func=AF.Identity,
                        scale=rs4[:, hh:hh + 1], bias=nm4[:, hh:hh + 1]),
                         reads=[okey, "rs4", "nm4"], writes=["rn_%d" % hh])
                S.op(_pe("rn"), lambda e: e.tensor_tensor(out=rn[:, :], in0=rn[:, :], in1=RG[:, :], op=ALU.mult),
                     reads=rnk + ["RG"], writes=rnk)
                S.op(_pe("rn2"), lambda e: e.tensor_tensor(out=mixed[:, 0:512], in0=rn[:, :], in1=sgl[:, j, :], op=ALU.mult),
                     reads=rnk + ["sgl%d" % j], writes=["mixed"])
                gk, gkey = B(4, 'eS2')
                for g_ in range(4):
                    S.op("pe", lambda e: e.matmul(gk[:, g_ * 128:(g_ + 1) * 128], WT[:, g_, :],
                                                  vsn[:, j, g_ * 128:(g_ + 1) * 128], start=True, stop=True),
                         reads=["WT", "vsn%d" % j], writes=[gkey], safe_same=[gkey])
                for g_ in range(4):
                    S.op("dve", lambda e: e.scalar_tensor_tensor(
                        out=mixed[:, 512 + g_ * 128:512 + (g_ + 1) * 128], in0=gk[:, g_ * 128:(g_ + 1) * 128],
                        scalar=sbias[:, g_:g_ + 1], in1=ug[:, j, g_ * 128:(g_ + 1) * 128],
                        op0=ALU.add, op1=ALU.mult),
                         reads=[gkey, "sbias", "ug%d" % j], writes=["mixed"])
                out_proj_block(j, otiles, B(5, 'op'), [B(6, 'op'), B(7, 'op')])
                norm_block(j, Gf, "Gf", B(3, 'op'))

            S1(0)
            for j in range(NBLK):
                if j + 1 < NBLK:
                    S1(j + 1)
                S2(j)

        def odd_mixer(l):
            S.cur_tag = "L%d/A" % l
            jl = l // 2
            w_in = c_w_in[jl]
            C = Cst[jl]
            ck = "Cst%d" % jl
            msk = "mst%d" % jl
            ms = mst[jl]
            sp_dma(Gm[:], mix_g[l:l + 1, :].partition_broadcast(128), "Gm")
            sp_dma(CNG[:], c_ng[jl:jl + 1, :].partition_broadcast(128), "CNG")
            sp_dma(bi[:], c_bi[jl], "bi")
            sp_dma(bfm[:], c_bf[jl], "bfm")
            S.op("dve", lambda e: e.tensor_scalar(bfm[:], bfm[:], -1.0, None, ALU.mult), reads=["bfm"], writes=["bfm"])
            sp_dma(Gf[:], ffn_g[l:l + 1, :].partition_broadcast(128), "Gf")
            sl, skey = load_tile([(0, 8, 8, wcols(w_in, 3072, 8))])
            wv = sl[:, 0:64].rearrange("p (a b) -> p a b", a=8)
            pi, pikey = newbank('gate')
            pf, pfkey = newbank('gate')
            for kc in range(8):
                S.op("pe", (lambda kc: lambda e: e.matmul(pi[0:4, :], wv[:, kc, 0:4], hnT[:, kc, :],
                                                          start=(kc == 0), stop=(kc == 7)))(kc),
                     reads=["hnT%d" % j for j in range(NBLK)] + [skey], writes=[pikey], safe_same=[pikey])
            for kc in range(8):
                S.op("pe", (lambda kc: lambda e: e.matmul(pf[0:4, :], wv[:, kc, 4:8], hnT[:, kc, :],
                                                          start=(kc == 0), stop=(kc == 7)))(kc),
                     reads=["hnT%d" % j for j in range(NBLK)] + [skey], writes=[pfkey], safe_same=[pfkey])
            S.op("act", lambda e: e.activation(out=li[:, :], in_=pi[0:4, :], func=AF.Identity, bias=bi[:, 0:1], scale=1.0),
                 reads=[pikey, "bi"], writes=["li"])
            S.op("act", lambda e: e.activation(out=ex[:, :], in_=pf[0:4, :], func=AF.Exp, bias=bfm[:, 0:1], scale=-1.0),
                 reads=[pfkey, "bfm"], writes=["lfn"])
            S.op("act", lambda e: e.activation(out=lfn[:, :], in_=ex[:, :], func=AF.Ln, bias=1.0, scale=1.0),
                 reads=["lfn"], writes=["lfn"])
            for j in range(NBLK):
                cs_ = slice(j * 128, (j + 1) * 128)
                S.op("dve", (lambda cs_: lambda e: e.tensor_tensor_scan(nb[:, :], lfn[:, cs_], zrow[:, :], 0.0,
                                                                        ALU.add, ALU.add))(cs_),
                     reads=["lfn", "zrow"], writes=["nb"])
                S.op("dve", (lambda cs_: lambda e: e.tensor_tensor(out=aa[:, :], in0=li[:, cs_], in1=nb[:, :],
                                                                   op=ALU.add))(cs_),
                     reads=["li", "nb"], writes=["aa"])
                S.op("dve", lambda e: e.tensor_tensor_scan(gg[:, :], aa[:, :], aa[:, :], ms[:, 0:1], ALU.max, ALU.max),
                     reads=["aa", msk], writes=["gg"])
                S.op("dve", (lambda j: lambda e: e.tensor_scalar(ngl[:, j:j + 1], gg[:, 127:128], -1.0, None, ALU.mult))(j),
                     reads=["gg"], writes=["ngl%d" % j])
                S.op("act", (lambda j: lambda e: e.activation(out=dec[:, j:j + 1], in_=ms[:, 0:1], func=AF.Exp,
                                                              bias=ngl[:, j:j + 1], scale=1.0))(j),
                     reads=[msk, "ngl%d" % j], writes=["dec%d" % j])
                S.op("act", (lambda j: lambda e: e.activation(out=R3[:, 0, :], in_=aa[:, :], func=AF.Exp,
                                                              bias=ngl[:, j:j + 1], scale=1.0))(j),
                     reads=["aa", "ngl%d" % j], writes=["R3_0"])
                S.op("act", lambda e: e.activation(out=R3[:, 1, :], in_=gg[:, :], func=AF.Exp,
                                                   bias=gg[:, 127:128], scale=-1.0),
                     reads=["gg"], writes=["R3_1"])
                S.op("dve", lambda e: e.tensor_tensor(out=tmpr[:, :], in0=nb[:, :], in1=gg[:, :], op=ALU.subtract),
                     reads=["nb", "gg"], writes=["tmpr"])
                S.op("act", lambda e: e.activation(out=R3[:, 2, :], in_=tmpr[:, :], func=AF.Exp),
                     reads=["tmpr"], writes=["R3_2"])
                S.op("dve", lambda e: e.tensor_tensor(out=ms[:, 0:1], in0=gg[:, 127:128], in1=nb[:, 127:128],
                                                      op=ALU.subtract),
                     reads=["gg", "nb"], writes=[msk])
                S.op("dve", (lambda j: lambda e: e.tensor_scalar(dg[:, j, :], idf[0:4, 0:4], dec[:, j:j + 1], None, ALU.mult))(j),
                     reads=["idf", "dec%d" % j], writes=["dg%d" % j])
                ck_, ckey = newbank('gate')
                for q_ in range(3):
                    S.op("pe", (lambda q_, ck_: lambda e: e.matmul(ck_[:, q_ * 4:(q_ + 1) * 4], R3[:, q_, :],
                                                                  idf[0:4, 0:4], start=True, stop=True))(q_, ck_),
                         reads=["R3_%d" % q_, "idf"], writes=[ckey], safe_same=[ckey])
                S.op("pe", (lambda j, ck_: lambda e: e.matmul(ck_[:, 12:16], ones4[:, :], dg[:, j, :],
                                                              start=True, stop=True))(j, ck_),
                     reads=["ones4", "dg%d" % j], writes=[ckey], safe_same=[ckey])
                S.op("dve", (lambda j, ck_: lambda e: e.tensor_copy(cols[:, j, :], ck_[:, 0:16]))(j, ck_),
                     reads=[ckey], writes=["cols%d" % j])
                S.op("dve", (lambda j: lambda e: e.tensor_scalar(cols[:, j, 0:4], cols[:, j, 0:4], 128.0 ** -0.5, None,
                                                                 ALU.mult))(j),
                     reads=["cols%d" % j], writes=["cols%d" % j])
            sl, skey = load_tile([(0, 8, 512, wcols(w_in, 0, 512))])
            for j in range(NBLK):
                bk, bkey = proj_block(j, sl, skey)
                for hh in range(4):
                    S.op("act", (lambda hh, bk, j: lambda e: e.activation(
                        out=rot[:, hh * 128:(hh + 1) * 128], in_=bk[:, hh * 128:(hh + 1) * 128], func=AF.Copy,
                        scale=cols[:, j, 4 + hh:5 + hh]))(hh, bk, j),
                         reads=[bkey, "cols%d" % j], writes=["rot"])
                bb, tkey = transpose4(lambda hh: rot[:, hh * 128:(hh + 1) * 128], "rot")
                tcopy("qrT", qrT[:, j, :, :], bb[:, 0:512].rearrange("p (a b) -> p a b", a=4), [tkey], ["qdT%d" % j])
            sl, skey = load_tile([(0, 8, 512, wcols(w_in, 512, 512))])
            for j in range(NBLK):
                bk, bkey = proj_block(j, sl, skey)
                for hh in range(4):
                    S.op("act", (lambda hh, bk, j: lambda e: e.activation(
                        out=kdc[:, j, hh * 128:(hh + 1) * 128], in_=bk[:, hh * 128:(hh + 1) * 128], func=AF.Copy,
                        scale=cols[:, j, hh:hh + 1]))(hh, bk, j),
                         reads=[bkey, "cols%d" % j], writes=["kd%d" % j])
                bb, tkey = transpose4((lambda j: lambda hh: kdc[:, j, hh * 128:(hh + 1) * 128])(j), "kd%d" % j)
                tcopy("kdT", kdT[:, j, :, :], bb[:, 0:512].rearrange("p (a b) -> p a b", a=4), [tkey], ["kT%d" % j])
            for vt in range(2):
                sl, skey = load_tile([(0, 8, 512, wcols(w_in, 1024 + vt * 512, 512))])
                for j in range(NBLK):
                    bk, bkey = proj_block(j, sl, skey)
                    S.op("act", (lambda j, bk, vt: lambda e: e.activation(
                        out=vext[:, j, 2 * vt:2 * vt + 2, 0:256], in_=bk[:, :].rearrange("p (a b) -> p a b", a=2),
                        func=AF.Copy))(j, bk, vt),
                         reads=[bkey], writes=["vb%d" % j])
            for j in range(NBLK):
                S.op(_pe("vmem"), (lambda j: lambda e: e.memset(vext[:, j, :, 256:257], 1.0))(j), reads=[], writes=["vb%d" % j])
            for ot in range(2):
                sl, skey = load_tile([(0, 8, 512, wcols(w_in, 2048 + ot * 512, 512))])
                for j in range(NBLK):
                    bk, bkey = proj_block(j, sl, skey)
                    S.op("act", (lambda j, bk, ot: lambda e: e.activation(
                        out=so[:, j, ot * 512:(ot + 1) * 512], in_=bk[:, :], func=AF.Sigmoid))(j, bk, ot),
                         reads=[bkey], writes=["so%d" % j])
            otiles = [load_tile([(0, 8, 512, wcols(c_w_out[jl], half * 512, 512))]) for half in range(2)]
            def S1(j):
                S.cur_tag = "L%d/S1" % l
                nd = NDs[j % 2]
                for hh in range(4):
                    chk = ck + "_%d" % hh
                    S.op("dve", lambda e: e.tensor_scalar(Cb[:, hh, 0:257], C[:, hh, 0:257],
                                                          cols[:, j, 12 + hh:13 + hh], None, ALU.mult),
                         reads=[chk, "cols%d" % j], writes=["Cb_%d" % hh])
                    pk, pkey = B(hh % 2, 'oS1')
                    S.op("pe", lambda e: e.matmul(pk[:, 0:128], kdT[:, j, hh, :], qrT[:, j, hh, :], start=True, stop=True),
                         reads=["kT%d" % j, "qdT%d" % j], writes=[pkey])
                    S.op("dve", lambda e: e.tensor_tensor(out=Pb[:, hh * 128:(hh + 1) * 128], in0=pk[:, 0:128],
                                                          in1=CM[:, :], op=ALU.mult),
                         reads=[pkey, "CM"], writes=["Pb_%d" % hh])
                    nk, nkey = B(2 + hh % 2, 'oS1')
                    S.op("pe", lambda e: e.matmul(nk[:, 0:257], Pb[:, hh * 128:(hh + 1) * 128],
                                                  vext[:, j, hh, 0:257], start=True, stop=False),
                         reads=["Pb_%d" % hh, "vb%d" % j], writes=[nkey])
                    S.op("pe", lambda e: e.matmul(nk[:, 0:257], qrT[:, j, hh, :], Cb[:, hh, 0:257], start=False, stop=True),
                         reads=["qdT%d" % j, "Cb_%d" % hh], writes=[nkey], safe_same=[nkey])
                    kk, kkey = B(hh % 2, 'oS1')
                    S.op("pe", lambda e: e.matmul(kk[:, 128:385], kdc[:, j, hh * 128:(hh + 1) * 128],
                                                  vext[:, j, hh, 0:257], start=True, stop=True),
                         reads=["kd%d" % j, "vb%d" % j], writes=[kkey])
                    S.op("dve", lambda e: e.scalar_tensor_tensor(
                        out=C[:, hh, 0:257], in0=C[:, hh, 0:257], scalar=cols[:, j, 12 + hh:13 + hh], in1=kk[:, 128:385],
                        op0=ALU.mult, op1=ALU.add),
                         reads=[kkey, chk, "cols%d" % j], writes=[chk])
                    S.op("act", lambda e: e.activation(out=nd[:, hh, 0:257], in_=nk[:, 0:257], func=AF.Copy),
                         reads=[nkey], writes=["NDs%d_%d" % (j % 2, hh)])

            def S2(j):
                S.cur_tag = "L%d/S2" % l
                nd = NDs[j % 2]
                ndk = ["NDs%d_%d" % (j % 2, hh) for hh in range(4)]
                hyk = ["hy_%d" % hh for hh in range(4)]
                S.op("act", lambda e: e.activation(out=low4[:, :], in_=nd[:, :, 256], func=AF.Abs),
                     reads=ndk, writes=["low4"])
                S.op("dve", lambda e: e.tensor_tensor(out=low4[:, :], in0=low4[:, :], in1=cols[:, j, 8:12], op=ALU.max),
                     reads=["low4", "cols%d" % j], writes=["low4"])
                S.op("dve", lambda e: e.reciprocal(rl4[:, :], low4[:, :]), reads=["low4"], writes=["rl4"])
                for hh in range(4):
                    S.op("act", lambda e: e.activation(out=nd[:, hh, 0:256], in_=nd[:, hh, 0:256], func=AF.Copy,
                                                       scale=rl4[:, hh:hh + 1]),
                         reads=[ndk[hh], "rl4"], writes=[ndk[hh]])
                ln_stats([nd[:, hh, 0:256] for hh in range(4)], ndk, 4)
                for hh in range(4):
                    S.op("act", lambda e: e.activation(out=hy[:, hh * 256:(hh + 1) * 256], in_=nd[:, hh, 0:256],
                                                       func=AF.Identity, scale=rs4[:, hh:hh + 1], bias=nm4[:, hh:hh + 1]),
                         reads=[ndk[hh], "rs4", "nm4"], writes=["hy_%d" % hh])
                S.op(_pe("hy"), lambda e: e.tensor_tensor(out=hy[:, :], in0=hy[:, :], in1=CNG[:, :], op=ALU.mult),
                     reads=hyk + ["CNG"], writes=hyk)
                S.op(_pe("hy2"), lambda e: e.tensor_tensor(out=mixed[:, :], in0=hy[:, :], in1=so[:, j, :], op=ALU.mult),
                     reads=hyk + ["so%d" % j], writes=["mixed"])
                out_proj_block(j, otiles, B(4, 'op'), [B(6, 'op'), B(7, 'op')])
                norm_block(j, Gf, "Gf", B(5, 'op'))

            S1(0)
            for j in range(NBLK):
                if j + 1 < NBLK:
                    S1(j + 1)
                S2(j)

        def ffn(l, last, r0):
            S.cur_tag = "L%d/F1" % l
            if not last:
                sp_dma(Gm[:], mix_g[l + 1:l + 2, :].partition_broadcast(128), "Gm")
            hk = ["hnT%d" % j for j in range(NBLK)]
            for ft in range(NFI // 2):
                sl, skey = load_tile([(0, 8, 256, wcols(w_gate[l], ft * 256, 256)),
                                      (2048, 8, 256, wcols(w_up[l], ft * 256, 256))])
                gvw = sl[:, 0:2048].rearrange("p (a b) -> p a b", a=8)
                uvw = sl[:, 2048:4096].rearrange("p (a b) -> p a b", a=8)
                for sub in range(2):
                    fi = ft * 2 + sub
                    pg, pgk = newbank('ffn1')
                    pu, puk = newbank('ffn1')
                    nhb = 2 if ft < KSPLIT else 1
                    wtok = SBT // nhb
                    for (pp, pkk, wvv) in ((pg, pgk, gvw), (pu, puk, uvw)):
                        for hb in range(nhb):
                            rk = ["hnT%d" % q for q in range(hb * NBLK // nhb, (hb + 1) * NBLK // nhb)]
                            for kc in range(8):
                                S.op("pe", lambda e: e.matmul(pp[:, hb * wtok:(hb + 1) * wtok],
                                                              wvv[:, kc, sub * 128:(sub + 1) * 128],
                                                              hnT[:, kc, hb * wtok:(hb + 1) * wtok],
                                                              start=(kc == 0), stop=(kc == 7)),
                                     reads=rk + [skey], writes=[pkk], safe_same=[pkk])
                    S.op("act", (lambda pg: lambda e: e.activation(out=sgt[:, :], in_=pg[:, :], func=AF.Silu))(pg),
                         reads=[pgk], writes=["sgt"])
                    S.op("dve", (lambda pu, fi: lambda e: e.tensor_tensor(out=actT[:, fi, :], in0=sgt[:, :], in1=pu[:, :],
                                                                          op=ALU.mult))(pu, fi),
                         reads=["sgt", puk], writes=["actT%d" % fi])
            S.cur_tag = "L%d/F2" % l
            accs = [[newbank('ffn2') for half in range(2)] for j in range(NBLK)]
            wdv = w_down[l].rearrange("(fi p) n -> p fi n", p=128)
            f0 = 0
            while f0 < NFI:
                nf = min(4, NFI - f0)
                sl, skey = load_tile([(0, nf, 1024, wdv[:, f0:f0 + nf, :])])
                wv = sl[:, 0:nf * 1024].rearrange("p (a b) -> p a b", a=nf)
                for fl in range(nf):
                    fi = f0 + fl
                    for j in range(NBLK):
                        for half in range(2):
                            ak, akey = accs[j][half]
                            S.op("pe", (lambda fi, fl, j, half, ak, wv: lambda e: e.matmul(
                                ak[:, :], actT[:, fi, j * 128:(j + 1) * 128], wv[:, fl, half * 512:(half + 1) * 512],
                                start=(fi == 0), stop=(fi == NFI - 1)))(fi, fl, j, half, ak, wv),
                                 reads=["actT%d" % fi, skey], writes=[akey], safe_same=[akey])
                f0 += nf
            for j in range(NBLK):
                S.cur_tag = "L%d/F3" % l
                for half in range(2):
                    ak, akey = accs[j][half]
                    S.op("dve", lambda e: e.tensor_tensor(
                        out=h[:, j, half * 512:(half + 1) * 512], in0=ak[:, :], in1=h[:, j, half * 512:(half + 1) * 512],
                        op=ALU.add), reads=[akey, hkey(j)], writes=[hkey(j)])
                if last:
                    final_block(j, r0)
                else:
                    norm_block(j, Gm, "Gm", accs[j][0])

        sp_dma(Gfin[:], fin_g[0:1, :].partition_broadcast(128), "Gfin")
        for sb in range(nsb):
            r0 = sb * SBT
            hpar[0] = sb % len(hbufs)
            h = hbufs[hpar[0]]
            for j in range(NBLK):
                S.dma("sp", lambda e: e.dma_start(out=h[:, j, :], in_=x[r0 + j * 128:r0 + (j + 1) * 128, :]),
                      write=hkey(j))
            S.dma("sp", lambda e: e.dma_start(out=CS[:, :, :], in_=cs_d[r0:r0 + SBT, :].rearrange("(j p) n -> p j n", p=128)),
                  write="CS")
            first_mix = ('mix' in parts)
            sp_dma(Gm[:], (mix_g if first_mix else ffn_g)[0:1, :].partition_broadcast(128), "Gm")
            for j in range(NBLK):
                norm_block(j, Gm, "Gm")
            for l in range(depth):
                if 'mix' in parts:
                    if l % 2 == 0:
                        even_mixer(l)
                    else:
                        odd_mixer(l)
                if 'ffn' in parts:
                    ffn(l, l == depth - 1, r0)
        S.emit(nc)
    return nc


_NC_CACHE = {}


def _prep_inputs(inputs, b, nsb):
    seq = nsb * SBT
    tabs, _ = _const_tables(seq)
    f = lambda a: np.ascontiguousarray(np.asarray(a, dtype=np.float32))
    m = {
        "x": f(inputs["x"][b, :seq]),
        "mix_norm_g": f(inputs["mix_norm_g"]),
        "ffn_norm_g": f(inputs["ffn_norm_g"]),
        "final_norm_g": f(inputs["final_norm_g"]).reshape(1, D),
        "ab_w_in": f(inputs["ab_w_in"]),
        "ab_w_out": f(inputs["ab_w_out"]),
        "ret_norm_g": f(inputs["ret_norm_g"]).reshape(2, 512),
        "sgu_ln_g": f(inputs["sgu_ln_g"]),
        "sgu_ln_b": f(inputs["sgu_ln_b"]),
        "sgu_wT": f(np.transpose(np.asarray(inputs["sgu_w"]), (0, 1, 3, 2))),
        "sgu_bT": f(np.transpose(np.asarray(inputs["sgu_b"]), (0, 2, 1))),
        "c_w_in": f(inputs["c_w_in"]),
        "c_w_out": f(inputs["c_w_out"]),
        "c_b_i": f(inputs["c_b_i"]).reshape(2, 4, 1),
        "c_b_f": f(inputs["c_b_f"]).reshape(2, 4, 1),
        "c_norm_g": f(inputs["c_norm_g"]).reshape(2, 1024),
        "ffn_w_gate": f(inputs["ffn_w_gate"]),
        "ffn_w_up": f(inputs["ffn_w_up"]),
        "ffn_w_down": f(inputs["ffn_w_down"]),
    }
    m.update(tabs)
    return m


def run(inputs, nsb=SEQ // SBT, depth=DEPTH, ncores=8, trace=False, parts=('mix', 'ffn')):
    key = (nsb, depth, parts)
    if key not in _NC_CACHE:
        _NC_CACHE[key] = build(nsb, depth, parts)
    nc = _NC_CACHE[key]
    in_maps = [_prep_inputs(inputs, b, nsb) for b in range(ncores)]
    res = run_bass_kernel_spmd(nc, in_maps, core_ids=list(range(ncores)), **({"trace": True} if trace else {}))
    out = np.stack([np.asarray(r["y"], dtype=np.float32) for r in res.results], axis=0)
    return out, res


def kernel(**inputs):
    out, _ = run(inputs)
    return out
```
